# Optimizing a Trainium2 kernel written in Bass

```python
import math
import jax, jax.numpy as jnp
from jax import lax
import numpy as np

D_MODEL = 1024
BATCH = 16
SEQ = 4096
DEPTH = 2

N_BRANCH = 4
BRANCH_W = 256
BLOCK_Q = 128
ROPE_THETA = 10000.0
EPS = 1e-6
POS_OFFSET_MAX = 1024
CONV_CH = BRANCH_W
CONV_K = 31
DIFF_VD = 64
DIFF_QK = 32
DIFF_HEADS = BRANCH_W // DIFF_VD
SB_HD = 64
SB_HEADS = BRANCH_W // SB_HD
DSA_HD = 64
DSA_HEADS = BRANCH_W // DSA_HD
IDX_HEADS = 8
IDX_HD = 32
TOPK_MAX = 256
D_FF = 2816
FFN_CONV_K = 3

SPLIT_SIZES = (
    2 * CONV_CH,
    DIFF_HEADS * 2 * DIFF_QK,
    DIFF_HEADS * 2 * DIFF_QK,
    DIFF_HEADS * DIFF_VD,
    SB_HEADS * SB_HD,
    SB_HEADS * SB_HD,
    SB_HEADS * SB_HD,
    DSA_HEADS * DSA_HD,
    DSA_HD,
    DSA_HD,
    IDX_HEADS * IDX_HD,
    IDX_HD,
    IDX_HEADS,
    N_BRANCH * D_MODEL,
)
D_IN = sum(SPLIT_SIZES)
SPLIT_POINTS = tuple(sum(SPLIT_SIZES[: i + 1]) for i in range(len(SPLIT_SIZES) - 1))

kernel_name = "hybrid_gated_four_mixer_block"


def rmsnorm(x, g):
    x32 = x.astype(jnp.float32)
    y = x32 * lax.rsqrt(jnp.mean(x32 * x32, axis=-1, keepdims=True) + EPS)
    return (y * g.astype(jnp.float32)).astype(x.dtype)


def layernorm(x, g, b):
    x32 = x.astype(jnp.float32)
    mu = jnp.mean(x32, axis=-1, keepdims=True)
    xc = x32 - mu
    y = xc * lax.rsqrt(jnp.mean(xc * xc, axis=-1, keepdims=True) + EPS)
    return (y * g.astype(jnp.float32) + b.astype(jnp.float32)).astype(x.dtype)


def rope(x, pos):
    d = x.shape[-1]
    inv_freq = ROPE_THETA ** (-jnp.arange(0, d, 2, dtype=jnp.float32) / d)
    ang = pos.astype(jnp.float32)[..., None] * inv_freq
    ang = ang.reshape(ang.shape[:2] + (1,) * (x.ndim - 3) + ang.shape[-1:])
    cos, sin = jnp.cos(ang), jnp.sin(ang)
    x1, x2 = jnp.split(x.astype(jnp.float32), 2, axis=-1)
    return jnp.concatenate([x1 * cos - x2 * sin, x2 * cos + x1 * sin], axis=-1).astype(x.dtype)


def causal_dwconv(x, w, b):
    k = w.shape[0]
    y = lax.conv_general_dilated(
        x, w[:, None, :].astype(x.dtype), window_strides=(1,), padding=[(k - 1, 0)],
        dimension_numbers=('NWC', 'WIO', 'NWC'), feature_group_count=x.shape[-1])
    return y + b.astype(x.dtype)


def conformer_conv(u, conv_w, conv_b, ln_g, ln_b):
    a, g = jnp.split(u, 2, axis=-1)
    h = a * jax.nn.sigmoid(g)
    h = causal_dwconv(h, conv_w, conv_b)
    h = layernorm(h, ln_g, ln_b)
    return jax.nn.silu(h)


def diff_attention(q, k, v, pos, lam, lam_init, subln_g):
    q = rope(q, pos)
    k = rope(k, pos)
    b, s_len = q.shape[0], q.shape[1]
    scale = DIFF_QK ** -0.5
    outs = []
    for t0 in range(0, s_len, BLOCK_Q):
        t1 = t0 + BLOCK_Q
        s = jnp.einsum('bqhmd,bkhmd->bhmqk', q[:, t0:t1], k[:, :t1]).astype(jnp.float32) * scale
        mask = jnp.arange(t1)[None, :] <= jnp.arange(t0, t1)[:, None]
        p = jax.nn.softmax(jnp.where(mask, s, -jnp.inf), axis=-1)
        a = p[:, :, 0] - lam * p[:, :, 1]
        outs.append(jnp.einsum('bhqk,bkhd->bqhd', a.astype(v.dtype), v[:, :t1]))
    o = jnp.concatenate(outs, axis=1)
    o = rmsnorm(o, subln_g) * (1.0 - lam_init)
    return o.reshape(b, s_len, -1)


def stick_breaking_attention(q, k, v):
    b, s_len = q.shape[0], q.shape[1]
    scale = SB_HD ** -0.5
    outs = []
    for t0 in range(0, s_len, BLOCK_Q):
        t1 = t0 + BLOCK_Q
        z = jnp.einsum('bqhd,bkhd->bhqk', q[:, t0:t1], k[:, :t1]).astype(jnp.float32) * scale
        strict = jnp.arange(t1)[None, :] < jnp.arange(t0, t1)[:, None]
        log_keep = jnp.where(strict, jax.nn.log_sigmoid(-z), 0.0)
        later = lax.cumsum(log_keep, axis=3, reverse=True) - log_keep
        w = jnp.where(strict, jnp.exp(jax.nn.log_sigmoid(z) + later), 0.0)
        outs.append(jnp.einsum('bhqk,bkhd->bqhd', w.astype(v.dtype), v[:, :t1]))
    o = jnp.concatenate(outs, axis=1)
    return o.reshape(b, s_len, -1)


def dsa_attention(q, k, v, qi, ki, wi, pos):
    q = rope(q, pos)
    k = rope(k, pos)
    qi = rope(qi, pos)
    ki = rope(ki, pos)
    b, s_len = q.shape[0], q.shape[1]
    topk = min(TOPK_MAX, s_len // 4)
    wi = wi.astype(jnp.float32) * IDX_HEADS ** -0.5
    scale = DSA_HD ** -0.5
    gather = jax.vmap(lambda arr, idx: arr[idx])
    outs = []
    for t0 in range(0, s_len, BLOCK_Q):
        t1 = t0 + BLOCK_Q
        kl = min(s_len, max(t1, topk))
        qpos = jnp.arange(t0, t1)
        logits = jnp.einsum('bqhd,bkd->bqhk', qi[:, t0:t1], ki[:, :kl]).astype(jnp.float32) * IDX_HD ** -0.5
        score = jnp.einsum('bqhk,bqh->bqk', jax.nn.relu(logits), wi[:, t0:t1])
        score = jnp.where(jnp.arange(kl)[None, None, :] <= qpos[None, :, None], score, -jnp.inf)
        _, idx = lax.top_k(score, topk)
        k_sel = gather(k, idx)
        v_sel = gather(v, idx)
        s = jnp.einsum('bqhd,bqkd->bqhk', q[:, t0:t1], k_sel).astype(jnp.float32) * scale
        valid = (idx <= qpos[None, :, None])[:, :, None, :]
        p = jax.nn.softmax(jnp.where(valid, s, -jnp.inf), axis=-1)
        outs.append(jnp.einsum('bqhk,bqkd->bqhd', p.astype(v.dtype), v_sel))
    o = jnp.concatenate(outs, axis=1)
    return o.reshape(b, s_len, -1)


def hybrid_mixer(h, pos, w_in, conv_w, conv_b, ln_g, ln_b, lam_q1, lam_k1, lam_q2, lam_k2,
                 lam_init, subln_g, w_branch, w_out):
    b, s_len, _ = h.shape
    z = h @ w_in
    (u_a, q_b, k_b, v_b, q_c, k_c, v_c, q_d, k_d, v_d, qi_d, ki_d, wi_d, gates) = jnp.split(
        z, SPLIT_POINTS, axis=-1)
    o_a = conformer_conv(u_a, conv_w, conv_b, ln_g, ln_b)
    f32 = jnp.float32
    lam = (jnp.exp(jnp.sum(lam_q1.astype(f32) * lam_k1.astype(f32)))
           - jnp.exp(jnp.sum(lam_q2.astype(f32) * lam_k2.astype(f32))) + lam_init)
    o_b = diff_attention(q_b.reshape(b, s_len, DIFF_HEADS, 2, DIFF_QK),
                         k_b.reshape(b, s_len, DIFF_HEADS, 2, DIFF_QK),
                         v_b.reshape(b, s_len, DIFF_HEADS, DIFF_VD), pos, lam, lam_init, subln_g)
    o_c = stick_breaking_attention(q_c.reshape(b, s_len, SB_HEADS, SB_HD),
                                   k_c.reshape(b, s_len, SB_HEADS, SB_HD),
                                   v_c.reshape(b, s_len, SB_HEADS, SB_HD))
    o_d = dsa_attention(q_d.reshape(b, s_len, DSA_HEADS, DSA_HD), k_d, v_d,
                        qi_d.reshape(b, s_len, IDX_HEADS, IDX_HD), ki_d, wi_d, pos)
    g = jax.nn.sigmoid(gates.astype(f32)).astype(h.dtype).reshape(b, s_len, N_BRANCH, D_MODEL)
    merged = g[:, :, 0] * (o_a @ w_branch[0])
    for i, o in ((1, o_b), (2, o_c), (3, o_d)):
        merged = merged + g[:, :, i] * (o @ w_branch[i])
    return merged @ w_out


def conv_ffn(h, w_up, conv_w, conv_b, w_down):
    u = causal_dwconv(h @ w_up, conv_w, conv_b)
    gate, val = jnp.split(u, 2, axis=-1)
    return (jax.nn.silu(gate) * val) @ w_down


def setup_inputs(seed: int = 0) -> dict:
    key = jax.random.key(seed)
    ks = jax.random.split(key, 26)
    f32 = jnp.float32

    def nrm(k, shape, scale):
        return jax.random.normal(k, shape, f32) * scale

    x = nrm(ks[0], (BATCH, SEQ, D_MODEL), 1.0)
    c = nrm(ks[1], (BATCH, D_MODEL), 1.0)
    positions = (jnp.arange(SEQ, dtype=jnp.int32)[None, :]
                 + jax.random.randint(ks[2], (BATCH, 1), 0, POS_OFFSET_MAX, dtype=jnp.int32))
    return {
        'x': x,
        'c': c,
        'positions': positions,
        'ada_w': nrm(ks[3], (DEPTH, D_MODEL, 6 * D_MODEL), 0.5 * D_MODEL ** -0.5),
        'ada_b': nrm(ks[4], (DEPTH, 6 * D_MODEL), 0.01),
        'mix_pre_g': 1.0 + nrm(ks[5], (DEPTH, D_MODEL), 0.05),
        'mix_post_g': 1.0 + nrm(ks[6], (DEPTH, D_MODEL), 0.05),
        'ffn_pre_g': 1.0 + nrm(ks[7], (DEPTH, D_MODEL), 0.05),
        'ffn_post_g': 1.0 + nrm(ks[8], (DEPTH, D_MODEL), 0.05),
        'w_in': nrm(ks[9], (DEPTH, D_MODEL, D_IN), D_MODEL ** -0.5),
        'conv_a_w': nrm(ks[10], (DEPTH, CONV_K, CONV_CH), CONV_K ** -0.5),
        'conv_a_b': nrm(ks[11], (DEPTH, CONV_CH), 0.01),
        'conv_a_ln_g': 1.0 + nrm(ks[12], (DEPTH, CONV_CH), 0.05),
        'conv_a_ln_b': nrm(ks[13], (DEPTH, CONV_CH), 0.01),
        'lam_q1': nrm(ks[14], (DEPTH, DIFF_QK), 0.1),
        'lam_k1': nrm(ks[15], (DEPTH, DIFF_QK), 0.1),
        'lam_q2': nrm(ks[16], (DEPTH, DIFF_QK), 0.1),
        'lam_k2': nrm(ks[17], (DEPTH, DIFF_QK), 0.1),
        'diff_subln_g': 1.0 + nrm(ks[18], (DEPTH, DIFF_VD), 0.05),
        'w_branch': nrm(ks[19], (DEPTH, N_BRANCH, BRANCH_W, D_MODEL), BRANCH_W ** -0.5),
        'w_out': nrm(ks[20], (DEPTH, D_MODEL, D_MODEL), D_MODEL ** -0.5),
        'w_up': nrm(ks[21], (DEPTH, D_MODEL, 2 * D_FF), D_MODEL ** -0.5),
        'ffn_conv_w': nrm(ks[22], (DEPTH, FFN_CONV_K, 2 * D_FF), FFN_CONV_K ** -0.5),
        'ffn_conv_b': nrm(ks[23], (DEPTH, 2 * D_FF), 0.01),
        'w_down': nrm(ks[24], (DEPTH, D_FF, D_MODEL), D_FF ** -0.5),
    }


def reference(x, c, positions, ada_w, ada_b, mix_pre_g, mix_post_g, ffn_pre_g, ffn_post_g,
              w_in, conv_a_w, conv_a_b, conv_a_ln_g, conv_a_ln_b, lam_q1, lam_k1, lam_q2, lam_k2,
              diff_subln_g, w_branch, w_out, w_up, ffn_conv_w, ffn_conv_b, w_down):
    c_act = jax.nn.silu(c)
    for l in range(DEPTH):
        lam_init = 0.8 - 0.6 * math.exp(-0.3 * l)
        mod = c_act @ ada_w[l] + ada_b[l]
        sh1, sc1, g1, sh2, sc2, g2 = [m[:, None, :] for m in jnp.split(mod, 6, axis=-1)]
        h = rmsnorm(x, mix_pre_g[l]) * (1.0 + sc1) + sh1
        y = hybrid_mixer(h, positions, w_in[l], conv_a_w[l], conv_a_b[l], conv_a_ln_g[l],
                         conv_a_ln_b[l], lam_q1[l], lam_k1[l], lam_q2[l], lam_k2[l], lam_init,
                         diff_subln_g[l], w_branch[l], w_out[l])
        x = x + g1 * rmsnorm(y, mix_post_g[l])
        h = rmsnorm(x, ffn_pre_g[l]) * (1.0 + sc2) + sh2
        y = conv_ffn(h, w_up[l], ffn_conv_w[l], ffn_conv_b[l], w_down[l])
        x = x + g2 * rmsnorm(y, ffn_post_g[l])
    return x
```

```python
import math
import contextlib
import numpy as np
import concourse.bass as bass
import concourse.mybir as mybir
from concourse.bass_utils import run_bass_kernel_spmd

F32 = mybir.dt.float32
BF16 = mybir.dt.bfloat16
I32 = mybir.dt.int32
AF = mybir.ActivationFunctionType
ALU = mybir.AluOpType
AX = mybir.AxisListType

D = 1024
DIN = 6824
DFF = 2816
EPS = 1e-6
THETA = 10000.0
NIT = 22
MAGIC = 12582912.0
TWO_PI = 2.0 * math.pi
C1 = 6.28125
C2 = TWO_PI - C1
PI_LO = 3.1415925


class Buf:
    __slots__ = ("w", "r")

    def __init__(self):
        self.w = None
        self.r = []


class Tl:
    __slots__ = ("t", "b")

    def __init__(self, t):
        self.t = t
        self.b = Buf()


def _b(x):
    return x.b if isinstance(x, Tl) else x


class Op:
    __slots__ = ("ex", "q", "emit", "deps", "sig", "seq", "isdma")


class _Rec:
    def __init__(self):
        self.call = None

    def __getattr__(self, name):
        def f(*a, **k):
            self.call = (name, a, k)
            return None
        return f


class Prog:
    NSLOT = 8

    def __init__(self, nc):
        self.nc = nc
        self.ops = []
        self.slot_rr = {}

    def op(self, ex, emit, r=(), w=()):
        o = Op()
        rec = _Rec()
        emit(rec)
        nm_, a_, k_ = rec.call
        o.ex = ex; o.q = ex; o.isdma = False; o.sig = False; o.seq = None
        o.emit = lambda eng, nm_=nm_, a_=a_, k_=k_: getattr(eng, nm_)(*a_, **k_)
        self._deps(o, r, w)
        self.ops.append(o)
        return o

    def dma(self, out, in_, r=(), w=(), q="sp", **kw):
        o = Op()
        o.q = q; o.isdma = True; o.sig = True; o.seq = None
        s = self.slot_rr.get(q, 0)
        self.slot_rr[q] = (s + 1) % self.NSLOT
        o.ex = ("dma", q, s)
        o.emit = lambda eng: eng.dma_start(out=out, in_=in_, **kw)
        self._deps(o, r, w)
        self.ops.append(o)
        return o

    def _deps(self, o, reads, writes):
        deps = []
        for x in reads:
            b = _b(x)
            if b.w is not None:
                deps.append((b.w, True))
        for x in writes:
            b = _b(x)
            if b.w is not None:
                deps.append((b.w, False))
            for rr in b.r:
                deps.append((rr, False))
        o.deps = deps
        for x in reads:
            _b(x).r.append(o)
        for x in writes:
            b = _b(x)
            b.w = o
            b.r = []

    def barrier(self):
        last = {}
        for o in self.ops:
            if o.emit is not None:
                last[o.ex] = o
        prev = list(last.values())
        for q in ("pe", "act", "dve", "pool", "sp"):
            o = Op()
            o.ex = "bar_" + q; o.q = q; o.isdma = False; o.sig = False; o.seq = None
            o.emit = None
            o.deps = [(p, True) for p in prev]
            self.ops.append(o)

    def emit_all(self):
        nc = self.nc
        for o in self.ops:
            for (p, raw) in o.deps:
                if p.isdma:
                    continue
                if p.ex == o.ex and not raw and not o.isdma:
                    continue
                p.sig = True
        cnt = {}
        for o in self.ops:
            if o.isdma:
                cnt[o.ex] = cnt.get(o.ex, 0) + 16
                o.seq = cnt[o.ex]
            elif o.sig:
                cnt[o.ex] = cnt.get(o.ex, 0) + 1
                o.seq = cnt[o.ex]
        prev_on_slot = {}
        for o in self.ops:
            if o.isdma:
                p = prev_on_slot.get(o.ex)
                if p is not None:
                    o.deps.append((p, True))
                prev_on_slot[o.ex] = o
        stack = contextlib.ExitStack()
        sems = {}
        for o in self.ops:
            if o.seq is not None and o.ex not in sems:
                nm = "s_" + ("_".join(str(x) for x in o.ex) if isinstance(o.ex, tuple) else o.ex)
                sems[o.ex] = stack.enter_context(nc.semaphore(nm))
        byq = {"pe": [], "act": [], "dve": [], "pool": [], "sp": []}
        for o in self.ops:
            byq[o.q].append(o)
        self.n_inst = 0

        def run(eng, lst):
            seen = {}
            for o in lst:
                need = {}
                for (p, raw) in o.deps:
                    if (not p.isdma) and p.ex == o.ex and not raw and not o.isdma:
                        continue
                    if p.seq is None:
                        continue
                    if need.get(p.ex, 0) < p.seq:
                        need[p.ex] = p.seq
                for ex, v in need.items():
                    if seen.get(ex, 0) >= v:
                        continue
                    seen[ex] = v
                    eng.wait_ge(sems[ex], v)
                    self.n_inst += 1
                if o.emit is None:
                    continue
                ins = o.emit(eng)
                self.n_inst += 1
                if o.isdma:
                    ins.then_inc(sems[o.ex], 16)
                elif o.sig:
                    ins.then_inc(sems[o.ex], 1)

        with stack, nc.Block() as block:
            @block.tensor
            def _(e):
                run(e, byq["pe"])

            @block.scalar
            def _(e):
                run(e, byq["act"])

            @block.vector
            def _(e):
                run(e, byq["dve"])

            @block.gpsimd
            def _(e):
                run(e, byq["pool"])

            @block.sync
            def _(e):
                run(e, byq["sp"])


def make_consts(S=4096):
    p = np.arange(128)[:, None]
    f = np.arange(128)[None, :]
    c = np.zeros((128, 1024), np.float32)
    c[:, 0:128] = (p == f)
    c[:, 128:256] = (p <= f)
    c[:, 256:384] = (p < f)
    c[:, 384:512] = (p > f)
    c[:, 512:640] = np.where(f <= p, 0.0, -1e30)
    sw32 = np.where(f % 32 < 16, f + 16, f - 16)
    c[:, 640:768] = (p == sw32)
    sw64 = np.where(f % 64 < 32, f + 32, f - 32)
    c[:, 768:896] = (p == sw64)
    pp = np.arange(128)
    c[:, 896] = THETA ** (-(2.0 * (pp % 16)) / 32.0)
    c[:, 897] = THETA ** (-(2.0 * (pp % 32)) / 64.0)
    c[:, 898] = np.where(pp % 32 < 16, -1.0, 1.0)
    c[:, 899] = np.where(pp % 64 < 32, -1.0, 1.0)
    c[:, 900] = EPS
    c[:, 901] = 1.0
    c[:, 902] = 0.0
    c[:, 903] = float(min(256, S // 4)) - 0.5
    for i in range(NIT + 2):
        c[:, 904 + i] = 2.0 ** (-(i + 1))
    return c


class _Stop(Exception):
    pass


def build(S, NSEQ, DEPTH, debug=False, cwin=None, stop=None):
    h = {}
    try:
        _build(h, S, NSEQ, DEPTH, debug, cwin, stop)
    except _Stop:
        pass
    h["P"].barrier()
    h["P"].emit_all()
    return h["nc"], h["P"]


def _build(holder, S, NSEQ, DEPTH, debug, cwin, stop):
    nc = bass.Bass("TRN2", target_bir_lowering=False)
    P = Prog(nc)
    holder["nc"] = nc
    holder["P"] = P
    T = S // 128
    NB = S // 512
    TOPK = min(256, S // 4)
    dbg_kind = "ExternalOutput" if debug else "Internal"

    def din(name, shape, dt=F32):
        return Tl(nc.dram_tensor(name, list(shape), dt, kind="ExternalInput").ap())

    def dscr(name, shape, dt):
        return nc.dram_tensor(name, list(shape), dt, kind=dbg_kind).ap()

    x_in = din("x", [NSEQ, S, D])
    c_in = din("c", [NSEQ, D])
    pos_in = din("positions", [NSEQ, S], I32)
    consts_in = din("consts", [128, 1024])
    W = {}
    for nm, shp in (("ada_w", [DEPTH, D, 6 * D]), ("ada_b", [DEPTH, 6 * D]), ("mix_pre_g", [DEPTH, D]),
                    ("mix_post_g", [DEPTH, D]), ("ffn_pre_g", [DEPTH, D]), ("ffn_post_g", [DEPTH, D]),
                    ("w_in", [DEPTH, D, DIN]), ("conv_a_w", [DEPTH, 31, 256]), ("conv_a_b", [DEPTH, 256]),
                    ("conv_a_ln_g", [DEPTH, 256]), ("conv_a_ln_b", [DEPTH, 256]), ("lam_q1", [DEPTH, 32]),
                    ("lam_k1", [DEPTH, 32]), ("lam_q2", [DEPTH, 32]), ("lam_k2", [DEPTH, 32]),
                    ("diff_subln_g", [DEPTH, 64]), ("w_branch", [DEPTH, 4, 256, D]), ("w_out", [DEPTH, D, D]),
                    ("w_up", [DEPTH, D, 2 * DFF]), ("ffn_conv_w", [DEPTH, 3, 2 * DFF]),
                    ("ffn_conv_b", [DEPTH, 2 * DFF]), ("w_down", [DEPTH, DFF, D])):
        W[nm] = din(nm, shp)
    out_t = Tl(nc.dram_tensor("out", [NSEQ, S, D], F32, kind="ExternalOutput").ap())

    modbuf = Tl(dscr("modbuf", [DEPTH, NSEQ, 6 * D], F32))
    xmid = dscr("xmid", [NSEQ, S, D], F32)
    xlay = dscr("xlay", [NSEQ, S, D], F32)
    hTs = dscr("hTs", [NSEQ, 128, 8, S], BF16)
    fm = {k: dscr(k, [NSEQ, 128, 2, S], BF16) for k in
          ("gluT", "qbT", "kbT", "qcT", "kcT", "qdT", "qiT", "oaT", "obT", "ocT", "odT")}
    kdT = dscr("kdT", [NSEQ, 64, S], BF16)
    kiT = dscr("kiT", [NSEQ, 32, S], BF16)
    vbs = dscr("vbs", [NSEQ, S, 256], BF16)
    vcs = dscr("vcs", [NSEQ, S, 256], BF16)
    vds = dscr("vds", [NSEQ, S, 64], BF16)
    wis = dscr("wis", [NSEQ, S, 8], F32)
    aTs = dscr("aTs", [NSEQ, 128, 22, S], BF16)
    hb = {}

    def HB(name, b):
        k = (name, b)
        if k not in hb:
            hb[k] = Buf()
        return hb[k]

    SB_BASE = (nc.sbuf_base + 63) // 64 * 64
    SB_TOP = nc.sbuf_top
    off = [SB_BASE]
    uid = [0]

    def sb(shape, dt, name="t"):
        esz = 4 if dt in (F32, I32) else 2
        nbytes = int(np.prod(shape[1:])) * esz
        nbytes = (nbytes + 63) // 64 * 64
        uid[0] += 1
        t = nc.alloc_sbuf_tensor_at(f"{name}_{uid[0]}", list(shape), dt, offset=off[0])
        off[0] += nbytes
        assert off[0] <= SB_TOP, f"SBUF overflow at {name}: {off[0]} > {SB_TOP}"
        return Tl(t)

    ps = [Tl(nc.alloc_psum_tensor(f"psb{i}", [128, 512], F32)) for i in range(8)]

    def psbf(i):
        return ps[i].t[:, :].bitcast(BF16)

    cst = sb([128, 1024], F32, "cst")
    P.dma(cst.t[:, :], consts_in.t[:, :], w=[cst])
    cbf = sb([128, 896], BF16, "cbf")
    P.op("dve", lambda e: e.tensor_copy(out=cbf.t[:, :], in_=cst.t[:, 0:896]), r=[cst], w=[cbf])
    ones_bf = sb([128, 128], BF16, "ones")
    P.op("pool", lambda e: e.memset(ones_bf.t[:, :], 1.0), w=[ones_bf])
    ones32 = sb([128, 128], F32, "ones32")
    P.op("pool", lambda e: e.memset(ones32.t[:, :], 1.0), w=[ones32])
    ident_bf = cbf.t[:, 0:128]
    le_bf = cbf.t[:, 128:256]
    lt_bf = cbf.t[:, 256:384]
    gt_bf = cbf.t[:, 384:512]
    negm = cst.t[:, 512:640]
    perm32 = cbf.t[:, 640:768]
    perm64 = cbf.t[:, 768:896]
    invf = {32: cst.t[:, 896:897], 64: cst.t[:, 897:898]}
    sgn = {32: cst.t[:, 898:899], 64: cst.t[:, 899:900]}
    eps_c = cst.t[:, 900:901]
    one_c = cst.t[:, 901:902]
    pow2 = cst.t[:, 904:904 + NIT + 2]
    topk_c = cst.t[:, 903:904]
    zero_c = cst.t[:, 902:903]
    PERSIST = off[0]

    import os as _os
    SUB = int(_os.environ.get('SUB', '0'))

    def ck(n):
        if SUB == n:
            raise _Stop()

    stage_no = [0]

    def stage_reset():
        P.barrier()
        stage_no[0] += 1
        if stop is not None and stage_no[0] >= stop:
            raise _Stop()
        print('stage sbuf bytes', off[0], 'of', SB_TOP, flush=True)
        off[0] = PERSIST

    def act(out, in_, func, r, w, **kw):
        P.op("act", lambda e: e.activation(out=out, in_=in_, func=func, **kw), r=r, w=w)

    def rstd_from_ss(ss, n, r, w_tmp):
        act(ss, ss, AF.Ln, r, w_tmp, scale=1.0 / n, bias=eps_c[0:ss.shape[0], :])
        act(ss, ss, AF.Exp, w_tmp, w_tmp, scale=-0.5)

    def load_cast(dst_tl, dst_ap_fn, src_ap_fn, pieces, stg, eng_cycle):
        for i, (dargs, sargs) in enumerate(pieces):
            st = stg[i % len(stg)]
            d_ap = dst_ap_fn(*dargs)
            s_ap = src_ap_fn(*sargs)
            shp = list(d_ap.shape)
            if len(shp) == 3:
                st_ap = st.t[0:shp[0], 0:shp[1], 0:shp[2]]
            else:
                st_ap = st.t[0:shp[0], 0, 0:shp[1]]
            P.dma(st_ap, s_ap, w=[st])
            ex = eng_cycle[i % len(eng_cycle)]
            P.op(ex, lambda e, o=d_ap, a=st_ap: e.tensor_copy(out=o, in_=a), r=[st], w=[dst_tl])

    cT = sb([128, 8, NSEQ], F32, "cT")
    for b_ in range(NSEQ):
        P.dma(cT.t[:, :, b_], c_in.t[b_].rearrange("(c p) -> p c", p=128), w=[cT], allow_slow_non_contiguous=True)
    act(cT.t[:, :, :], cT.t[:, :, :], AF.Silu, [cT], [cT])
    adst = [sb([128, 8, 512], F32, "adst") for _ in range(2)]
    modsb = sb([NSEQ, 6 * D], F32, "modsb")
    adab = sb([NSEQ, 6 * D], F32, "adab")
    for l in range(DEPTH):
        P.dma(adab.t[:, :], W["ada_b"].t[l:l + 1, :].partition_broadcast(NSEQ), w=[adab])
        wv = W["ada_w"].t[l].rearrange("(kc p) n -> p kc n", p=128)
        for nb in range(12):
            st = adst[nb % 2]
            P.dma(st.t[:, :, :], wv[:, :, nb * 512:(nb + 1) * 512], w=[st])
            pb = ps[nb % 2]
            for kc in range(8):
                P.op("pe", lambda e, st=st, pb=pb, kc=kc: e.matmul(
                    pb.t[0:NSEQ, :], lhsT=cT.t[:, kc, :], rhs=st.t[:, kc, :], start=(kc == 0), stop=(kc == 7)),
                    r=[cT, st], w=[pb])
            P.op("dve", lambda e, pb=pb, nb=nb: e.tensor_tensor(
                out=modsb.t[:, nb * 512:(nb + 1) * 512], in0=pb.t[0:NSEQ, :],
                in1=adab.t[:, nb * 512:(nb + 1) * 512], op=ALU.add), r=[pb, adab], w=[modsb])
        P.dma(modbuf.t[l], modsb.t[:, :], r=[modsb], w=[modbuf])
    stage_reset()

    def col8(ap1d):
        return ap1d.rearrange("(c p) -> p c", p=128)

    def norm_transpose(xt, hT, tt, Acol, Bcol, xs, ss, junk, pbank):
        act(junk.t[:, :], xt.t[:, :], AF.Square, [xt], [junk, ss], accum_out=ss.t[:, 0:1])
        rstd_from_ss(ss.t[:, 0:1], D, [ss], [ss])
        P.op("dve", lambda e: e.tensor_scalar(out=xs.t[:, :], in0=xt.t[:, :], scalar1=ss.t[:, 0:1], scalar2=None,
                                              op0=ALU.mult), r=[xt, ss], w=[xs])
        pv = psbf(pbank)
        for c in range(8):
            P.op("pe", lambda e, c=c: e.transpose(pv[:, c * 128:(c + 1) * 128], xs.t[:, c * 128:(c + 1) * 128],
                                                  ident_bf), r=[xs, cbf], w=[ps[pbank]])
        for c in range(8):
            act(hT.t[:, c, tt * 128:(tt + 1) * 128], pv[:, c * 128:(c + 1) * 128], AF.Identity,
                [ps[pbank], Acol, Bcol], [hT], scale=Acol.t[:, c:c + 1], bias=Bcol.t[:, c:c + 1])

    def post_norm_residual(ysb, xt, GG, ss, junk, dst_ap, dst_buf):
        act(junk.t[:, :], ysb.t[:, :], AF.Square, [ysb], [junk, ss], accum_out=ss.t[:, 0:1])
        rstd_from_ss(ss.t[:, 0:1], D, [ss], [ss])
        P.op("dve", lambda e: e.scalar_tensor_tensor(out=ysb.t[:, :], in0=ysb.t[:, :], scalar=ss.t[:, 0:1],
                                                     in1=GG.t[:, :], op0=ALU.mult, op1=ALU.mult),
             r=[ysb, ss, GG], w=[ysb])
        P.op("dve", lambda e: e.tensor_tensor(out=ysb.t[:, :], in0=ysb.t[:, :], in1=xt.t[:, :], op=ALU.add),
             r=[ysb, xt], w=[ysb])
        P.dma(dst_ap, ysb.t[:, :], r=[ysb], w=[dst_buf])

    def mod_cols(l, b, k):
        return col8(modbuf.t[l, b, k * D:(k + 1) * D])

    def make_AB(l, b, gname, ksh, ksc, Acol, Bcol, tmp):
        P.dma(Bcol.t[:, :], mod_cols(l, b, ksh), r=[modbuf], w=[Bcol], allow_slow_non_contiguous=True)
        P.dma(tmp.t[:, :], mod_cols(l, b, ksc), r=[modbuf], w=[tmp], allow_slow_non_contiguous=True)
        P.dma(Acol.t[:, :], col8(W[gname].t[l]), w=[Acol], allow_slow_non_contiguous=True)
        P.op("dve", lambda e: e.scalar_tensor_tensor(out=Acol.t[:, :], in0=tmp.t[:, :], scalar=1.0, in1=Acol.t[:, :],
                                                     op0=ALU.add, op1=ALU.mult), r=[tmp, Acol], w=[Acol])

    def make_GG(l, b, gname, kg, GG, tmpb):
        P.dma(GG.t[:, :], W[gname].t[l:l + 1, :].partition_broadcast(128), w=[GG])
        P.dma(tmpb.t[:, :], modbuf.t[l, b:b + 1, kg * D:(kg + 1) * D].partition_broadcast(128), r=[modbuf], w=[tmpb])
        P.op("dve", lambda e: e.tensor_tensor(out=GG.t[:, :], in0=GG.t[:, :], in1=tmpb.t[:, :], op=ALU.mult),
             r=[GG, tmpb], w=[GG])

    for l in range(DEPTH):
        x_src = x_in.t if l == 0 else xlay
        x_dst = out_t.t if l == DEPTH - 1 else xlay
        lam_init = 0.8 - 0.6 * math.exp(-0.3 * l)

        W1 = sb([128, 8, 2728], BF16, "W1")
        stg = [sb([128, 8, 512], F32, "stg") for _ in range(2)]
        wv = W["w_in"].t[l].rearrange("(kc p) n -> p kc n", p=128)
        pieces = []
        c0 = 0
        while c0 < 2728:
            n = min(512, 2728 - c0)
            pieces.append(((c0, n), (c0, n)))
            c0 += n
        load_cast(W1, lambda c0, n: W1.t[:, :, c0:c0 + n], lambda c0, n: wv[:, :, c0:c0 + n], pieces, stg,
                  ["pool", "dve"])
        Acol = sb([128, 8], F32, "Acol"); Bcol = sb([128, 8], F32, "Bcol"); tmpc = sb([128, 8], F32, "tmpc")
        xts = [sb([128, D], F32, "xt") for _ in range(2)]
        xs = sb([128, D], BF16, "xs"); junk = sb([128, D], BF16, "junk"); ss = sb([128, 2], F32, "ss")
        hTb = [sb([128, 8, 512], BF16, "hT") for _ in range(2)]
        posi = sb([128, 512], I32, "posi"); posf = sb([128, 512], F32, "posf")
        ang = sb([128, 512], F32, "ang"); kk = sb([128, 512], F32, "kk"); rr = sb([128, 512], F32, "rr")
        r2 = sb([128, 512], F32, "r2"); mm_ = sb([128, 512], F32, "mm")
        cosT = {32: sb([128, 512], F32, "cos32"), 64: sb([128, 512], F32, "cos64")}
        sinT = {32: sb([128, 512], F32, "sin32"), 64: sb([128, 512], F32, "sin64")}
        qsb = [sb([128, 512], BF16, "qsb") for _ in range(2)]
        t1 = sb([128, 512], F32, "t1"); t2 = sb([128, 512], F32, "t2")
        outb = [sb([128, 512], BF16, "outb") for _ in range(3)]
        sg = sb([128, 512], F32, "sg")
        vtm = [sb([128, 512], BF16, "vtm") for _ in range(2)]
        vtd = [sb([128, 64], BF16, "vtd") for _ in range(2)]
        wtm = [sb([128, 8], F32, "wtm") for _ in range(2)]
        ob_i = [0]

        def next_outb():
            ob_i[0] += 1
            return outb[ob_i[0] % 3]

        for b in range(NSEQ):
            make_AB(l, b, "mix_pre_g", 0, 1, Acol, Bcol, tmpc)
            for tb in range(NB):
                t0 = tb * 512
                hT = hTb[(b * NB + tb) % 2]
                for tt in range(4):
                    xt = xts[tt % 2]
                    P.dma(xt.t[:, :], x_src[b, t0 + tt * 128:t0 + (tt + 1) * 128, :],
                          r=[HB("x%d" % l, b)], w=[xt])
                    norm_transpose(xt, hT, tt, Acol, Bcol, xs, ss, junk, 7)
                P.dma(hTs[b][:, :, t0:t0 + 512], hT.t[:, :, :], r=[hT], w=[HB("hTs", b)])
                ck(1)
                P.dma(posi.t[:, :], pos_in.t[b:b + 1, t0:t0 + 512].partition_broadcast(128), w=[posi])
                P.op("dve", lambda e: e.tensor_copy(out=posf.t[:, :], in_=posi.t[:, :]), r=[posi], w=[posf])
                for dd in (32, 64):
                    P.op("dve", lambda e, dd=dd: e.tensor_scalar(out=ang.t[:, :], in0=posf.t[:, :], scalar1=invf[dd],
                                                               scalar2=None, op0=ALU.mult), r=[posf, cst], w=[ang])
                    P.op("dve", lambda e: e.tensor_scalar(out=kk.t[:, :], in0=ang.t[:, :], scalar1=1.0 / TWO_PI,
                                                          scalar2=MAGIC, op0=ALU.mult, op1=ALU.add), r=[ang], w=[kk])
                    P.op("dve", lambda e: e.tensor_scalar(out=kk.t[:, :], in0=kk.t[:, :], scalar1=MAGIC, scalar2=None,
                                                          op0=ALU.subtract), r=[kk], w=[kk])
                    P.op("dve", lambda e: e.scalar_tensor_tensor(out=rr.t[:, :], in0=kk.t[:, :], scalar=-C1,
                                                                 in1=ang.t[:, :], op0=ALU.mult, op1=ALU.add),
                         r=[kk, ang], w=[rr])
                    P.op("dve", lambda e: e.scalar_tensor_tensor(out=rr.t[:, :], in0=kk.t[:, :], scalar=-C2,
                                                                 in1=rr.t[:, :], op0=ALU.mult, op1=ALU.add),
                         r=[kk, rr], w=[rr])
                    P.op("dve", lambda e: e.tensor_scalar(out=rr.t[:, :], in0=rr.t[:, :], scalar1=PI_LO,
                                                          scalar2=-PI_LO, op0=ALU.min, op1=ALU.max), r=[rr], w=[rr])
                    act(sinT[dd].t[:, :], rr.t[:, :], AF.Sin, [rr, cst], [sinT[dd]], scale=sgn[dd])
                    P.op("dve", lambda e: e.tensor_scalar(out=r2.t[:, :], in0=rr.t[:, :], scalar1=math.pi / 2,
                                                          scalar2=None, op0=ALU.add), r=[rr], w=[r2])
                    P.op("dve", lambda e: e.tensor_scalar(out=mm_.t[:, :], in0=r2.t[:, :], scalar1=math.pi,
                                                          scalar2=-TWO_PI, op0=ALU.is_gt, op1=ALU.mult),
                         r=[r2], w=[mm_])
                    P.op("dve", lambda e: e.tensor_tensor(out=r2.t[:, :], in0=r2.t[:, :], in1=mm_.t[:, :],
                                                          op=ALU.add), r=[r2, mm_], w=[r2])
                    P.op("dve", lambda e: e.tensor_scalar(out=r2.t[:, :], in0=r2.t[:, :], scalar1=PI_LO,
                                                          scalar2=-PI_LO, op0=ALU.min, op1=ALU.max), r=[r2], w=[r2])
                    act(cosT[dd].t[:, :], r2.t[:, :], AF.Sin, [r2], [cosT[dd]])

                ck(2)
                bank = [0]

                def proj_fm(c0, m):
                    pb = ps[bank[0] % 4]
                    bank[0] += 1
                    for kc in range(8):
                        P.op("pe", lambda e, pb=pb, kc=kc: e.matmul(pb.t[0:m, :], lhsT=W1.t[:, kc, c0:c0 + m],
                                                                    rhs=hT.t[:, kc, :], start=(kc == 0),
                                                                    stop=(kc == 7)), r=[W1, hT], w=[pb])
                    return pb

                def store(dst_ap, srct, m, key):
                    P.dma(dst_ap, srct.t[0:m, :], r=[srct], w=[HB(key, b)])

                for cc in range(2):
                    pa = proj_fm(cc * 128, 128)
                    pg = proj_fm(256 + cc * 128, 128)
                    act(sg.t[:, :], pg.t[:, :], AF.Sigmoid, [pg], [sg])
                    ob = next_outb()
                    P.op("dve", lambda e, pa=pa, ob=ob: e.tensor_tensor(out=ob.t[:, :], in0=pa.t[:, :], in1=sg.t[:, :],
                                                                        op=ALU.mult), r=[pa, sg], w=[ob])
                    store(fm["gluT"][b][:, cc, t0:t0 + 512], ob, 128, "gluT")

                ck(3)

                def roped(c0, m, dd, dst_ap, key):
                    pq = proj_fm(c0, m)
                    q = qsb[bank[0] % 2]
                    act(q.t[0:m, :], pq.t[0:m, :], AF.Identity, [pq], [q])
                    ck(8)
                    pw = ps[4 + bank[0] % 2]
                    pm = perm32 if dd == 32 else perm64
                    P.op("pe", lambda e: e.matmul(pw.t[0:m, :], lhsT=pm[0:m, 0:m], rhs=q.t[0:m, :], start=True,
                                                  stop=True), r=[q, cbf], w=[pw])
                    ck(9)
                    P.op("dve", lambda e: e.tensor_tensor(out=t1.t[0:m, :], in0=q.t[0:m, :], in1=cosT[dd].t[0:m, :],
                                                          op=ALU.mult), r=[q, cosT[dd]], w=[t1])
                    P.op("dve", lambda e: e.tensor_tensor(out=t2.t[0:m, :], in0=pw.t[0:m, :], in1=sinT[dd].t[0:m, :],
                                                          op=ALU.mult), r=[pw, sinT[dd]], w=[t2])
                    ob = next_outb()
                    P.op("dve", lambda e: e.tensor_tensor(out=ob.t[0:m, :], in0=t1.t[0:m, :], in1=t2.t[0:m, :],
                                                          op=ALU.add), r=[t1, t2], w=[ob])
                    store(dst_ap, ob, m, key)

                def plain(c0, m, dst_ap, key):
                    pq = proj_fm(c0, m)
                    ob = next_outb()
                    act(ob.t[0:m, :], pq.t[0:m, :], AF.Identity, [pq], [ob])
                    store(dst_ap, ob, m, key)

                for cc in range(2):
                    roped(512 + cc * 128, 128, 32, fm["qbT"][b][:, cc, t0:t0 + 512], "qbT")
                    ck(5)
                    roped(768 + cc * 128, 128, 32, fm["kbT"][b][:, cc, t0:t0 + 512], "kbT")
                    ck(6)
                    plain(1280 + cc * 128, 128, fm["qcT"][b][:, cc, t0:t0 + 512], "qcT")
                    ck(7)
                    plain(1536 + cc * 128, 128, fm["kcT"][b][:, cc, t0:t0 + 512], "kcT")
                    roped(2048 + cc * 128, 128, 64, fm["qdT"][b][:, cc, t0:t0 + 512], "qdT")
                    roped(2432 + cc * 128, 128, 32, fm["qiT"][b][:, cc, t0:t0 + 512], "qiT")
                roped(2304, 64, 64, kdT[b][:, t0:t0 + 512], "kdT")
                roped(2688, 32, 32, kiT[b][:, t0:t0 + 512], "kiT")
                ck(4)
                for tt in range(4):
                    pv_ = ps[6]
                    for kc in range(8):
                        P.op("pe", lambda e, kc=kc, tt=tt: e.matmul(pv_.t[:, 0:256], lhsT=hT.t[:, kc, tt * 128:(tt + 1) * 128],
                                                                    rhs=W1.t[:, kc, 1024:1280], start=(kc == 0),
                                                                    stop=(kc == 7)), r=[W1, hT], w=[pv_])
                    for kc in range(8):
                        P.op("pe", lambda e, kc=kc, tt=tt: e.matmul(pv_.t[:, 256:512], lhsT=hT.t[:, kc, tt * 128:(tt + 1) * 128],
                                                                    rhs=W1.t[:, kc, 1792:2048], start=(kc == 0),
                                                                    stop=(kc == 7)), r=[W1, hT], w=[pv_])
                    v = vtm[tt % 2]
                    act(v.t[:, :], pv_.t[:, :], AF.Identity, [pv_], [v])
                    P.dma(vbs[b, t0 + tt * 128:t0 + (tt + 1) * 128, :], v.t[:, 0:256], r=[v], w=[HB("vbs", b)])
                    P.dma(vcs[b, t0 + tt * 128:t0 + (tt + 1) * 128, :], v.t[:, 256:512], r=[v], w=[HB("vcs", b)])
                    pv2 = ps[5]
                    for kc in range(8):
                        P.op("pe", lambda e, kc=kc, tt=tt: e.matmul(pv2.t[:, 0:64], lhsT=hT.t[:, kc, tt * 128:(tt + 1) * 128],
                                                                    rhs=W1.t[:, kc, 2368:2432], start=(kc == 0),
                                                                    stop=(kc == 7)), r=[W1, hT], w=[pv2])
                    for kc in range(8):
                        P.op("pe", lambda e, kc=kc, tt=tt: e.matmul(pv2.t[:, 64:72], lhsT=hT.t[:, kc, tt * 128:(tt + 1) * 128],
                                                                    rhs=W1.t[:, kc, 2720:2728], start=(kc == 0),
                                                                    stop=(kc == 7)), r=[W1, hT], w=[pv2])
                    vd_ = vtd[tt % 2]; wt_ = wtm[tt % 2]
                    act(vd_.t[:, :], pv2.t[:, 0:64], AF.Identity, [pv2], [vd_])
                    act(wt_.t[:, :], pv2.t[:, 64:72], AF.Identity, [pv2], [wt_])
                    P.dma(vds[b, t0 + tt * 128:t0 + (tt + 1) * 128, :], vd_.t[:, :], r=[vd_], w=[HB("vds", b)])
                    P.dma(wis[b, t0 + tt * 128:t0 + (tt + 1) * 128, :], wt_.t[:, :], r=[wt_], w=[HB("wis", b)])
        stage_reset()

        cw = sb([128, 2, 31], F32, "cw")
        for cc in range(2):
            P.dma(cw.t[:, cc, :], W["conv_a_w"].t[l][:, cc * 128:(cc + 1) * 128].rearrange("j p -> p j"), w=[cw],
                  allow_slow_non_contiguous=True)
        cb3 = sb([128, 6], F32, "cb3")
        for i, nm in enumerate(("conv_a_b", "conv_a_ln_g", "conv_a_ln_b")):
            P.dma(cb3.t[:, 2 * i:2 * i + 2], W[nm].t[l].rearrange("(c p) -> p c", p=128), w=[cb3],
                  allow_slow_non_contiguous=True)
        dg = sb([128, 62, 128], BF16, "dg")
        for cc in range(2):
            for j in range(31):
                P.op("dve", lambda e, cc=cc, j=j: e.tensor_scalar(out=dg.t[:, cc * 31 + j, :], in0=cst.t[:, 0:128],
                                                                 scalar1=cw.t[:, cc, j:j + 1], scalar2=None,
                                                                 op0=ALU.mult), r=[cst, cw], w=[dg])
        o256 = sb([128, 128], F32, "o256")
        P.op("pool", lambda e: e.memset(o256.t[:, :], 1.0 / 256.0), w=[o256])
        gl = sb([128, 2, S + 32], BF16, "gl")
        cv = [sb([128, 2, 512], F32, "cv") for _ in range(2)]
        sq = sb([128, 2, 512], F32, "sq")
        mean_sb = sb([128, 512], F32, "mean_sb")
        m2 = sb([128, 512], F32, "m2"); var = sb([128, 512], F32, "var"); xc = sb([128, 512], F32, "xc")
        oa = [sb([128, 512], BF16, "oa") for _ in range(2)]
        for b in range(NSEQ):
            P.op("pool", lambda e: e.memset(gl.t[:, :, 0:32], 0.0), w=[gl])
            P.dma(gl.t[:, :, 32:32 + S], fm["gluT"][b][:, :, :], r=[HB("gluT", b)], w=[gl])
            for tb in range(NB):
                t0 = tb * 512
                cvt = cv[tb % 2]
                for cc in range(2):
                    pb = ps[cc]
                    for j in range(31):
                        P.op("pe", lambda e, cc=cc, j=j, pb=pb: e.matmul(
                            pb.t[:, :], lhsT=dg.t[:, cc * 31 + j, :], rhs=gl.t[:, cc, t0 + 2 + j:t0 + 2 + j + 512],
                            start=(j == 0), stop=(j == 30)), r=[dg, gl], w=[pb])
                    act(cvt.t[:, cc, :], pb.t[:, :], AF.Identity, [pb, cb3], [cvt], bias=cb3.t[:, cc:cc + 1])
                    act(sq.t[:, cc, :], cvt.t[:, cc, :], AF.Square, [cvt], [sq])
                pm_ = ps[2]; pe2 = ps[3]
                for cc in range(2):
                    P.op("pe", lambda e, cc=cc: e.matmul(pm_.t[:, :], lhsT=o256.t[:, :], rhs=cvt.t[:, cc, :],
                                                         start=(cc == 0), stop=(cc == 1)), r=[o256, cvt], w=[pm_])
                for cc in range(2):
                    P.op("pe", lambda e, cc=cc: e.matmul(pe2.t[:, :], lhsT=o256.t[:, :], rhs=sq.t[:, cc, :],
                                                         start=(cc == 0), stop=(cc == 1)), r=[o256, sq], w=[pe2])
                act(mean_sb.t[:, :], pm_.t[:, :], AF.Identity, [pm_], [mean_sb])
                act(m2.t[:, :], mean_sb.t[:, :], AF.Square, [mean_sb], [m2])
                P.op("dve", lambda e: e.tensor_tensor(out=var.t[:, :], in0=pe2.t[:, :], in1=m2.t[:, :], op=ALU.subtract),
                     r=[pe2, m2], w=[var])
                act(var.t[:, :], var.t[:, :], AF.Ln, [var], [var], bias=eps_c)
                act(var.t[:, :], var.t[:, :], AF.Exp, [var], [var], scale=-0.5)
                for cc in range(2):
                    P.op("dve", lambda e, cc=cc: e.tensor_tensor(out=xc.t[:, :], in0=cvt.t[:, cc, :], in1=mean_sb.t[:, :],
                                                                 op=ALU.subtract), r=[cvt, mean_sb], w=[xc])
                    P.op("dve", lambda e: e.tensor_tensor(out=xc.t[:, :], in0=xc.t[:, :], in1=var.t[:, :], op=ALU.mult),
                         r=[xc, var], w=[xc])
                    o_ = oa[cc]
                    act(o_.t[:, :], xc.t[:, :], AF.Silu, [xc, cb3], [o_], scale=cb3.t[:, 2 + cc:3 + cc],
                        bias=cb3.t[:, 4 + cc:5 + cc])
                    P.dma(fm["oaT"][b][:, cc, t0:t0 + 512], o_.t[:, :], r=[o_], w=[HB("oaT", b)])
        stage_reset()

        lamt = sb([128, 8], F32, "lamt")
        lq = sb([128, 4, 32], F32, "lq")
        for i, nm in enumerate(("lam_q1", "lam_k1", "lam_q2", "lam_k2")):
            P.dma(lq.t[:, i, :], W[nm].t[l:l + 1, :].partition_broadcast(128), w=[lq])
        lj = sb([128, 32], F32, "lj")
        P.op("pool", lambda e: e.memset(lamt.t[:, :], 0.0), w=[lamt])
        for i in range(2):
            P.op("dve", lambda e, i=i: e.tensor_tensor(out=lj.t[:, :], in0=lq.t[:, 2 * i, :], in1=lq.t[:, 2 * i + 1, :],
                                                       op=ALU.mult), r=[lq], w=[lj])
            P.op("dve", lambda e, i=i: e.tensor_reduce(out=lamt.t[:, i:i + 1], in_=lj.t[:, :], axis=AX.X, op=ALU.add),
                 r=[lj, lamt], w=[lamt])
            act(lamt.t[:, i:i + 1], lamt.t[:, i:i + 1], AF.Exp, [lamt], [lamt])
        P.op("dve", lambda e: e.tensor_tensor(out=lamt.t[:, 2:3], in0=lamt.t[:, 1:2], in1=lamt.t[:, 0:1],
                                              op=ALU.subtract), r=[lamt], w=[lamt])
        P.op("dve", lambda e: e.tensor_scalar(out=lamt.t[:, 2:3], in0=lamt.t[:, 2:3], scalar1=-lam_init, scalar2=None,
                                              op0=ALU.add), r=[lamt], w=[lamt])
        neglam = lamt.t[:, 2:3]
        gsub = sb([128, 1], F32, "gsub")
        for hh in range(2):
            P.dma(gsub.t[hh * 64:(hh + 1) * 64, :], W["diff_subln_g"].t[l].rearrange("(d o) -> d o", o=1), w=[gsub],
                  allow_slow_non_contiguous=True)
        P.op("dve", lambda e: e.tensor_scalar(out=gsub.t[:, :], in0=gsub.t[:, :], scalar1=1.0 - lam_init, scalar2=None,
                                              op0=ALU.mult), r=[gsub], w=[gsub])
        o64 = sb([128, 64], F32, "o64")
        P.op("pool", lambda e: e.memset(o64.t[:, :], 1.0 / 64.0), w=[o64])
        qT = sb([128, 2, S], BF16, "qT"); kT = sb([128, 2, S], BF16, "kT"); vv = sb([128, T, 256], BF16, "vv")
        pt = [sb([128, 512], BF16, "pt") for _ in range(3)]
        rc = [sb([64, 512], F32, "rc") for _ in range(2)]
        tO = [sb([64, 512], F32, "tO") for _ in range(2)]
        od = sb([64, 512], F32, "od"); osq = sb([64, 512], F32, "osq"); rs = sb([64, 512], F32, "rs")
        obo = [sb([64, 512], BF16, "obo") for _ in range(2)]
        pti = [0]
        SC_B = 32 ** -0.5
        for b in range(NSEQ):
            P.dma(qT.t[:, :, :], fm["qbT"][b][:, :, :], r=[HB("qbT", b)], w=[qT])
            P.dma(kT.t[:, :, :], fm["kbT"][b][:, :, :], r=[HB("kbT", b)], w=[kT])
            P.dma(vv.t[:, :, :], vbs[b].rearrange("(t p) f -> p t f", p=128), r=[HB("vbs", b)], w=[vv])
            for h in range(4):
                cc = h // 2
                for qb in range(NB):
                    q0 = qb * 512
                    nkt = qb * 4 + 4
                    for kt in range(nkt):
                        i = kt - qb * 4
                        cs = max(i, 0) * 128
                        for m in range(2):
                            j = (h % 2) * 2 + m
                            kw = {"tile_position": (96, 0)} if j == 3 else {}
                            pS = ps[(kt * 2 + m) % 3]
                            P.op("pe", lambda e, pS=pS, j=j, kw=kw, kt=kt, cs=cs: e.matmul(
                                pS.t[:, cs:512], lhsT=kT.t[32 * j:32 * j + 32, cc, kt * 128:(kt + 1) * 128],
                                rhs=qT.t[32 * j:32 * j + 32, cc, q0 + cs:q0 + 512], start=True, stop=True, **kw),
                                r=[kT, qT], w=[pS])
                            pti[0] += 1
                            p_ = pt[pti[0] % 3]
                            act(p_.t[:, cs:512], pS.t[:, cs:512], AF.Exp, [pS], [p_], scale=SC_B)
                            if i >= 0:
                                P.op("dve", lambda e, p_=p_, cs=cs: e.tensor_tensor(
                                    out=p_.t[:, cs:cs + 128], in0=p_.t[:, cs:cs + 128], in1=le_bf, op=ALU.mult),
                                    r=[p_, cbf], w=[p_])
                            pO = ps[3 + m]; pSm = ps[5 + m]
                            P.op("pe", lambda e, pO=pO, p_=p_, kt=kt, cs=cs: e.matmul(
                                pO.t[0:64, cs:512], lhsT=vv.t[:, kt, h * 64:(h + 1) * 64], rhs=p_.t[:, cs:512],
                                start=(kt == 0), stop=(kt == nkt - 1)), r=[vv, p_], w=[pO])
                            P.op("pe", lambda e, pSm=pSm, p_=p_, kt=kt, cs=cs: e.matmul(
                                pSm.t[0:64, cs:512], lhsT=ones_bf.t[:, 0:64], rhs=p_.t[:, cs:512],
                                start=(kt == 0), stop=(kt == nkt - 1)), r=[ones_bf, p_], w=[pSm])
                    for m in range(2):
                        P.op("dve", lambda e, m=m: e.reciprocal(out=rc[m].t[:, :], in_=ps[5 + m].t[0:64, :]),
                             r=[ps[5 + m]], w=[rc[m]])
                        P.op("dve", lambda e, m=m: e.tensor_tensor(out=tO[m].t[:, :], in0=ps[3 + m].t[0:64, :],
                                                                   in1=rc[m].t[:, :], op=ALU.mult),
                             r=[ps[3 + m], rc[m]], w=[tO[m]])
                    P.op("dve", lambda e: e.scalar_tensor_tensor(out=od.t[:, :], in0=tO[1].t[:, :], scalar=neglam[0:64, :],
                                                                 in1=tO[0].t[:, :], op0=ALU.mult, op1=ALU.add),
                         r=[tO[0], tO[1], lamt], w=[od])
                    act(osq.t[:, :], od.t[:, :], AF.Square, [od], [osq])
                    pst_ = ps[7]
                    P.op("pe", lambda e: e.matmul(pst_.t[0:64, :], lhsT=o64.t[0:64, :], rhs=osq.t[:, :], start=True,
                                                  stop=True), r=[o64, osq], w=[pst_])
                    act(rs.t[:, :], pst_.t[0:64, :], AF.Ln, [pst_], [rs], bias=eps_c[0:64, :])
                    act(rs.t[:, :], rs.t[:, :], AF.Exp, [rs], [rs], scale=-0.5)
                    o_ = obo[(h * NB + qb) % 2]
                    hb0 = (h % 2) * 64
                    P.op("dve", lambda e, o_=o_, hb0=hb0: e.scalar_tensor_tensor(
                        out=o_.t[:, :], in0=od.t[:, :], scalar=gsub.t[0:64, :], in1=rs.t[:, :],
                        op0=ALU.mult, op1=ALU.mult), r=[od, gsub, rs], w=[o_])
                    P.dma(fm["obT"][b][hb0:hb0 + 64, cc, q0:q0 + 512], o_.t[:, :], r=[o_], w=[HB("obT", b)])
        stage_reset()

        qT = sb([128, 2, S], BF16, "qT"); kT = sb([128, 2, S], BF16, "kT"); vv = sb([128, T, 256], BF16, "vv")
        ee = [sb([128, 512], F32, "ee") for _ in range(2)]
        spb = [sb([128, 512], BF16, "spb") for _ in range(2)]
        lsum = sb([128, 512], BF16, "lsum")
        a1 = [sb([128, 512], F32, "a1") for _ in range(2)]
        wT = [sb([128, 512], BF16, "wT") for _ in range(2)]
        oco = [sb([64, 512], BF16, "oco") for _ in range(2)]
        SC_C = 64 ** -0.5
        it = [0]
        for b in range(NSEQ):
            P.dma(qT.t[:, :, :], fm["qcT"][b][:, :, :], r=[HB("qcT", b)], w=[qT])
            P.dma(kT.t[:, :, :], fm["kcT"][b][:, :, :], r=[HB("kcT", b)], w=[kT])
            P.dma(vv.t[:, :, :], vcs[b].rearrange("(t p) f -> p t f", p=128), r=[HB("vcs", b)], w=[vv])
            for h in range(4):
                cc = h // 2
                hb0 = (h % 2) * 64
                for qb in range(NB):
                    q0 = qb * 512
                    top = qb * 4 + 3
                    lo_kt = 0 if cwin is None else max(0, top - 3 - cwin)
                    P.op("pool", lambda e: e.memset(lsum.t[:, :], 0.0), w=[lsum])
                    pO = ps[6 + (h * NB + qb) % 2]
                    for kt in range(top, lo_kt - 1, -1):
                        i = kt - qb * 4
                        cs = max(i, 0) * 128
                        it[0] += 1
                        n = it[0]
                        pZ = ps[n % 3]; pL = ps[3 + n % 3]
                        e_ = ee[n % 2]; s_ = spb[n % 2]; a_ = a1[n % 2]; w_ = wT[n % 2]
                        P.op("pe", lambda e, pZ=pZ, kt=kt, cs=cs: e.matmul(
                            pZ.t[:, cs:512], lhsT=kT.t[hb0:hb0 + 64, cc, kt * 128:(kt + 1) * 128],
                            rhs=qT.t[hb0:hb0 + 64, cc, q0 + cs:q0 + 512], start=True, stop=True), r=[kT, qT], w=[pZ])
                        act(e_.t[:, cs:512], pZ.t[:, cs:512], AF.Exp, [pZ], [e_], scale=SC_C)
                        act(s_.t[:, cs:512], e_.t[:, cs:512], AF.Ln, [e_], [s_], bias=one_c)
                        if i >= 0:
                            P.op("dve", lambda e, s_=s_, cs=cs: e.tensor_tensor(
                                out=s_.t[:, cs:cs + 128], in0=s_.t[:, cs:cs + 128], in1=lt_bf, op=ALU.mult),
                                r=[s_, cbf], w=[s_])
                        first = (kt == top)
                        P.op("pe", lambda e, pL=pL, s_=s_, cs=cs, first=first: e.matmul(
                            pL.t[:, cs:512], lhsT=gt_bf, rhs=s_.t[:, cs:512], start=True, stop=first),
                            r=[cbf, s_], w=[pL])
                        if not first:
                            P.op("pe", lambda e, pL=pL, cs=cs: e.matmul(
                                pL.t[:, cs:512], lhsT=ones_bf.t[:, :], rhs=lsum.t[:, cs:512], start=False, stop=True),
                                r=[ones_bf, lsum], w=[pL])
                        P.op("dve", lambda e, a_=a_, pZ=pZ, s_=s_, cs=cs: e.scalar_tensor_tensor(
                            out=a_.t[:, cs:512], in0=pZ.t[:, cs:512], scalar=SC_C, in1=s_.t[:, cs:512],
                            op0=ALU.mult, op1=ALU.subtract), r=[pZ, s_], w=[a_])
                        P.op("dve", lambda e, a_=a_, pL=pL, cs=cs: e.tensor_tensor(
                            out=a_.t[:, cs:512], in0=a_.t[:, cs:512], in1=pL.t[:, cs:512], op=ALU.subtract),
                            r=[a_, pL], w=[a_])
                        act(w_.t[:, cs:512], a_.t[:, cs:512], AF.Exp, [a_], [w_])
                        if i >= 0:
                            P.op("dve", lambda e, w_=w_, cs=cs: e.tensor_tensor(
                                out=w_.t[:, cs:cs + 128], in0=w_.t[:, cs:cs + 128], in1=lt_bf, op=ALU.mult),
                                r=[w_, cbf], w=[w_])
                        P.op("pe", lambda e, pO=pO, w_=w_, kt=kt, cs=cs, first=first: e.matmul(
                            pO.t[0:64, cs:512], lhsT=vv.t[:, kt, h * 64:(h + 1) * 64], rhs=w_.t[:, cs:512],
                            start=first, stop=(kt == lo_kt)), r=[vv, w_], w=[pO])
                        if kt > lo_kt:
                            P.op("dve", lambda e, s_=s_, cs=cs: e.tensor_tensor(
                                out=lsum.t[:, cs:512], in0=lsum.t[:, cs:512], in1=s_.t[:, cs:512], op=ALU.add),
                                r=[lsum, s_], w=[lsum])
                    o_ = oco[(h * NB + qb) % 2]
                    act(o_.t[:, :], pO.t[0:64, :], AF.Identity, [pO], [o_])
                    P.dma(fm["ocT"][b][hb0:hb0 + 64, cc, q0:q0 + 512], o_.t[:, :], r=[o_], w=[HB("ocT", b)])
        stage_reset()

        qiT = sb([128, 2, S], BF16, "qiT"); ki4 = sb([128, S], BF16, "ki4")
        wi_ = sb([128, T, 8], F32, "wi")
        qdT = sb([128, 2, S], BF16, "qdT"); kd2 = sb([128, S], BF16, "kd2"); vd = sb([128, T, 64], BF16, "vd")
        sc = [sb([128, S], F32, "sc") for _ in range(2)]
        rl = [sb([128, 512], F32, "rl") for _ in range(3)]
        st = [sb([128, 8], F32, "st") for _ in range(2)]
        steps = [sb([128, NIT + 2], F32, "steps") for _ in range(2)]
        cnt = [sb([128, NIT + 2], F32, "cnt") for _ in range(2)]
        g2 = sb([128, 1], F32, "g2")
        jk = sb([128, S], BF16, "jk")
        mb = [sb([128, S], BF16, "mb") for _ in range(2)]
        MT = [sb([128, T, 128], BF16, "MT") for _ in range(2)]
        pd = [sb([128, 4, 128], BF16, "pd") for _ in range(3)]
        rcd = sb([64, 512], F32, "rcd")
        odo = [sb([64, 4, 128], BF16, "odo") for _ in range(2)]
        SC_D = 64 ** -0.5
        n_ = [0]
        for b in range(NSEQ):
            P.dma(qiT.t[:, :, :], fm["qiT"][b][:, :, :], r=[HB("qiT", b)], w=[qiT])
            for g in range(4):
                P.dma(ki4.t[32 * g:32 * g + 32, :], kiT[b][:, :], r=[HB("kiT", b)], w=[ki4])
            P.dma(wi_.t[:, :, :], wis[b].rearrange("(t p) f -> p t f", p=128), r=[HB("wis", b)], w=[wi_])
            P.dma(qdT.t[:, :, :], fm["qdT"][b][:, :, :], r=[HB("qdT", b)], w=[qdT])
            for g in range(2):
                P.dma(kd2.t[64 * g:64 * g + 64, :], kdT[b][:, :], r=[HB("kdT", b)], w=[kd2])
            P.dma(vd.t[:, :, :], vds[b].rearrange("(t p) f -> p t f", p=128), r=[HB("vds", b)], w=[vd])
            for qt in range(T):
                kl = (qt + 1) * 128
                s_ = sc[qt % 2]; st_ = st[qt % 2]; stp = steps[qt % 2]; cn = cnt[qt % 2]
                m_ = mb[qt % 2]; mt_ = MT[qt % 2]
                for kb in range((kl + 511) // 512):
                    k0 = kb * 512
                    nk = min(512, kl - k0)
                    for hi in range(8):
                        n_[0] += 1
                        pI = ps[n_[0] % 3]
                        r_ = rl[n_[0] % 3]
                        g = hi % 4
                        kw = {"tile_position": (96, 0)} if g == 3 else {}
                        P.op("pe", lambda e, pI=pI, g=g, hi=hi, kw=kw, k0=k0, nk=nk: e.matmul(
                            pI.t[:, 0:nk], lhsT=qiT.t[32 * g:32 * g + 32, hi // 4, qt * 128:(qt + 1) * 128],
                            rhs=ki4.t[32 * g:32 * g + 32, k0:k0 + nk], start=True, stop=True, **kw),
                            r=[qiT, ki4], w=[pI])
                        act(r_.t[:, 0:nk], pI.t[:, 0:nk], AF.Relu, [pI], [r_])
                        if hi == 0:
                            P.op("dve", lambda e, r_=r_, k0=k0, nk=nk: e.tensor_scalar(
                                out=s_.t[:, k0:k0 + nk], in0=r_.t[:, 0:nk], scalar1=wi_.t[:, qt, 0:1], scalar2=None,
                                op0=ALU.mult), r=[r_, wi_], w=[s_])
                        else:
                            P.op("dve", lambda e, r_=r_, k0=k0, nk=nk, hi=hi: e.scalar_tensor_tensor(
                                out=s_.t[:, k0:k0 + nk], in0=r_.t[:, 0:nk], scalar=wi_.t[:, qt, hi:hi + 1],
                                in1=s_.t[:, k0:k0 + nk], op0=ALU.mult, op1=ALU.add), r=[r_, wi_, s_], w=[s_])
                ck(20)
                P.op("dve", lambda e: e.tensor_reduce(out=st_.t[:, 0:1], in_=s_.t[:, 0:kl], axis=AX.X, op=ALU.min),
                     r=[s_], w=[st_])
                P.op("dve", lambda e: e.tensor_tensor(out=s_.t[:, kl - 128:kl], in0=s_.t[:, kl - 128:kl], in1=negm,
                                                      op=ALU.add), r=[s_, cst], w=[s_])
                P.op("dve", lambda e: e.tensor_reduce(out=st_.t[:, 1:2], in_=s_.t[:, 0:kl], axis=AX.X, op=ALU.max),
                     r=[s_, st_], w=[st_])
                P.op("dve", lambda e: e.tensor_tensor(out=st_.t[:, 2:3], in0=st_.t[:, 1:2], in1=st_.t[:, 0:1],
                                                      op=ALU.subtract), r=[st_], w=[st_])
                P.op("dve", lambda e: e.tensor_scalar(out=st_.t[:, 2:3], in0=st_.t[:, 2:3], scalar1=1.02, scalar2=0.002,
                                                      op0=ALU.mult, op1=ALU.add), r=[st_], w=[st_])
                P.op("dve", lambda e: e.scalar_tensor_tensor(out=st_.t[:, 3:4], in0=st_.t[:, 2:3], scalar=-0.5,
                                                             in1=st_.t[:, 1:2], op0=ALU.mult, op1=ALU.add),
                     r=[st_], w=[st_])
                P.op("dve", lambda e: e.tensor_scalar(out=stp.t[:, :], in0=pow2, scalar1=st_.t[:, 2:3], scalar2=None,
                                                      op0=ALU.mult), r=[st_, cst], w=[stp])
                ck(21)
                P.op("pool", lambda e: e.memset(cn.t[:, :], 0.0), w=[cn])
                for itn in range(NIT):
                    P.op("dve", lambda e, itn=itn: e.tensor_scalar(
                        out=jk.t[:, 0:kl], in0=s_.t[:, 0:kl], scalar1=st_.t[:, 3:4], scalar2=zero_c, op0=ALU.is_ge,
                        op1=ALU.add, accum_out=cn.t[:, itn:itn + 1]), r=[s_, st_, cn], w=[jk, cn])
                    P.op("dve", lambda e, itn=itn: e.tensor_scalar(
                        out=g2.t[:, :], in0=cn.t[:, itn:itn + 1], scalar1=topk_c, scalar2=stp.t[:, itn:itn + 1],
                        op0=ALU.is_ge, op1=ALU.mult), r=[cn, stp, cst], w=[g2])
                    P.op("dve", lambda e, itn=itn: e.scalar_tensor_tensor(
                        out=st_.t[:, 3:4], in0=st_.t[:, 3:4], scalar=stp.t[:, itn + 1:itn + 2], in1=g2.t[:, :],
                        op0=ALU.subtract, op1=ALU.add), r=[st_, stp, g2], w=[st_])
                P.op("dve", lambda e: e.tensor_tensor(out=st_.t[:, 3:4], in0=st_.t[:, 3:4], in1=stp.t[:, NIT:NIT + 1],
                                                      op=ALU.subtract), r=[st_, stp], w=[st_])
                P.op("dve", lambda e: e.tensor_scalar(out=m_.t[:, 0:kl], in0=s_.t[:, 0:kl], scalar1=st_.t[:, 3:4],
                                                      scalar2=None, op0=ALU.is_ge), r=[s_, st_], w=[m_])
                ck(22)
                for k8 in range((qt + 8) // 8):
                    nt_ = min(8, qt + 1 - k8 * 8)
                    pT = ps[3 + (n_[0] + k8) % 2]
                    pTv = pT.t[:, :].bitcast(BF16)
                    for u in range(nt_):
                        kt = k8 * 8 + u
                        P.op("pe", lambda e, pTv=pTv, u=u, kt=kt: e.transpose(
                            pTv[:, u * 128:(u + 1) * 128], m_.t[:, kt * 128:(kt + 1) * 128], ident_bf),
                            r=[m_, cbf], w=[pT])
                    act(mt_.t[:, k8 * 8:k8 * 8 + nt_, :], pTv[:, 0:nt_ * 128].rearrange("p (u q) -> p u q", q=128),
                        AF.Identity, [pT], [mt_])
                ck(23)
                pO = ps[5]; pSm = ps[6]
                for kt in range(qt + 1):
                    n_[0] += 1
                    pAB = (ps[0], ps[1]) if n_[0] % 2 == 0 else (ps[2], ps[7])
                    p_ = pd[n_[0] % 3]
                    for h in range(4):
                        hb0 = (h % 2) * 64
                        pS = pAB[h % 2]
                        P.op("pe", lambda e, pS=pS, h=h, hb0=hb0, kt=kt: e.matmul(
                            pS.t[:, (h // 2) * 128:(h // 2 + 1) * 128], lhsT=kd2.t[hb0:hb0 + 64, kt * 128:(kt + 1) * 128],
                            rhs=qdT.t[hb0:hb0 + 64, h // 2, qt * 128:(qt + 1) * 128], start=True, stop=True),
                            r=[kd2, qdT], w=[pS])
                    for g in range(2):
                        act(p_.t[:, 2 * g:2 * g + 2, :], pAB[g].t[:, 0:256].rearrange("p (h q) -> p h q", q=128), AF.Exp,
                            [pAB[g]], [p_], scale=SC_D)
                    for hp in range(4):
                        P.op("dve", lambda e, p_=p_, kt=kt, hp=hp: e.tensor_tensor(
                            out=p_.t[:, hp, :], in0=p_.t[:, hp, :], in1=mt_.t[:, kt, :], op=ALU.mult),
                            r=[p_, mt_], w=[p_])
                    pr = p_.t[:, :, :].rearrange("p h q -> p (h q)")
                    P.op("pe", lambda e, pr=pr, kt=kt: e.matmul(pO.t[0:64, :], lhsT=vd.t[:, kt, :], rhs=pr,
                                                                start=(kt == 0), stop=(kt == qt)), r=[vd, p_], w=[pO])
                    P.op("pe", lambda e, pr=pr, kt=kt: e.matmul(pSm.t[0:64, :], lhsT=ones_bf.t[:, 0:64], rhs=pr,
                                                                start=(kt == 0), stop=(kt == qt)), r=[ones_bf, p_], w=[pSm])
                P.op("dve", lambda e: e.reciprocal(out=rcd.t[:, :], in_=pSm.t[0:64, :]), r=[pSm], w=[rcd])
                o_ = odo[qt % 2]
                P.op("dve", lambda e, o_=o_: e.tensor_tensor(out=o_.t[:, :, :].rearrange("p h q -> p (h q)"),
                                                             in0=pO.t[0:64, :], in1=rcd.t[:, :], op=ALU.mult),
                     r=[pO, rcd], w=[o_])
                for hp in range(4):
                    h = (hp % 2) * 2 + hp // 2
                    hb0 = (h % 2) * 64
                    P.dma(fm["odT"][b][hb0:hb0 + 64, h // 2, qt * 128:(qt + 1) * 128], o_.t[:, hp, :], r=[o_],
                          w=[HB("odT", b)])
        stage_reset()

        Wg = sb([128, 8, 4096], BF16, "Wg"); Wbr = sb([128, 8, D], BF16, "Wbr"); Wo = sb([128, 8, D], BF16, "Wo")
        stg = [sb([128, 8, 256], F32, "stg") for _ in range(2)]
        wv = W["w_in"].t[l].rearrange("(kc p) n -> p kc n", p=128)
        load_cast(Wg, lambda c0, n: Wg.t[:, :, c0:c0 + n], lambda c0, n: wv[:, :, 2728 + c0:2728 + c0 + n],
                  [((c0, 256), (c0, 256)) for c0 in range(0, 4096, 256)], stg, ["pool", "dve"])
        wbv = W["w_branch"].t[l].rearrange("i (kc p) n -> p (i kc) n", p=128)
        load_cast(Wbr, lambda c0, n: Wbr.t[:, :, c0:c0 + n], lambda c0, n: wbv[:, :, c0:c0 + n],
                  [((c0, 256), (c0, 256)) for c0 in range(0, D, 256)], stg, ["pool", "dve"])
        wov = W["w_out"].t[l].rearrange("(kc p) n -> p kc n", p=128)
        load_cast(Wo, lambda c0, n: Wo.t[:, :, c0:c0 + n], lambda c0, n: wov[:, :, c0:c0 + n],
                  [((c0, 256), (c0, 256)) for c0 in range(0, D, 256)], stg, ["pool", "dve"])
        GG = sb([128, D], F32, "GG"); tmpb = sb([128, D], F32, "tmpb")
        hTb = [sb([128, 8, 512], BF16, "hT") for _ in range(2)]
        oTb = [[sb([128, 2, 512], BF16, "oT") for _ in range(4)] for _ in range(2)]
        sgm = [sb([128, 512], F32, "sgm") for _ in range(2)]
        acc = sb([128, 512], F32, "acc"); tm = sb([128, 512], F32, "tm")
        mT = sb([128, 8, 512], BF16, "mT")
        ysb = [sb([128, D], F32, "ysb") for _ in range(2)]
        xts = [sb([128, D], F32, "xt") for _ in range(2)]
        junk = sb([128, D], BF16, "junk"); ss = sb([128, 2], F32, "ss")
        onames = ("oaT", "obT", "ocT", "odT")
        n3 = [0]
        for b in range(NSEQ):
            make_GG(l, b, "mix_post_g", 2, GG, tmpb)
            for tb in range(NB):
                t0 = tb * 512
                hT = hTb[tb % 2]; oT = oTb[tb % 2]
                P.dma(hT.t[:, :, :], hTs[b][:, :, t0:t0 + 512], r=[HB("hTs", b)], w=[hT])
                for i in range(4):
                    P.dma(oT[i].t[:, :, :], fm[onames[i]][b][:, :, t0:t0 + 512], r=[HB(onames[i], b)], w=[oT[i]])
                for oc in range(8):
                    for i in range(4):
                        n3[0] += 1
                        pG = ps[n3[0] % 2]; pB = ps[2 + n3[0] % 2]; sg_ = sgm[n3[0] % 2]
                        for kc in range(8):
                            P.op("pe", lambda e, pG=pG, kc=kc, i=i, oc=oc: e.matmul(
                                pG.t[:, :], lhsT=Wg.t[:, kc, i * D + oc * 128:i * D + (oc + 1) * 128], rhs=hT.t[:, kc, :],
                                start=(kc == 0), stop=(kc == 7)), r=[Wg, hT], w=[pG])
                        for kc in range(2):
                            P.op("pe", lambda e, pB=pB, kc=kc, i=i, oc=oc: e.matmul(
                                pB.t[:, :], lhsT=Wbr.t[:, i * 2 + kc, oc * 128:(oc + 1) * 128], rhs=oT[i].t[:, kc, :],
                                start=(kc == 0), stop=(kc == 1)), r=[Wbr, oT[i]], w=[pB])
                        act(sg_.t[:, :], pG.t[:, :], AF.Sigmoid, [pG], [sg_])
                        if i == 0:
                            P.op("dve", lambda e, sg_=sg_, pB=pB: e.tensor_tensor(out=acc.t[:, :], in0=sg_.t[:, :],
                                                                                  in1=pB.t[:, :], op=ALU.mult),
                                 r=[sg_, pB], w=[acc])
                        else:
                            P.op("dve", lambda e, sg_=sg_, pB=pB: e.tensor_tensor(out=tm.t[:, :], in0=sg_.t[:, :],
                                                                                  in1=pB.t[:, :], op=ALU.mult),
                                 r=[sg_, pB], w=[tm])
                            if i < 3:
                                P.op("dve", lambda e: e.tensor_tensor(out=acc.t[:, :], in0=acc.t[:, :], in1=tm.t[:, :],
                                                                       op=ALU.add), r=[acc, tm], w=[acc])
                            else:
                                P.op("dve", lambda e, oc=oc: e.tensor_tensor(out=mT.t[:, oc, :], in0=acc.t[:, :],
                                                                              in1=tm.t[:, :], op=ALU.add),
                                     r=[acc, tm], w=[mT])
                for tt in range(4):
                    y_ = ysb[tt % 2]; xt = xts[tt % 2]
                    P.dma(xt.t[:, :], x_src[b, t0 + tt * 128:t0 + (tt + 1) * 128, :], r=[HB("x%d" % l, b)], w=[xt])
                    for hf in range(2):
                        pY = ps[4 + hf]
                        for kc in range(8):
                            P.op("pe", lambda e, pY=pY, kc=kc, hf=hf, tt=tt: e.matmul(
                                pY.t[:, :], lhsT=mT.t[:, kc, tt * 128:(tt + 1) * 128], rhs=Wo.t[:, kc, hf * 512:(hf + 1) * 512],
                                start=(kc == 0), stop=(kc == 7)), r=[mT, Wo], w=[pY])
                        act(y_.t[:, hf * 512:(hf + 1) * 512], pY.t[:, :], AF.Identity, [pY], [y_])
                    post_norm_residual(y_, xt, GG, ss, junk, xmid[b, t0 + tt * 128:t0 + (tt + 1) * 128, :],
                                       HB("xmid%d" % l, b))
        stage_reset()

        Wu = sb([128, 8, 2 * DFF], BF16, "Wu")
        stg = [sb([128, 8, 256], F32, "stg") for _ in range(2)]
        wv = W["w_up"].t[l].rearrange("(kc p) n -> p kc n", p=128)
        load_cast(Wu, lambda c0, n: Wu.t[:, :, c0:c0 + n], lambda c0, n: wv[:, :, c0:c0 + n],
                  [((c0, 256), (c0, 256)) for c0 in range(0, 2 * DFF, 256)], stg, ["pool", "dve"])
        fcw = sb([128, 44, 3], F32, "fcw"); fcb = sb([128, 44], F32, "fcb")
        for j in range(3):
            P.dma(fcw.t[:, :, j], W["ffn_conv_w"].t[l][j].rearrange("(c p) -> p c", p=128), w=[fcw],
                  allow_slow_non_contiguous=True)
        P.dma(fcb.t[:, :], W["ffn_conv_b"].t[l].rearrange("(c p) -> p c", p=128), w=[fcb],
              allow_slow_non_contiguous=True)
        Acol = sb([128, 8], F32, "Acol"); Bcol = sb([128, 8], F32, "Bcol"); tmpc = sb([128, 8], F32, "tmpc")
        xts = [sb([128, D], F32, "xt") for _ in range(2)]
        xs = sb([128, D], BF16, "xs"); junk = sb([128, D], BF16, "junk"); ss = sb([128, 2], F32, "ss")
        hTb = [sb([128, 8, 512], BF16, "hT") for _ in range(2)]
        halo = sb([128, 44, 2], F32, "halo")
        ub = [sb([128, 514], F32, "ub") for _ in range(4)]
        c1_ = [sb([128, 512], F32, "c1") for _ in range(2)]
        gs = sb([128, 512], F32, "gs")
        aT = [sb([128, 22, 512], BF16, "aT") for _ in range(2)]
        n4 = [0]
        for b in range(NSEQ):
            make_AB(l, b, "ffn_pre_g", 3, 4, Acol, Bcol, tmpc)
            P.op("pool", lambda e: e.memset(halo.t[:, :, :], 0.0), w=[halo])
            for tb in range(NB):
                t0 = tb * 512
                hT = hTb[tb % 2]; a_ = aT[tb % 2]
                for tt in range(4):
                    xt = xts[tt % 2]
                    P.dma(xt.t[:, :], xmid[b, t0 + tt * 128:t0 + (tt + 1) * 128, :], r=[HB("xmid%d" % l, b)], w=[xt])
                    norm_transpose(xt, hT, tt, Acol, Bcol, xs, ss, junk, 7)
                for j in range(22):
                    res = []
                    for gv in range(2):
                        ch = gv * 22 + j
                        n4[0] += 1
                        pU = ps[n4[0] % 4]; u_ = ub[n4[0] % 4]; c_ = c1_[gv]
                        for kc in range(8):
                            P.op("pe", lambda e, pU=pU, kc=kc, ch=ch: e.matmul(
                                pU.t[:, :], lhsT=Wu.t[:, kc, ch * 128:(ch + 1) * 128], rhs=hT.t[:, kc, :],
                                start=(kc == 0), stop=(kc == 7)), r=[Wu, hT], w=[pU])
                        P.op("pool", lambda e, u_=u_, ch=ch: e.tensor_copy(out=u_.t[:, 0:2], in_=halo.t[:, ch, :]),
                             r=[halo], w=[u_])
                        act(u_.t[:, 2:514], pU.t[:, :], AF.Identity, [pU], [u_])
                        P.op("pool", lambda e, u_=u_, ch=ch: e.tensor_copy(out=halo.t[:, ch, :], in_=u_.t[:, 512:514]),
                             r=[u_], w=[halo])
                        P.op("dve", lambda e, u_=u_, c_=c_, ch=ch: e.tensor_scalar(
                            out=c_.t[:, :], in0=u_.t[:, 2:514], scalar1=fcw.t[:, ch, 2:3], scalar2=fcb.t[:, ch:ch + 1],
                            op0=ALU.mult, op1=ALU.add), r=[u_, fcw, fcb], w=[c_])
                        P.op("dve", lambda e, u_=u_, c_=c_, ch=ch: e.scalar_tensor_tensor(
                            out=c_.t[:, :], in0=u_.t[:, 1:513], scalar=fcw.t[:, ch, 1:2], in1=c_.t[:, :],
                            op0=ALU.mult, op1=ALU.add), r=[u_, fcw, c_], w=[c_])
                        P.op("dve", lambda e, u_=u_, c_=c_, ch=ch: e.scalar_tensor_tensor(
                            out=c_.t[:, :], in0=u_.t[:, 0:512], scalar=fcw.t[:, ch, 0:1], in1=c_.t[:, :],
                            op0=ALU.mult, op1=ALU.add), r=[u_, fcw, c_], w=[c_])
                        res.append(c_)
                    act(gs.t[:, :], res[0].t[:, :], AF.Silu, [res[0]], [gs])
                    P.op("dve", lambda e, j=j, a_=a_, rv=res[1]: e.tensor_tensor(out=a_.t[:, j, :], in0=gs.t[:, :],
                                                                                 in1=rv.t[:, :], op=ALU.mult),
                         r=[gs, res[1]], w=[a_])
                P.dma(aTs[b][:, :, t0:t0 + 512], a_.t[:, :, :], r=[a_], w=[HB("aTs", b)])
        stage_reset()

        Wd = sb([128, 22, D], BF16, "Wd")
        stg = [sb([128, 8, 512], F32, "stg") for _ in range(2)]
        wdv = W["w_down"].t[l].rearrange("(kc p) n -> p kc n", p=128)
        pieces = []
        for k0 in (0, 8, 16):
            nk = min(8, 22 - k0)
            for c0 in (0, 512):
                pieces.append(((k0, nk, c0), (k0, nk, c0)))
        load_cast(Wd, lambda k0, nk, c0: Wd.t[:, k0:k0 + nk, c0:c0 + 512],
                  lambda k0, nk, c0: wdv[:, k0:k0 + nk, c0:c0 + 512], pieces, stg, ["pool", "dve"])
        GG = sb([128, D], F32, "GG"); tmpb = sb([128, D], F32, "tmpb")
        aT = [sb([128, 22, 512], BF16, "aT") for _ in range(2)]
        ysb = [sb([128, D], F32, "ysb") for _ in range(2)]
        xts = [sb([128, D], F32, "xt") for _ in range(2)]
        junk = sb([128, D], BF16, "junk"); ss = sb([128, 2], F32, "ss")
        for b in range(NSEQ):
            make_GG(l, b, "ffn_post_g", 5, GG, tmpb)
            for tb in range(NB):
                t0 = tb * 512
                a_ = aT[tb % 2]
                P.dma(a_.t[:, :, :], aTs[b][:, :, t0:t0 + 512], r=[HB("aTs", b)], w=[a_])
                for tt in range(4):
                    y_ = ysb[tt % 2]; xt = xts[tt % 2]
                    P.dma(xt.t[:, :], xmid[b, t0 + tt * 128:t0 + (tt + 1) * 128, :], r=[HB("xmid%d" % l, b)], w=[xt])
                    for hf in range(2):
                        pY = ps[(tt * 2 + hf) % 4]
                        for kc in range(22):
                            P.op("pe", lambda e, pY=pY, kc=kc, hf=hf, tt=tt: e.matmul(
                                pY.t[:, :], lhsT=a_.t[:, kc, tt * 128:(tt + 1) * 128], rhs=Wd.t[:, kc, hf * 512:(hf + 1) * 512],
                                start=(kc == 0), stop=(kc == 21)), r=[a_, Wd], w=[pY])
                        act(y_.t[:, hf * 512:(hf + 1) * 512], pY.t[:, :], AF.Identity, [pY], [y_])
                    post_norm_residual(y_, xt, GG, ss, junk, x_dst[b, t0 + tt * 128:t0 + (tt + 1) * 128, :],
                                       HB("x%d" % (l + 1), b) if l < DEPTH - 1 else out_t)
        stage_reset()


_CACHE = {}


def kernel(**inputs):
    NCORES = 8
    x = np.ascontiguousarray(inputs["x"], dtype=np.float32)
    Bt, S, _ = x.shape
    NSEQ = Bt // NCORES
    DEPTH = inputs["ada_w"].shape[0]
    key = (S, NSEQ, DEPTH)
    if key not in _CACHE:
        _CACHE[key] = build(S, NSEQ, DEPTH)[0]
    nc = _CACHE[key]
    consts = make_consts(S)
    wnames = ("ada_w", "ada_b", "mix_pre_g", "mix_post_g", "ffn_pre_g", "ffn_post_g", "w_in", "conv_a_w",
              "conv_a_b", "conv_a_ln_g", "conv_a_ln_b", "lam_q1", "lam_k1", "lam_q2", "lam_k2", "diff_subln_g",
              "w_branch", "w_out", "w_up", "ffn_conv_w", "ffn_conv_b", "w_down")
    shared = {n: np.ascontiguousarray(inputs[n], dtype=np.float32) for n in wnames}
    c = np.ascontiguousarray(inputs["c"], dtype=np.float32)
    pos = np.ascontiguousarray(inputs["positions"], dtype=np.int32)
    in_maps = []
    for i in range(NCORES):
        m = dict(shared)
        m["x"] = x[i * NSEQ:(i + 1) * NSEQ]
        m["c"] = c[i * NSEQ:(i + 1) * NSEQ]
        m["positions"] = pos[i * NSEQ:(i + 1) * NSEQ]
        m["consts"] = consts
        in_maps.append(m)
    res = run_bass_kernel_spmd(nc, in_maps, core_ids=list(range(NCORES)))
    return np.concatenate([r["out"] for r in res.results], axis=0).astype(np.float32)
```

```python
import math
import contextlib
import numpy as np
import concourse.bass as bass
import concourse.mybir as mybir
from concourse.bass_utils import run_bass_kernel_spmd

F32 = mybir.dt.float32
BF16 = mybir.dt.bfloat16
I32 = mybir.dt.int32
AF = mybir.ActivationFunctionType
ALU = mybir.AluOpType
AX = mybir.AxisListType

D = 1024
DIN = 6824
DFF = 2816
EPS = 1e-6
THETA = 10000.0
NIT = 22
MAGIC = 12582912.0
TWO_PI = 2.0 * math.pi
C1 = 6.28125
C2 = TWO_PI - C1
PI_LO = 3.1415925


class Buf:
    __slots__ = ("w", "r")

    def __init__(self):
        self.w = None
        self.r = []


class Tl:
    __slots__ = ("t", "b")

    def __init__(self, t):
        self.t = t
        self.b = Buf()


def _b(x):
    return x.b if isinstance(x, Tl) else x


class Op:
    __slots__ = ("ex", "q", "emit", "deps", "sig", "seq", "isdma")


class _Rec:
    def __init__(self):
        self.call = None

    def __getattr__(self, name):
        def f(*a, **k):
            self.call = (name, a, k)
            return None
        return f


class Prog:
    NSLOT = 8

    def __init__(self, nc):
        self.nc = nc
        self.ops = []
        self.slot_rr = {}

    def op(self, ex, emit, r=(), w=()):
        o = Op()
        rec = _Rec()
        emit(rec)
        nm_, a_, k_ = rec.call
        o.ex = ex; o.q = ex; o.isdma = False; o.sig = False; o.seq = None
        o.emit = lambda eng, nm_=nm_, a_=a_, k_=k_: getattr(eng, nm_)(*a_, **k_)
        self._deps(o, r, w)
        self.ops.append(o)
        return o

    def dma(self, out, in_, r=(), w=(), q="sp", **kw):
        o = Op()
        o.q = q; o.isdma = True; o.sig = True; o.seq = None
        s = self.slot_rr.get(q, 0)
        self.slot_rr[q] = (s + 1) % self.NSLOT
        o.ex = ("dma", q, s)
        o.emit = lambda eng: eng.dma_start(out=out, in_=in_, **kw)
        self._deps(o, r, w)
        self.ops.append(o)
        return o

    def _deps(self, o, reads, writes):
        deps = []
        for x in reads:
            b = _b(x)
            if b.w is not None:
                deps.append((b.w, True))
        for x in writes:
            b = _b(x)
            if b.w is not None:
                deps.append((b.w, False))
            for rr in b.r:
                deps.append((rr, False))
        o.deps = deps
        for x in reads:
            _b(x).r.append(o)
        for x in writes:
            b = _b(x)
            b.w = o
            b.r = []

    def barrier(self):
        last = {}
        for o in self.ops:
            if o.emit is not None:
                last[o.ex] = o
        prev = list(last.values())
        for q in ("pe", "act", "dve", "pool", "sp"):
            o = Op()
            o.ex = "bar_" + q; o.q = q; o.isdma = False; o.sig = False; o.seq = None
            o.emit = None
            o.deps = [(p, True) for p in prev]
            self.ops.append(o)

    def emit_all(self):
        nc = self.nc
        for o in self.ops:
            for (p, raw) in o.deps:
                if p.isdma:
                    continue
                if p.ex == o.ex and not raw and not o.isdma:
                    continue
                p.sig = True
        cnt = {}
        for o in self.ops:
            if o.isdma:
                cnt[o.ex] = cnt.get(o.ex, 0) + 16
                o.seq = cnt[o.ex]
            elif o.sig:
                cnt[o.ex] = cnt.get(o.ex, 0) + 1
                o.seq = cnt[o.ex]
        prev_on_slot = {}
        for o in self.ops:
            if o.isdma:
                p = prev_on_slot.get(o.ex)
                if p is not None:
                    o.deps.append((p, True))
                prev_on_slot[o.ex] = o
        stack = contextlib.ExitStack()
        sems = {}
        for o in self.ops:
            if o.seq is not None and o.ex not in sems:
                nm = "s_" + ("_".join(str(x) for x in o.ex) if isinstance(o.ex, tuple) else o.ex)
                sems[o.ex] = stack.enter_context(nc.semaphore(nm))
        byq = {"pe": [], "act": [], "dve": [], "pool": [], "sp": []}
        for o in self.ops:
            byq[o.q].append(o)
        self.n_inst = 0

        def run(eng, lst):
            seen = {}
            for o in lst:
                need = {}
                for (p, raw) in o.deps:
                    if (not p.isdma) and p.ex == o.ex and not raw and not o.isdma:
                        continue
                    if p.seq is None:
                        continue
                    if need.get(p.ex, 0) < p.seq:
                        need[p.ex] = p.seq
                for ex, v in need.items():
                    if seen.get(ex, 0) >= v:
                        continue
                    seen[ex] = v
                    eng.wait_ge(sems[ex], v)
                    self.n_inst += 1
                if o.emit is None:
                    continue
                ins = o.emit(eng)
                self.n_inst += 1
                if o.isdma:
                    ins.then_inc(sems[o.ex], 16)
                elif o.sig:
                    ins.then_inc(sems[o.ex], 1)

        with stack, nc.Block() as block:
            @block.tensor
            def _(e):
                run(e, byq["pe"])

            @block.scalar
            def _(e):
                run(e, byq["act"])

            @block.vector
            def _(e):
                run(e, byq["dve"])

            @block.gpsimd
            def _(e):
                run(e, byq["pool"])

            @block.sync
            def _(e):
                run(e, byq["sp"])


def make_consts(S=4096):
    p = np.arange(128)[:, None]
    f = np.arange(128)[None, :]
    c = np.zeros((128, 1024), np.float32)
    c[:, 0:128] = (p == f)
    c[:, 128:256] = (p <= f)
    c[:, 256:384] = (p < f)
    c[:, 384:512] = (p > f)
    c[:, 512:640] = np.where(f <= p, 0.0, -1e30)
    sw32 = np.where(f % 32 < 16, f + 16, f - 16)
    c[:, 640:768] = (p == sw32)
    sw64 = np.where(f % 64 < 32, f + 32, f - 32)
    c[:, 768:896] = (p == sw64)
    pp = np.arange(128)
    c[:, 896] = THETA ** (-(2.0 * (pp % 16)) / 32.0)
    c[:, 897] = THETA ** (-(2.0 * (pp % 32)) / 64.0)
    c[:, 898] = np.where(pp % 32 < 16, -1.0, 1.0)
    c[:, 899] = np.where(pp % 64 < 32, -1.0, 1.0)
    c[:, 900] = EPS
    c[:, 901] = 1.0
    c[:, 902] = 0.0
    c[:, 903] = float(min(256, S // 4)) - 0.5
    for i in range(NIT + 2):
        c[:, 904 + i] = 2.0 ** (-(i + 1))
    return c


class _Stop(Exception):
    pass


def build(S, NSEQ, DEPTH, debug=False, cwin=None, stop=None):
    h = {}
    try:
        _build(h, S, NSEQ, DEPTH, debug, cwin, stop)
    except _Stop:
        pass
    h["P"].barrier()
    h["P"].emit_all()
    return h["nc"], h["P"]


def _build(holder, S, NSEQ, DEPTH, debug, cwin, stop):
    nc = bass.Bass("TRN2", target_bir_lowering=False)
    P = Prog(nc)
    holder["nc"] = nc
    holder["P"] = P
    T = S // 128
    NB = S // 512
    TOPK = min(256, S // 4)
    dbg_kind = "ExternalOutput" if debug else "Internal"

    def din(name, shape, dt=F32):
        return Tl(nc.dram_tensor(name, list(shape), dt, kind="ExternalInput").ap())

    def dscr(name, shape, dt):
        return nc.dram_tensor(name, list(shape), dt, kind=dbg_kind).ap()

    x_in = din("x", [NSEQ, S, D])
    c_in = din("c", [NSEQ, D])
    pos_in = din("positions", [NSEQ, S], I32)
    consts_in = din("consts", [128, 1024])
    W = {}
    for nm, shp in (("ada_w", [DEPTH, D, 6 * D]), ("ada_b", [DEPTH, 6 * D]), ("mix_pre_g", [DEPTH, D]),
                    ("mix_post_g", [DEPTH, D]), ("ffn_pre_g", [DEPTH, D]), ("ffn_post_g", [DEPTH, D]),
                    ("w_in", [DEPTH, D, DIN]), ("conv_a_w", [DEPTH, 31, 256]), ("conv_a_b", [DEPTH, 256]),
                    ("conv_a_ln_g", [DEPTH, 256]), ("conv_a_ln_b", [DEPTH, 256]), ("lam_q1", [DEPTH, 32]),
                    ("lam_k1", [DEPTH, 32]), ("lam_q2", [DEPTH, 32]), ("lam_k2", [DEPTH, 32]),
                    ("diff_subln_g", [DEPTH, 64]), ("w_branch", [DEPTH, 4, 256, D]), ("w_out", [DEPTH, D, D]),
                    ("w_up", [DEPTH, D, 2 * DFF]), ("ffn_conv_w", [DEPTH, 3, 2 * DFF]),
                    ("ffn_conv_b", [DEPTH, 2 * DFF]), ("w_down", [DEPTH, DFF, D])):
        W[nm] = din(nm, shp)
    out_t = Tl(nc.dram_tensor("out", [NSEQ, S, D], F32, kind="ExternalOutput").ap())

    modbuf = Tl(dscr("modbuf", [DEPTH, NSEQ, 6 * D], F32))
    xmid = dscr("xmid", [NSEQ, S, D], F32)
    xlay = dscr("xlay", [NSEQ, S, D], F32)
    hTs = dscr("hTs", [NSEQ, 128, 8, S], BF16)
    fm = {k: dscr(k, [NSEQ, 128, 2, S], BF16) for k in
          ("gluT", "qbT", "kbT", "qcT", "kcT", "qdT", "qiT", "oaT", "obT", "ocT", "odT")}
    kdT = dscr("kdT", [NSEQ, 64, S], BF16)
    kiT = dscr("kiT", [NSEQ, 32, S], BF16)
    vbs = dscr("vbs", [NSEQ, S, 256], BF16)
    vcs = dscr("vcs", [NSEQ, S, 256], BF16)
    vds = dscr("vds", [NSEQ, S, 64], BF16)
    wis = dscr("wis", [NSEQ, S, 8], F32)
    aTs = dscr("aTs", [NSEQ, 128, 22, S], BF16)
    hb = {}

    def HB(name, b):
        k = (name, b)
        if k not in hb:
            hb[k] = Buf()
        return hb[k]

    SB_BASE = (nc.sbuf_base + 63) // 64 * 64
    SB_TOP = nc.sbuf_top
    off = [SB_BASE]
    uid = [0]

    def sb(shape, dt, name="t"):
        esz = 4 if dt in (F32, I32) else 2
        nbytes = int(np.prod(shape[1:])) * esz
        nbytes = (nbytes + 63) // 64 * 64
        uid[0] += 1
        t = nc.alloc_sbuf_tensor_at(f"{name}_{uid[0]}", list(shape), dt, offset=off[0])
        off[0] += nbytes
        assert off[0] <= SB_TOP, f"SBUF overflow at {name}: {off[0]} > {SB_TOP}"
        return Tl(t)

    ps = [Tl(nc.alloc_psum_tensor(f"psb{i}", [128, 512], F32)) for i in range(8)]

    def psbf(i):
        return ps[i].t[:, :].bitcast(BF16)

    cst = sb([128, 1024], F32, "cst")
    P.dma(cst.t[:, :], consts_in.t[:, :], w=[cst])
    cbf = sb([128, 896], BF16, "cbf")
    P.op("dve", lambda e: e.tensor_copy(out=cbf.t[:, :], in_=cst.t[:, 0:896]), r=[cst], w=[cbf])
    ones_bf = sb([128, 128], BF16, "ones")
    P.op("pool", lambda e: e.memset(ones_bf.t[:, :], 1.0), w=[ones_bf])
    ones32 = sb([128, 128], F32, "ones32")
    P.op("pool", lambda e: e.memset(ones32.t[:, :], 1.0), w=[ones32])
    ident_bf = cbf.t[:, 0:128]
    le_bf = cbf.t[:, 128:256]
    lt_bf = cbf.t[:, 256:384]
    gt_bf = cbf.t[:, 384:512]
    negm = cst.t[:, 512:640]
    perm32 = cbf.t[:, 640:768]
    perm64 = cbf.t[:, 768:896]
    invf = {32: cst.t[:, 896:897], 64: cst.t[:, 897:898]}
    sgn = {32: cst.t[:, 898:899], 64: cst.t[:, 899:900]}
    eps_c = cst.t[:, 900:901]
    one_c = cst.t[:, 901:902]
    pow2 = cst.t[:, 904:904 + NIT + 2]
    topk_c = cst.t[:, 903:904]
    zero_c = cst.t[:, 902:903]
    PERSIST = off[0]

    import os as _os
    SUB = int(_os.environ.get('SUB', '0'))

    def ck(n):
        if SUB == n:
            raise _Stop()

    stage_no = [0]

    def stage_reset():
        P.barrier()
        stage_no[0] += 1
        if stop is not None and stage_no[0] >= stop:
            raise _Stop()
        print('stage sbuf bytes', off[0], 'of', SB_TOP, flush=True)
        off[0] = PERSIST

    def act(out, in_, func, r, w, **kw):
        P.op("act", lambda e: e.activation(out=out, in_=in_, func=func, **kw), r=r, w=w)

    def rstd_from_ss(ss, n, r, w_tmp):
        act(ss, ss, AF.Ln, r, w_tmp, scale=1.0 / n, bias=eps_c[0:ss.shape[0], :])
        act(ss, ss, AF.Exp, w_tmp, w_tmp, scale=-0.5)

    def load_cast(dst_tl, dst_ap_fn, src_ap_fn, pieces, stg, eng_cycle):
        for i, (dargs, sargs) in enumerate(pieces):
            st = stg[i % len(stg)]
            d_ap = dst_ap_fn(*dargs)
            s_ap = src_ap_fn(*sargs)
            shp = list(d_ap.shape)
            if len(shp) == 3:
                st_ap = st.t[0:shp[0], 0:shp[1], 0:shp[2]]
            else:
                st_ap = st.t[0:shp[0], 0, 0:shp[1]]
            P.dma(st_ap, s_ap, w=[st])
            ex = eng_cycle[i % len(eng_cycle)]
            P.op(ex, lambda e, o=d_ap, a=st_ap: e.tensor_copy(out=o, in_=a), r=[st], w=[dst_tl])

    cT = sb([128, 8, NSEQ], F32, "cT")
    for b_ in range(NSEQ):
        P.dma(cT.t[:, :, b_], c_in.t[b_].rearrange("(c p) -> p c", p=128), w=[cT], allow_slow_non_contiguous=True)
    act(cT.t[:, :, :], cT.t[:, :, :], AF.Silu, [cT], [cT])
    adst = [sb([128, 8, 512], F32, "adst") for _ in range(2)]
    modsb = sb([NSEQ, 6 * D], F32, "modsb")
    adab = sb([NSEQ, 6 * D], F32, "adab")
    for l in range(DEPTH):
        P.dma(adab.t[:, :], W["ada_b"].t[l:l + 1, :].partition_broadcast(NSEQ), w=[adab])
        wv = W["ada_w"].t[l].rearrange("(kc p) n -> p kc n", p=128)
        for nb in range(12):
            st = adst[nb % 2]
            P.dma(st.t[:, :, :], wv[:, :, nb * 512:(nb + 1) * 512], w=[st])
            pb = ps[nb % 2]
            for kc in range(8):
                P.op("pe", lambda e, st=st, pb=pb, kc=kc: e.matmul(
                    pb.t[0:NSEQ, :], lhsT=cT.t[:, kc, :], rhs=st.t[:, kc, :], start=(kc == 0), stop=(kc == 7)),
                    r=[cT, st], w=[pb])
            P.op("dve", lambda e, pb=pb, nb=nb: e.tensor_tensor(
                out=modsb.t[:, nb * 512:(nb + 1) * 512], in0=pb.t[0:NSEQ, :],
                in1=adab.t[:, nb * 512:(nb + 1) * 512], op=ALU.add), r=[pb, adab], w=[modsb])
        P.dma(modbuf.t[l], modsb.t[:, :], r=[modsb], w=[modbuf])
    stage_reset()

    def col8(ap1d):
        return ap1d.rearrange("(c p) -> p c", p=128)

    def norm_transpose(xt, hT, tt, Acol, Bcol, xs, ss, junk, pbank):
        act(junk.t[:, :], xt.t[:, :], AF.Square, [xt], [junk, ss], accum_out=ss.t[:, 0:1])
        rstd_from_ss(ss.t[:, 0:1], D, [ss], [ss])
        P.op("dve", lambda e: e.tensor_scalar(out=xs.t[:, :], in0=xt.t[:, :], scalar1=ss.t[:, 0:1], scalar2=None,
                                              op0=ALU.mult), r=[xt, ss], w=[xs])
        pv = psbf(pbank)
        for c in range(8):
            P.op("pe", lambda e, c=c: e.transpose(pv[:, c * 128:(c + 1) * 128], xs.t[:, c * 128:(c + 1) * 128],
                                                  ident_bf), r=[xs, cbf], w=[ps[pbank]])
        for c in range(8):
            act(hT.t[:, c, tt * 128:(tt + 1) * 128], pv[:, c * 128:(c + 1) * 128], AF.Identity,
                [ps[pbank], Acol, Bcol], [hT], scale=Acol.t[:, c:c + 1], bias=Bcol.t[:, c:c + 1])

    def post_norm_residual(ysb, xt, GG, ss, junk, dst_ap, dst_buf):
        act(junk.t[:, :], ysb.t[:, :], AF.Square, [ysb], [junk, ss], accum_out=ss.t[:, 0:1])
        rstd_from_ss(ss.t[:, 0:1], D, [ss], [ss])
        P.op("dve", lambda e: e.scalar_tensor_tensor(out=ysb.t[:, :], in0=ysb.t[:, :], scalar=ss.t[:, 0:1],
                                                     in1=GG.t[:, :], op0=ALU.mult, op1=ALU.mult),
             r=[ysb, ss, GG], w=[ysb])
        P.op("dve", lambda e: e.tensor_tensor(out=ysb.t[:, :], in0=ysb.t[:, :], in1=xt.t[:, :], op=ALU.add),
             r=[ysb, xt], w=[ysb])
        P.dma(dst_ap, ysb.t[:, :], r=[ysb], w=[dst_buf])

    def mod_cols(l, b, k):
        return col8(modbuf.t[l, b, k * D:(k + 1) * D])

    def make_AB(l, b, gname, ksh, ksc, Acol, Bcol, tmp):
        P.dma(Bcol.t[:, :], mod_cols(l, b, ksh), r=[modbuf], w=[Bcol], allow_slow_non_contiguous=True)
        P.dma(tmp.t[:, :], mod_cols(l, b, ksc), r=[modbuf], w=[tmp], allow_slow_non_contiguous=True)
        P.dma(Acol.t[:, :], col8(W[gname].t[l]), w=[Acol], allow_slow_non_contiguous=True)
        P.op("dve", lambda e: e.scalar_tensor_tensor(out=Acol.t[:, :], in0=tmp.t[:, :], scalar=1.0, in1=Acol.t[:, :],
                                                     op0=ALU.add, op1=ALU.mult), r=[tmp, Acol], w=[Acol])

    def make_GG(l, b, gname, kg, GG, tmpb):
        P.dma(GG.t[:, :], W[gname].t[l:l + 1, :].partition_broadcast(128), w=[GG])
        P.dma(tmpb.t[:, :], modbuf.t[l, b:b + 1, kg * D:(kg + 1) * D].partition_broadcast(128), r=[modbuf], w=[tmpb])
        P.op("dve", lambda e: e.tensor_tensor(out=GG.t[:, :], in0=GG.t[:, :], in1=tmpb.t[:, :], op=ALU.mult),
             r=[GG, tmpb], w=[GG])

    for l in range(DEPTH):
        x_src = x_in.t if l == 0 else xlay
        x_dst = out_t.t if l == DEPTH - 1 else xlay
        lam_init = 0.8 - 0.6 * math.exp(-0.3 * l)

        W1 = sb([128, 8, 2728], BF16, "W1")
        stg = [sb([128, 8, 512], F32, "stg") for _ in range(2)]
        wv = W["w_in"].t[l].rearrange("(kc p) n -> p kc n", p=128)
        pieces = []
        c0 = 0
        while c0 < 2728:
            n = min(512, 2728 - c0)
            pieces.append(((c0, n), (c0, n)))
            c0 += n
        load_cast(W1, lambda c0, n: W1.t[:, :, c0:c0 + n], lambda c0, n: wv[:, :, c0:c0 + n], pieces, stg,
                  ["pool", "dve"])
        Acol = sb([128, 8], F32, "Acol"); Bcol = sb([128, 8], F32, "Bcol"); tmpc = sb([128, 8], F32, "tmpc")
        xts = [sb([128, D], F32, "xt") for _ in range(2)]
        xs = sb([128, D], BF16, "xs"); junk = sb([128, D], BF16, "junk"); ss = sb([128, 2], F32, "ss")
        hTb = [sb([128, 8, 512], BF16, "hT") for _ in range(2)]
        posi = sb([128, 512], I32, "posi"); posf = sb([128, 512], F32, "posf")
        ang = sb([128, 512], F32, "ang"); kk = sb([128, 512], F32, "kk"); rr = sb([128, 512], F32, "rr")
        r2 = sb([128, 512], F32, "r2"); mm_ = sb([128, 512], F32, "mm")
        cosT = {32: sb([128, 512], F32, "cos32"), 64: sb([128, 512], F32, "cos64")}
        sinT = {32: sb([128, 512], F32, "sin32"), 64: sb([128, 512], F32, "sin64")}
        qsb = [sb([128, 512], BF16, "qsb") for _ in range(2)]
        t1 = sb([128, 512], F32, "t1"); t2 = sb([128, 512], F32, "t2")
        outb = [sb([128, 512], BF16, "outb") for _ in range(3)]
        sg = sb([128, 512], F32, "sg")
        vtm = [sb([128, 512], BF16, "vtm") for _ in range(2)]
        vtd = [sb([128, 64], BF16, "vtd") for _ in range(2)]
        wtm = [sb([128, 8], F32, "wtm") for _ in range(2)]
        ob_i = [0]

        def next_outb():
            ob_i[0] += 1
            return outb[ob_i[0] % 3]

        for b in range(NSEQ):
            make_AB(l, b, "mix_pre_g", 0, 1, Acol, Bcol, tmpc)
            for tb in range(NB):
                t0 = tb * 512
                hT = hTb[(b * NB + tb) % 2]
                for tt in range(4):
                    xt = xts[tt % 2]
                    P.dma(xt.t[:, :], x_src[b, t0 + tt * 128:t0 + (tt + 1) * 128, :],
                          r=[HB("x%d" % l, b)], w=[xt])
                    norm_transpose(xt, hT, tt, Acol, Bcol, xs, ss, junk, 7)
                P.dma(hTs[b][:, :, t0:t0 + 512], hT.t[:, :, :], r=[hT], w=[HB("hTs", b)])
                ck(1)
                P.dma(posi.t[:, :], pos_in.t[b:b + 1, t0:t0 + 512].partition_broadcast(128), w=[posi])
                P.op("dve", lambda e: e.tensor_copy(out=posf.t[:, :], in_=posi.t[:, :]), r=[posi], w=[posf])
                for dd in (32, 64):
                    P.op("dve", lambda e, dd=dd: e.tensor_scalar(out=ang.t[:, :], in0=posf.t[:, :], scalar1=invf[dd],
                                                               scalar2=None, op0=ALU.mult), r=[posf, cst], w=[ang])
                    P.op("dve", lambda e: e.tensor_scalar(out=kk.t[:, :], in0=ang.t[:, :], scalar1=1.0 / TWO_PI,
                                                          scalar2=MAGIC, op0=ALU.mult, op1=ALU.add), r=[ang], w=[kk])
                    P.op("dve", lambda e: e.tensor_scalar(out=kk.t[:, :], in0=kk.t[:, :], scalar1=MAGIC, scalar2=None,
                                                          op0=ALU.subtract), r=[kk], w=[kk])
                    P.op("dve", lambda e: e.scalar_tensor_tensor(out=rr.t[:, :], in0=kk.t[:, :], scalar=-C1,
                                                                 in1=ang.t[:, :], op0=ALU.mult, op1=ALU.add),
                         r=[kk, ang], w=[rr])
                    P.op("dve", lambda e: e.scalar_tensor_tensor(out=rr.t[:, :], in0=kk.t[:, :], scalar=-C2,
                                                                 in1=rr.t[:, :], op0=ALU.mult, op1=ALU.add),
                         r=[kk, rr], w=[rr])
                    P.op("dve", lambda e: e.tensor_scalar(out=rr.t[:, :], in0=rr.t[:, :], scalar1=PI_LO,
                                                          scalar2=-PI_LO, op0=ALU.min, op1=ALU.max), r=[rr], w=[rr])
                    act(sinT[dd].t[:, :], rr.t[:, :], AF.Sin, [rr, cst], [sinT[dd]], scale=sgn[dd])
                    P.op("dve", lambda e: e.tensor_scalar(out=r2.t[:, :], in0=rr.t[:, :], scalar1=math.pi / 2,
                                                          scalar2=None, op0=ALU.add), r=[rr], w=[r2])
                    P.op("dve", lambda e: e.tensor_scalar(out=mm_.t[:, :], in0=r2.t[:, :], scalar1=math.pi,
                                                          scalar2=-TWO_PI, op0=ALU.is_gt, op1=ALU.mult),
                         r=[r2], w=[mm_])
                    P.op("dve", lambda e: e.tensor_tensor(out=r2.t[:, :], in0=r2.t[:, :], in1=mm_.t[:, :],
                                                          op=ALU.add), r=[r2, mm_], w=[r2])
                    P.op("dve", lambda e: e.tensor_scalar(out=r2.t[:, :], in0=r2.t[:, :], scalar1=PI_LO,
                                                          scalar2=-PI_LO, op0=ALU.min, op1=ALU.max), r=[r2], w=[r2])
                    act(cosT[dd].t[:, :], r2.t[:, :], AF.Sin, [r2], [cosT[dd]])

                ck(2)
                bank = [0]

                def proj_fm(c0, m):
                    pb = ps[bank[0] % 4]
                    bank[0] += 1
                    for kc in range(8):
                        P.op("pe", lambda e, pb=pb, kc=kc: e.matmul(pb.t[0:m, :], lhsT=W1.t[:, kc, c0:c0 + m],
                                                                    rhs=hT.t[:, kc, :], start=(kc == 0),
                                                                    stop=(kc == 7)), r=[W1, hT], w=[pb])
                    return pb

                def store(dst_ap, srct, m, key):
                    P.dma(dst_ap, srct.t[0:m, :], r=[srct], w=[HB(key, b)])

                for cc in range(2):
                    pa = proj_fm(cc * 128, 128)
                    pg = proj_fm(256 + cc * 128, 128)
                    act(sg.t[:, :], pg.t[:, :], AF.Sigmoid, [pg], [sg])
                    ob = next_outb()
                    P.op("dve", lambda e, pa=pa, ob=ob: e.tensor_tensor(out=ob.t[:, :], in0=pa.t[:, :], in1=sg.t[:, :],
                                                                        op=ALU.mult), r=[pa, sg], w=[ob])
                    store(fm["gluT"][b][:, cc, t0:t0 + 512], ob, 128, "gluT")

                ck(3)

                def roped(c0, m, dd, dst_ap, key):
                    pq = proj_fm(c0, m)
                    q = qsb[bank[0] % 2]
                    act(q.t[0:m, :], pq.t[0:m, :], AF.Identity, [pq], [q])
                    ck(8)
                    pw = ps[4 + bank[0] % 2]
                    pm = perm32 if dd == 32 else perm64
                    P.op("pe", lambda e: e.matmul(pw.t[0:m, :], lhsT=pm[0:m, 0:m], rhs=q.t[0:m, :], start=True,
                                                  stop=True), r=[q, cbf], w=[pw])
                    ck(9)
                    P.op("dve", lambda e: e.tensor_tensor(out=t1.t[0:m, :], in0=q.t[0:m, :], in1=cosT[dd].t[0:m, :],
                                                          op=ALU.mult), r=[q, cosT[dd]], w=[t1])
                    P.op("dve", lambda e: e.tensor_tensor(out=t2.t[0:m, :], in0=pw.t[0:m, :], in1=sinT[dd].t[0:m, :],
                                                          op=ALU.mult), r=[pw, sinT[dd]], w=[t2])
                    ob = next_outb()
                    P.op("dve", lambda e: e.tensor_tensor(out=ob.t[0:m, :], in0=t1.t[0:m, :], in1=t2.t[0:m, :],
                                                          op=ALU.add), r=[t1, t2], w=[ob])
                    store(dst_ap, ob, m, key)

                def plain(c0, m, dst_ap, key):
                    pq = proj_fm(c0, m)
                    ob = next_outb()
                    act(ob.t[0:m, :], pq.t[0:m, :], AF.Identity, [pq], [ob])
                    store(dst_ap, ob, m, key)

                for cc in range(2):
                    roped(512 + cc * 128, 128, 32, fm["qbT"][b][:, cc, t0:t0 + 512], "qbT")
                    ck(5)
                    roped(768 + cc * 128, 128, 32, fm["kbT"][b][:, cc, t0:t0 + 512], "kbT")
                    ck(6)
                    plain(1280 + cc * 128, 128, fm["qcT"][b][:, cc, t0:t0 + 512], "qcT")
                    ck(7)
                    plain(1536 + cc * 128, 128, fm["kcT"][b][:, cc, t0:t0 + 512], "kcT")
                    roped(2048 + cc * 128, 128, 64, fm["qdT"][b][:, cc, t0:t0 + 512], "qdT")
                    roped(2432 + cc * 128, 128, 32, fm["qiT"][b][:, cc, t0:t0 + 512], "qiT")
                roped(2304, 64, 64, kdT[b][:, t0:t0 + 512], "kdT")
                roped(2688, 32, 32, kiT[b][:, t0:t0 + 512], "kiT")
                ck(4)
                for tt in range(4):
                    pv_ = ps[6]
                    for kc in range(8):
                        P.op("pe", lambda e, kc=kc, tt=tt: e.matmul(pv_.t[:, 0:256], lhsT=hT.t[:, kc, tt * 128:(tt + 1) * 128],
                                                                    rhs=W1.t[:, kc, 1024:1280], start=(kc == 0),
                                                                    stop=(kc == 7)), r=[W1, hT], w=[pv_])
                    for kc in range(8):
                        P.op("pe", lambda e, kc=kc, tt=tt: e.matmul(pv_.t[:, 256:512], lhsT=hT.t[:, kc, tt * 128:(tt + 1) * 128],
                                                                    rhs=W1.t[:, kc, 1792:2048], start=(kc == 0),
                                                                    stop=(kc == 7)), r=[W1, hT], w=[pv_])
                    v = vtm[tt % 2]
                    act(v.t[:, :], pv_.t[:, :], AF.Identity, [pv_], [v])
                    P.dma(vbs[b, t0 + tt * 128:t0 + (tt + 1) * 128, :], v.t[:, 0:256], r=[v], w=[HB("vbs", b)])
                    P.dma(vcs[b, t0 + tt * 128:t0 + (tt + 1) * 128, :], v.t[:, 256:512], r=[v], w=[HB("vcs", b)])
                    pv2 = ps[5]
                    for kc in range(8):
                        P.op("pe", lambda e, kc=kc, tt=tt: e.matmul(pv2.t[:, 0:64], lhsT=hT.t[:, kc, tt * 128:(tt + 1) * 128],
                                                                    rhs=W1.t[:, kc, 2368:2432], start=(kc == 0),
                                                                    stop=(kc == 7)), r=[W1, hT], w=[pv2])
                    for kc in range(8):
                        P.op("pe", lambda e, kc=kc, tt=tt: e.matmul(pv2.t[:, 64:72], lhsT=hT.t[:, kc, tt * 128:(tt + 1) * 128],
                                                                    rhs=W1.t[:, kc, 2720:2728], start=(kc == 0),
                                                                    stop=(kc == 7)), r=[W1, hT], w=[pv2])
                    vd_ = vtd[tt % 2]; wt_ = wtm[tt % 2]
                    act(vd_.t[:, :], pv2.t[:, 0:64], AF.Identity, [pv2], [vd_])
                    act(wt_.t[:, :], pv2.t[:, 64:72], AF.Identity, [pv2], [wt_])
                    P.dma(vds[b, t0 + tt * 128:t0 + (tt + 1) * 128, :], vd_.t[:, :], r=[vd_], w=[HB("vds", b)])
                    P.dma(wis[b, t0 + tt * 128:t0 + (tt + 1) * 128, :], wt_.t[:, :], r=[wt_], w=[HB("wis", b)])
        stage_reset()

        cw = sb([128, 2, 31], F32, "cw")
        for cc in range(2):
            P.dma(cw.t[:, cc, :], W["conv_a_w"].t[l][:, cc * 128:(cc + 1) * 128].rearrange("j p -> p j"), w=[cw],
                  allow_slow_non_contiguous=True)
        cb3 = sb([128, 6], F32, "cb3")
        for i, nm in enumerate(("conv_a_b", "conv_a_ln_g", "conv_a_ln_b")):
            P.dma(cb3.t[:, 2 * i:2 * i + 2], W[nm].t[l].rearrange("(c p) -> p c", p=128), w=[cb3],
                  allow_slow_non_contiguous=True)
        dg = sb([128, 62, 128], BF16, "dg")
        for cc in range(2):
            for j in range(31):
                P.op("dve", lambda e, cc=cc, j=j: e.tensor_scalar(out=dg.t[:, cc * 31 + j, :], in0=cst.t[:, 0:128],
                                                                 scalar1=cw.t[:, cc, j:j + 1], scalar2=None,
                                                                 op0=ALU.mult), r=[cst, cw], w=[dg])
        o256 = sb([128, 128], F32, "o256")
        P.op("pool", lambda e: e.memset(o256.t[:, :], 1.0 / 256.0), w=[o256])
        gl = sb([128, 2, S + 32], BF16, "gl")
        cv = [sb([128, 2, 512], F32, "cv") for _ in range(2)]
        sq = sb([128, 2, 512], F32, "sq")
        mean_sb = sb([128, 512], F32, "mean_sb")
        m2 = sb([128, 512], F32, "m2"); var = sb([128, 512], F32, "var"); xc = sb([128, 512], F32, "xc")
        oa = [sb([128, 512], BF16, "oa") for _ in range(2)]
        for b in range(NSEQ):
            P.op("pool", lambda e: e.memset(gl.t[:, :, 0:32], 0.0), w=[gl])
            P.dma(gl.t[:, :, 32:32 + S], fm["gluT"][b][:, :, :], r=[HB("gluT", b)], w=[gl])
            for tb in range(NB):
                t0 = tb * 512
                cvt = cv[tb % 2]
                for cc in range(2):
                    pb = ps[cc]
                    for j in range(31):
                        P.op("pe", lambda e, cc=cc, j=j, pb=pb: e.matmul(
                            pb.t[:, :], lhsT=dg.t[:, cc * 31 + j, :], rhs=gl.t[:, cc, t0 + 2 + j:t0 + 2 + j + 512],
                            start=(j == 0), stop=(j == 30)), r=[dg, gl], w=[pb])
                    act(cvt.t[:, cc, :], pb.t[:, :], AF.Identity, [pb, cb3], [cvt], bias=cb3.t[:, cc:cc + 1])
                    act(sq.t[:, cc, :], cvt.t[:, cc, :], AF.Square, [cvt], [sq])
                pm_ = ps[2]; pe2 = ps[3]
                for cc in range(2):
                    P.op("pe", lambda e, cc=cc: e.matmul(pm_.t[:, :], lhsT=o256.t[:, :], rhs=cvt.t[:, cc, :],
                                                         start=(cc == 0), stop=(cc == 1)), r=[o256, cvt], w=[pm_])
                for cc in range(2):
                    P.op("pe", lambda e, cc=cc: e.matmul(pe2.t[:, :], lhsT=o256.t[:, :], rhs=sq.t[:, cc, :],
                                                         start=(cc == 0), stop=(cc == 1)), r=[o256, sq], w=[pe2])
                act(mean_sb.t[:, :], pm_.t[:, :], AF.Identity, [pm_], [mean_sb])
                act(m2.t[:, :], mean_sb.t[:, :], AF.Square, [mean_sb], [m2])
                P.op("dve", lambda e: e.tensor_tensor(out=var.t[:, :], in0=pe2.t[:, :], in1=m2.t[:, :], op=ALU.subtract),
                     r=[pe2, m2], w=[var])
                act(var.t[:, :], var.t[:, :], AF.Ln, [var], [var], bias=eps_c)
                act(var.t[:, :], var.t[:, :], AF.Exp, [var], [var], scale=-0.5)
                for cc in range(2):
                    P.op("dve", lambda e, cc=cc: e.tensor_tensor(out=xc.t[:, :], in0=cvt.t[:, cc, :], in1=mean_sb.t[:, :],
                                                                 op=ALU.subtract), r=[cvt, mean_sb], w=[xc])
                    P.op("dve", lambda e: e.tensor_tensor(out=xc.t[:, :], in0=xc.t[:, :], in1=var.t[:, :], op=ALU.mult),
                         r=[xc, var], w=[xc])
                    o_ = oa[cc]
                    act(o_.t[:, :], xc.t[:, :], AF.Silu, [xc, cb3], [o_], scale=cb3.t[:, 2 + cc:3 + cc],
                        bias=cb3.t[:, 4 + cc:5 + cc])
                    P.dma(fm["oaT"][b][:, cc, t0:t0 + 512], o_.t[:, :], r=[o_], w=[HB("oaT", b)])
        stage_reset()

        lamt = sb([128, 8], F32, "lamt")
        lq = sb([128, 4, 32], F32, "lq")
        for i, nm in enumerate(("lam_q1", "lam_k1", "lam_q2", "lam_k2")):
            P.dma(lq.t[:, i, :], W[nm].t[l:l + 1, :].partition_broadcast(128), w=[lq])
        lj = sb([128, 32], F32, "lj")
        P.op("pool", lambda e: e.memset(lamt.t[:, :], 0.0), w=[lamt])
        for i in range(2):
            P.op("dve", lambda e, i=i: e.tensor_tensor(out=lj.t[:, :], in0=lq.t[:, 2 * i, :], in1=lq.t[:, 2 * i + 1, :],
                                                       op=ALU.mult), r=[lq], w=[lj])
            P.op("dve", lambda e, i=i: e.tensor_reduce(out=lamt.t[:, i:i + 1], in_=lj.t[:, :], axis=AX.X, op=ALU.add),
                 r=[lj, lamt], w=[lamt])
            act(lamt.t[:, i:i + 1], lamt.t[:, i:i + 1], AF.Exp, [lamt], [lamt])
        P.op("dve", lambda e: e.tensor_tensor(out=lamt.t[:, 2:3], in0=lamt.t[:, 1:2], in1=lamt.t[:, 0:1],
                                              op=ALU.subtract), r=[lamt], w=[lamt])
        P.op("dve", lambda e: e.tensor_scalar(out=lamt.t[:, 2:3], in0=lamt.t[:, 2:3], scalar1=-lam_init, scalar2=None,
                                              op0=ALU.add), r=[lamt], w=[lamt])
        neglam = lamt.t[:, 2:3]
        gsub = sb([128, 1], F32, "gsub")
        for hh in range(2):
            P.dma(gsub.t[hh * 64:(hh + 1) * 64, :], W["diff_subln_g"].t[l].rearrange("(d o) -> d o", o=1), w=[gsub],
                  allow_slow_non_contiguous=True)
        P.op("dve", lambda e: e.tensor_scalar(out=gsub.t[:, :], in0=gsub.t[:, :], scalar1=1.0 - lam_init, scalar2=None,
                                              op0=ALU.mult), r=[gsub], w=[gsub])
        o64 = sb([128, 64], F32, "o64")
        P.op("pool", lambda e: e.memset(o64.t[:, :], 1.0 / 64.0), w=[o64])
        qT = sb([128, 2, S], BF16, "qT"); kT = sb([128, 2, S], BF16, "kT"); vv = sb([128, T, 256], BF16, "vv")
        pt = [sb([128, 512], BF16, "pt") for _ in range(3)]
        rc = [sb([64, 512], F32, "rc") for _ in range(2)]
        tO = [sb([64, 512], F32, "tO") for _ in range(2)]
        od = sb([64, 512], F32, "od"); osq = sb([64, 512], F32, "osq"); rs = sb([64, 512], F32, "rs")
        obo = [sb([64, 512], BF16, "obo") for _ in range(2)]
        pti = [0]
        SC_B = 32 ** -0.5
        for b in range(NSEQ):
            P.dma(qT.t[:, :, :], fm["qbT"][b][:, :, :], r=[HB("qbT", b)], w=[qT])
            P.dma(kT.t[:, :, :], fm["kbT"][b][:, :, :], r=[HB("kbT", b)], w=[kT])
            P.dma(vv.t[:, :, :], vbs[b].rearrange("(t p) f -> p t f", p=128), r=[HB("vbs", b)], w=[vv])
            for h in range(4):
                cc = h // 2
                for qb in range(NB):
                    q0 = qb * 512
                    nkt = qb * 4 + 4
                    for kt in range(nkt):
                        i = kt - qb * 4
                        cs = max(i, 0) * 128
                        for m in range(2):
                            j = (h % 2) * 2 + m
                            kw = {"tile_position": (96, 0)} if j == 3 else {}
                            pS = ps[(kt * 2 + m) % 3]
                            P.op("pe", lambda e, pS=pS, j=j, kw=kw, kt=kt, cs=cs: e.matmul(
                                pS.t[:, cs:512], lhsT=kT.t[32 * j:32 * j + 32, cc, kt * 128:(kt + 1) * 128],
                                rhs=qT.t[32 * j:32 * j + 32, cc, q0 + cs:q0 + 512], start=True, stop=True, **kw),
                                r=[kT, qT], w=[pS])
                            pti[0] += 1
                            p_ = pt[pti[0] % 3]
                            act(p_.t[:, cs:512], pS.t[:, cs:512], AF.Exp, [pS], [p_], scale=SC_B)
                            if i >= 0:
                                P.op("dve", lambda e, p_=p_, cs=cs: e.tensor_tensor(
                                    out=p_.t[:, cs:cs + 128], in0=p_.t[:, cs:cs + 128], in1=le_bf, op=ALU.mult),
                                    r=[p_, cbf], w=[p_])
                            pO = ps[3 + m]; pSm = ps[5 + m]
                            P.op("pe", lambda e, pO=pO, p_=p_, kt=kt, cs=cs: e.matmul(
                                pO.t[0:64, cs:512], lhsT=vv.t[:, kt, h * 64:(h + 1) * 64], rhs=p_.t[:, cs:512],
                                start=(kt == 0), stop=(kt == nkt - 1)), r=[vv, p_], w=[pO])
                            P.op("pe", lambda e, pSm=pSm, p_=p_, kt=kt, cs=cs: e.matmul(
                                pSm.t[0:64, cs:512], lhsT=ones_bf.t[:, 0:64], rhs=p_.t[:, cs:512],
                                start=(kt == 0), stop=(kt == nkt - 1)), r=[ones_bf, p_], w=[pSm])
                    for m in range(2):
                        P.op("dve", lambda e, m=m: e.reciprocal(out=rc[m].t[:, :], in_=ps[5 + m].t[0:64, :]),
                             r=[ps[5 + m]], w=[rc[m]])
                        P.op("dve", lambda e, m=m: e.tensor_tensor(out=tO[m].t[:, :], in0=ps[3 + m].t[0:64, :],
                                                                   in1=rc[m].t[:, :], op=ALU.mult),
                             r=[ps[3 + m], rc[m]], w=[tO[m]])
                    P.op("dve", lambda e: e.scalar_tensor_tensor(out=od.t[:, :], in0=tO[1].t[:, :], scalar=neglam[0:64, :],
                                                                 in1=tO[0].t[:, :], op0=ALU.mult, op1=ALU.add),
                         r=[tO[0], tO[1], lamt], w=[od])
                    act(osq.t[:, :], od.t[:, :], AF.Square, [od], [osq])
                    pst_ = ps[7]
                    P.op("pe", lambda e: e.matmul(pst_.t[0:64, :], lhsT=o64.t[0:64, :], rhs=osq.t[:, :], start=True,
                                                  stop=True), r=[o64, osq], w=[pst_])
                    act(rs.t[:, :], pst_.t[0:64, :], AF.Ln, [pst_], [rs], bias=eps_c[0:64, :])
                    act(rs.t[:, :], rs.t[:, :], AF.Exp, [rs], [rs], scale=-0.5)
                    o_ = obo[(h * NB + qb) % 2]
                    hb0 = (h % 2) * 64
                    P.op("dve", lambda e, o_=o_, hb0=hb0: e.scalar_tensor_tensor(
                        out=o_.t[:, :], in0=od.t[:, :], scalar=gsub.t[0:64, :], in1=rs.t[:, :],
                        op0=ALU.mult, op1=ALU.mult), r=[od, gsub, rs], w=[o_])
                    P.dma(fm["obT"][b][hb0:hb0 + 64, cc, q0:q0 + 512], o_.t[:, :], r=[o_], w=[HB("obT", b)])
        stage_reset()

        qT = sb([128, 2, S], BF16, "qT"); kT = sb([128, 2, S], BF16, "kT"); vv = sb([128, T, 256], BF16, "vv")
        ee = [sb([128, 512], F32, "ee") for _ in range(4)]
        spb = [sb([128, 512], BF16, "spb") for _ in range(4)]
        lsum2 = [sb([128, 512], BF16, "lsum") for _ in range(2)]
        a1 = [sb([128, 512], F32, "a1") for _ in range(4)]
        wT = [sb([128, 512], BF16, "wT") for _ in range(4)]
        oco = [sb([64, 512], BF16, "oco") for _ in range(2)]
        SC_C = 64 ** -0.5
        it = [0]
        for b in range(NSEQ):
            P.dma(qT.t[:, :, :], fm["qcT"][b][:, :, :], r=[HB("qcT", b)], w=[qT])
            P.dma(kT.t[:, :, :], fm["kcT"][b][:, :, :], r=[HB("kcT", b)], w=[kT])
            P.dma(vv.t[:, :, :], vcs[b].rearrange("(t p) f -> p t f", p=128), r=[HB("vcs", b)], w=[vv])
            for hp in (0, 2):
                for qb in range(NB):
                    q0 = qb * 512
                    top = qb * 4 + 3
                    lo_kt = 0 if cwin is None else max(0, top - 3 - cwin)
                    for hh in range(2):
                        P.op("pool", lambda e, hh=hh: e.memset(lsum2[hh].t[:, :], 0.0), w=[lsum2[hh]])

                    def phA(hh, kt):
                        h = hp + hh
                        cc = h // 2
                        hb0 = (h % 2) * 64
                        i = kt - qb * 4
                        cs = max(i, 0) * 128
                        it[0] += 1
                        n = it[0]
                        c = dict(h=h, i=i, cs=cs, kt=kt, pZ=ps[n % 3], pL=ps[3 + n % 3], e_=ee[n % 4], s_=spb[n % 4],
                                 a_=a1[n % 4], w_=wT[n % 4], pO=ps[6 + hh], lsum=lsum2[hh], first=(kt == top))
                        pZ, e_, s_ = c["pZ"], c["e_"], c["s_"]
                        P.op("pe", lambda e: e.matmul(
                            pZ.t[:, cs:512], lhsT=kT.t[hb0:hb0 + 64, cc, kt * 128:(kt + 1) * 128],
                            rhs=qT.t[hb0:hb0 + 64, cc, q0 + cs:q0 + 512], start=True, stop=True), r=[kT, qT], w=[pZ])
                        act(e_.t[:, cs:512], pZ.t[:, cs:512], AF.Exp, [pZ], [e_], scale=SC_C)
                        act(s_.t[:, cs:512], e_.t[:, cs:512], AF.Ln, [e_], [s_], bias=one_c)
                        if i >= 0:
                            P.op("dve", lambda e: e.tensor_tensor(
                                out=s_.t[:, cs:cs + 128], in0=s_.t[:, cs:cs + 128], in1=lt_bf, op=ALU.mult),
                                r=[s_, cbf], w=[s_])
                        return c

                    def phB(c):
                        pZ, pL, s_, a_, w_, lsum, cs, first, i, kt = (c[k] for k in
                                                                     ("pZ", "pL", "s_", "a_", "w_", "lsum", "cs", "first", "i", "kt"))
                        P.op("pe", lambda e: e.matmul(pL.t[:, cs:512], lhsT=gt_bf, rhs=s_.t[:, cs:512], start=True,
                                                      stop=first), r=[cbf, s_], w=[pL])
                        if not first:
                            P.op("pe", lambda e: e.matmul(pL.t[:, cs:512], lhsT=ones_bf.t[:, :], rhs=lsum.t[:, cs:512],
                                                          start=False, stop=True), r=[ones_bf, lsum], w=[pL])
                        if kt > lo_kt:
                            P.op("dve", lambda e: e.tensor_tensor(
                                out=lsum.t[:, cs:512], in0=lsum.t[:, cs:512], in1=s_.t[:, cs:512], op=ALU.add),
                                r=[lsum, s_], w=[lsum])
                        P.op("dve", lambda e: e.scalar_tensor_tensor(
                            out=a_.t[:, cs:512], in0=pZ.t[:, cs:512], scalar=SC_C, in1=s_.t[:, cs:512],
                            op0=ALU.mult, op1=ALU.subtract), r=[pZ, s_], w=[a_])
                        P.op("dve", lambda e: e.tensor_tensor(
                            out=a_.t[:, cs:512], in0=a_.t[:, cs:512], in1=pL.t[:, cs:512], op=ALU.subtract),
                            r=[a_, pL], w=[a_])
                        act(w_.t[:, cs:512], a_.t[:, cs:512], AF.Exp, [a_], [w_])
                        if i >= 0:
                            P.op("dve", lambda e: e.tensor_tensor(
                                out=w_.t[:, cs:cs + 128], in0=w_.t[:, cs:cs + 128], in1=lt_bf, op=ALU.mult),
                                r=[w_, cbf], w=[w_])

                    def phC(c):
                        pO, w_, cs, first, kt, h = (c[k] for k in ("pO", "w_", "cs", "first", "kt", "h"))
                        P.op("pe", lambda e: e.matmul(
                            pO.t[0:64, cs:512], lhsT=vv.t[:, kt, h * 64:(h + 1) * 64], rhs=w_.t[:, cs:512],
                            start=first, stop=(kt == lo_kt)), r=[vv, w_], w=[pO])

                    kts = list(range(top, lo_kt - 1, -1))
                    ctxA = [phA(hh, kts[0]) for hh in range(2)]
                    for ki, kt in enumerate(kts):
                        cur = ctxA
                        phB(cur[0])
                        if ki + 1 < len(kts):
                            ctxA = [phA(0, kts[ki + 1])]
                        phB(cur[1])
                        if ki + 1 < len(kts):
                            ctxA.append(phA(1, kts[ki + 1]))
                        phC(cur[0])
                        phC(cur[1])
                    for hh in range(2):
                        h = hp + hh
                        o_ = oco[hh]
                        act(o_.t[:, :], ps[6 + hh].t[0:64, :], AF.Identity, [ps[6 + hh]], [o_])
                        P.dma(fm["ocT"][b][(h % 2) * 64:(h % 2) * 64 + 64, h // 2, q0:q0 + 512], o_.t[:, :], r=[o_],
                              w=[HB("ocT", b)])
        stage_reset()

        qiT = sb([128, 2, S], BF16, "qiT"); ki4 = sb([128, S], BF16, "ki4")
        wi_ = sb([128, T, 8], F32, "wi")
        qdT = sb([128, 2, S], BF16, "qdT"); kd2 = sb([128, S], BF16, "kd2"); vd = sb([128, T, 64], BF16, "vd")
        sc = [sb([128, S], F32, "sc") for _ in range(2)]
        rl = [sb([128, 512], F32, "rl") for _ in range(3)]
        st = [sb([128, 8], F32, "st") for _ in range(2)]
        steps = [sb([128, NIT + 2], F32, "steps") for _ in range(2)]
        cnt = [sb([128, NIT + 2], F32, "cnt") for _ in range(2)]
        g2s = [sb([128, 1], F32, "g2") for _ in range(2)]
        jks = [sb([128, S], BF16, "jk") for _ in range(2)]
        mb = [sb([128, S], BF16, "mb") for _ in range(2)]
        MT = [sb([128, T, 128], BF16, "MT") for _ in range(2)]
        pd = [sb([128, 4, 128], BF16, "pd") for _ in range(3)]
        rcd = sb([64, 512], F32, "rcd")
        odo = [sb([64, 4, 128], BF16, "odo") for _ in range(2)]
        SC_D = 64 ** -0.5
        n_ = [0]
        for b in range(NSEQ):
            P.dma(qiT.t[:, :, :], fm["qiT"][b][:, :, :], r=[HB("qiT", b)], w=[qiT])
            for g in range(4):
                P.dma(ki4.t[32 * g:32 * g + 32, :], kiT[b][:, :], r=[HB("kiT", b)], w=[ki4])
            P.dma(wi_.t[:, :, :], wis[b].rearrange("(t p) f -> p t f", p=128), r=[HB("wis", b)], w=[wi_])
            P.dma(qdT.t[:, :, :], fm["qdT"][b][:, :, :], r=[HB("qdT", b)], w=[qdT])
            for g in range(2):
                P.dma(kd2.t[64 * g:64 * g + 64, :], kdT[b][:, :], r=[HB("kdT", b)], w=[kd2])
            P.dma(vd.t[:, :, :], vds[b].rearrange("(t p) f -> p t f", p=128), r=[HB("vds", b)], w=[vd])
            def sel_(qt):
                return ((qt + 1) * 128, sc[qt % 2], st[qt % 2], steps[qt % 2], cnt[qt % 2], mb[qt % 2], MT[qt % 2],
                        g2s[qt % 2], jks[qt % 2])

            def phase1(qt):
                kl, s_, st_, stp, cn, m_, mt_, g2, jk = sel_(qt)
                for kb in range((kl + 511) // 512):
                    k0 = kb * 512
                    nk = min(512, kl - k0)
                    for hi in range(8):
                        n_[0] += 1
                        pI = ps[n_[0] % 3]
                        r_ = rl[n_[0] % 3]
                        g = hi % 4
                        kw = {"tile_position": (96, 0)} if g == 3 else {}
                        P.op("pe", lambda e, pI=pI, g=g, hi=hi, kw=kw, k0=k0, nk=nk: e.matmul(
                            pI.t[:, 0:nk], lhsT=qiT.t[32 * g:32 * g + 32, hi // 4, qt * 128:(qt + 1) * 128],
                            rhs=ki4.t[32 * g:32 * g + 32, k0:k0 + nk], start=True, stop=True, **kw),
                            r=[qiT, ki4], w=[pI])
                        act(r_.t[:, 0:nk], pI.t[:, 0:nk], AF.Relu, [pI], [r_])
                        if hi == 0:
                            P.op("dve", lambda e, r_=r_, k0=k0, nk=nk: e.tensor_scalar(
                                out=s_.t[:, k0:k0 + nk], in0=r_.t[:, 0:nk], scalar1=wi_.t[:, qt, 0:1], scalar2=None,
                                op0=ALU.mult), r=[r_, wi_], w=[s_])
                        else:
                            P.op("dve", lambda e, r_=r_, k0=k0, nk=nk, hi=hi: e.scalar_tensor_tensor(
                                out=s_.t[:, k0:k0 + nk], in0=r_.t[:, 0:nk], scalar=wi_.t[:, qt, hi:hi + 1],
                                in1=s_.t[:, k0:k0 + nk], op0=ALU.mult, op1=ALU.add), r=[r_, wi_, s_], w=[s_])
                ck(20)
                P.op("dve", lambda e: e.tensor_reduce(out=st_.t[:, 0:1], in_=s_.t[:, 0:kl], axis=AX.X, op=ALU.min),
                     r=[s_], w=[st_])
                P.op("dve", lambda e: e.tensor_tensor(out=s_.t[:, kl - 128:kl], in0=s_.t[:, kl - 128:kl], in1=negm,
                                                      op=ALU.add), r=[s_, cst], w=[s_])
                P.op("dve", lambda e: e.tensor_reduce(out=st_.t[:, 1:2], in_=s_.t[:, 0:kl], axis=AX.X, op=ALU.max),
                     r=[s_, st_], w=[st_])
                P.op("dve", lambda e: e.tensor_tensor(out=st_.t[:, 2:3], in0=st_.t[:, 1:2], in1=st_.t[:, 0:1],
                                                      op=ALU.subtract), r=[st_], w=[st_])
                P.op("dve", lambda e: e.tensor_scalar(out=st_.t[:, 2:3], in0=st_.t[:, 2:3], scalar1=1.02, scalar2=0.002,
                                                      op0=ALU.mult, op1=ALU.add), r=[st_], w=[st_])
                P.op("dve", lambda e: e.scalar_tensor_tensor(out=st_.t[:, 3:4], in0=st_.t[:, 2:3], scalar=-0.5,
                                                             in1=st_.t[:, 1:2], op0=ALU.mult, op1=ALU.add),
                     r=[st_], w=[st_])
                P.op("dve", lambda e: e.tensor_scalar(out=stp.t[:, :], in0=pow2, scalar1=st_.t[:, 2:3], scalar2=None,
                                                      op0=ALU.mult), r=[st_, cst], w=[stp])
                ck(21)
                P.op("pool", lambda e: e.memset(cn.t[:, :], 0.0), w=[cn])

            def phase2(qt, itn):
                kl, s_, st_, stp, cn, m_, mt_, g2, jk = sel_(qt)
                P.op("dve", lambda e, itn=itn: e.tensor_scalar(
                    out=jk.t[:, 0:kl], in0=s_.t[:, 0:kl], scalar1=st_.t[:, 3:4], scalar2=zero_c, op0=ALU.is_ge,
                    op1=ALU.add, accum_out=cn.t[:, itn:itn + 1]), r=[s_, st_, cn], w=[jk, cn])
                P.op("dve", lambda e, itn=itn: e.tensor_scalar(
                    out=g2.t[:, :], in0=cn.t[:, itn:itn + 1], scalar1=topk_c, scalar2=stp.t[:, itn:itn + 1],
                    op0=ALU.is_ge, op1=ALU.mult), r=[cn, stp, cst], w=[g2])
                P.op("dve", lambda e, itn=itn: e.scalar_tensor_tensor(
                    out=st_.t[:, 3:4], in0=st_.t[:, 3:4], scalar=stp.t[:, itn + 1:itn + 2], in1=g2.t[:, :],
                    op0=ALU.subtract, op1=ALU.add), r=[st_, stp, g2], w=[st_])

            def phase3(qt):
                kl, s_, st_, stp, cn, m_, mt_, g2, jk = sel_(qt)
                P.op("dve", lambda e: e.tensor_tensor(out=st_.t[:, 3:4], in0=st_.t[:, 3:4], in1=stp.t[:, NIT:NIT + 1],
                                                      op=ALU.subtract), r=[st_, stp], w=[st_])
                P.op("dve", lambda e: e.tensor_scalar(out=m_.t[:, 0:kl], in0=s_.t[:, 0:kl], scalar1=st_.t[:, 3:4],
                                                      scalar2=None, op0=ALU.is_ge), r=[s_, st_], w=[m_])
                ck(22)
                for k8 in range((qt + 8) // 8):
                    nt_ = min(8, qt + 1 - k8 * 8)
                    pT = ps[3 + (n_[0] + k8) % 2]
                    pTv = pT.t[:, :].bitcast(BF16)
                    for u in range(nt_):
                        kt = k8 * 8 + u
                        P.op("pe", lambda e, pTv=pTv, u=u, kt=kt: e.transpose(
                            pTv[:, u * 128:(u + 1) * 128], m_.t[:, kt * 128:(kt + 1) * 128], ident_bf),
                            r=[m_, cbf], w=[pT])
                    act(mt_.t[:, k8 * 8:k8 * 8 + nt_, :], pTv[:, 0:nt_ * 128].rearrange("p (u q) -> p u q", q=128),
                        AF.Identity, [pT], [mt_])
                ck(23)
                pO = ps[5]; pSm = ps[6]
                for kt in range(qt + 1):
                    n_[0] += 1
                    pAB = (ps[0], ps[1]) if n_[0] % 2 == 0 else (ps[2], ps[7])
                    p_ = pd[n_[0] % 3]
                    for h in range(4):
                        hb0 = (h % 2) * 64
                        pS = pAB[h % 2]
                        P.op("pe", lambda e, pS=pS, h=h, hb0=hb0, kt=kt: e.matmul(
                            pS.t[:, (h // 2) * 128:(h // 2 + 1) * 128], lhsT=kd2.t[hb0:hb0 + 64, kt * 128:(kt + 1) * 128],
                            rhs=qdT.t[hb0:hb0 + 64, h // 2, qt * 128:(qt + 1) * 128], start=True, stop=True),
                            r=[kd2, qdT], w=[pS])
                    for g in range(2):
                        act(p_.t[:, 2 * g:2 * g + 2, :], pAB[g].t[:, 0:256].rearrange("p (h q) -> p h q", q=128), AF.Exp,
                            [pAB[g]], [p_], scale=SC_D)
                    for hp in range(4):
                        P.op("dve", lambda e, p_=p_, kt=kt, hp=hp: e.tensor_tensor(
                            out=p_.t[:, hp, :], in0=p_.t[:, hp, :], in1=mt_.t[:, kt, :], op=ALU.mult),
                            r=[p_, mt_], w=[p_])
                    pr = p_.t[:, :, :].rearrange("p h q -> p (h q)")
                    P.op("pe", lambda e, pr=pr, kt=kt: e.matmul(pO.t[0:64, :], lhsT=vd.t[:, kt, :], rhs=pr,
                                                                start=(kt == 0), stop=(kt == qt)), r=[vd, p_], w=[pO])
                    P.op("pe", lambda e, pr=pr, kt=kt: e.matmul(pSm.t[0:64, :], lhsT=ones_bf.t[:, 0:64], rhs=pr,
                                                                start=(kt == 0), stop=(kt == qt)), r=[ones_bf, p_], w=[pSm])
                P.op("dve", lambda e: e.reciprocal(out=rcd.t[:, :], in_=pSm.t[0:64, :]), r=[pSm], w=[rcd])
                o_ = odo[qt % 2]
                P.op("dve", lambda e, o_=o_: e.tensor_tensor(out=o_.t[:, :, :].rearrange("p h q -> p (h q)"),
                                                             in0=pO.t[0:64, :], in1=rcd.t[:, :], op=ALU.mult),
                     r=[pO, rcd], w=[o_])
                for hp in range(4):
                    h = (hp % 2) * 2 + hp // 2
                    hb0 = (h % 2) * 64
                    P.dma(fm["odT"][b][hb0:hb0 + 64, h // 2, qt * 128:(qt + 1) * 128], o_.t[:, hp, :], r=[o_],
                          w=[HB("odT", b)])

            for qp in range(0, T, 2):
                for qt in (qp, qp + 1):
                    phase1(qt)
                for itn in range(NIT):
                    for qt in (qp, qp + 1):
                        phase2(qt, itn)
                for qt in (qp, qp + 1):
                    phase3(qt)
        stage_reset()

        Wg = sb([128, 8, 4096], BF16, "Wg"); Wbr = sb([128, 8, D], BF16, "Wbr"); Wo = sb([128, 8, D], BF16, "Wo")
        stg = [sb([128, 8, 256], F32, "stg") for _ in range(2)]
        wv = W["w_in"].t[l].rearrange("(kc p) n -> p kc n", p=128)
        load_cast(Wg, lambda c0, n: Wg.t[:, :, c0:c0 + n], lambda c0, n: wv[:, :, 2728 + c0:2728 + c0 + n],
                  [((c0, 256), (c0, 256)) for c0 in range(0, 4096, 256)], stg, ["pool", "dve"])
        wbv = W["w_branch"].t[l].rearrange("i (kc p) n -> p (i kc) n", p=128)
        load_cast(Wbr, lambda c0, n: Wbr.t[:, :, c0:c0 + n], lambda c0, n: wbv[:, :, c0:c0 + n],
                  [((c0, 256), (c0, 256)) for c0 in range(0, D, 256)], stg, ["pool", "dve"])
        wov = W["w_out"].t[l].rearrange("(kc p) n -> p kc n", p=128)
        load_cast(Wo, lambda c0, n: Wo.t[:, :, c0:c0 + n], lambda c0, n: wov[:, :, c0:c0 + n],
                  [((c0, 256), (c0, 256)) for c0 in range(0, D, 256)], stg, ["pool", "dve"])
        GG = sb([128, D], F32, "GG"); tmpb = sb([128, D], F32, "tmpb")
        hTb = [sb([128, 8, 512], BF16, "hT") for _ in range(2)]
        oTb = [[sb([128, 2, 512], BF16, "oT") for _ in range(4)] for _ in range(2)]
        sgm = [sb([128, 512], F32, "sgm") for _ in range(2)]
        acc = sb([128, 512], F32, "acc"); tm = sb([128, 512], F32, "tm")
        mT = sb([128, 8, 512], BF16, "mT")
        ysb = [sb([128, D], F32, "ysb") for _ in range(2)]
        xts = [sb([128, D], F32, "xt") for _ in range(2)]
        junk = sb([128, D], BF16, "junk"); ss = sb([128, 2], F32, "ss")
        onames = ("oaT", "obT", "ocT", "odT")
        n3 = [0]
        for b in range(NSEQ):
            make_GG(l, b, "mix_post_g", 2, GG, tmpb)
            for tb in range(NB):
                t0 = tb * 512
                hT = hTb[tb % 2]; oT = oTb[tb % 2]
                P.dma(hT.t[:, :, :], hTs[b][:, :, t0:t0 + 512], r=[HB("hTs", b)], w=[hT])
                for i in range(4):
                    P.dma(oT[i].t[:, :, :], fm[onames[i]][b][:, :, t0:t0 + 512], r=[HB(onames[i], b)], w=[oT[i]])
                for oc in range(8):
                    for i in range(4):
                        n3[0] += 1
                        pG = ps[n3[0] % 2]; pB = ps[2 + n3[0] % 2]; sg_ = sgm[n3[0] % 2]
                        for kc in range(8):
                            P.op("pe", lambda e, pG=pG, kc=kc, i=i, oc=oc: e.matmul(
                                pG.t[:, :], lhsT=Wg.t[:, kc, i * D + oc * 128:i * D + (oc + 1) * 128], rhs=hT.t[:, kc, :],
                                start=(kc == 0), stop=(kc == 7)), r=[Wg, hT], w=[pG])
                        for kc in range(2):
                            P.op("pe", lambda e, pB=pB, kc=kc, i=i, oc=oc: e.matmul(
                                pB.t[:, :], lhsT=Wbr.t[:, i * 2 + kc, oc * 128:(oc + 1) * 128], rhs=oT[i].t[:, kc, :],
                                start=(kc == 0), stop=(kc == 1)), r=[Wbr, oT[i]], w=[pB])
                        act(sg_.t[:, :], pG.t[:, :], AF.Sigmoid, [pG], [sg_])
                        if i == 0:
                            P.op("dve", lambda e, sg_=sg_, pB=pB: e.tensor_tensor(out=acc.t[:, :], in0=sg_.t[:, :],
                                                                                  in1=pB.t[:, :], op=ALU.mult),
                                 r=[sg_, pB], w=[acc])
                        else:
                            P.op("dve", lambda e, sg_=sg_, pB=pB: e.tensor_tensor(out=tm.t[:, :], in0=sg_.t[:, :],
                                                                                  in1=pB.t[:, :], op=ALU.mult),
                                 r=[sg_, pB], w=[tm])
                            if i < 3:
                                P.op("dve", lambda e: e.tensor_tensor(out=acc.t[:, :], in0=acc.t[:, :], in1=tm.t[:, :],
                                                                       op=ALU.add), r=[acc, tm], w=[acc])
                            else:
                                P.op("dve", lambda e, oc=oc: e.tensor_tensor(out=mT.t[:, oc, :], in0=acc.t[:, :],
                                                                              in1=tm.t[:, :], op=ALU.add),
                                     r=[acc, tm], w=[mT])
                for tt in range(4):
                    y_ = ysb[tt % 2]; xt = xts[tt % 2]
                    P.dma(xt.t[:, :], x_src[b, t0 + tt * 128:t0 + (tt + 1) * 128, :], r=[HB("x%d" % l, b)], w=[xt])
                    for hf in range(2):
                        pY = ps[4 + hf]
                        for kc in range(8):
                            P.op("pe", lambda e, pY=pY, kc=kc, hf=hf, tt=tt: e.matmul(
                                pY.t[:, :], lhsT=mT.t[:, kc, tt * 128:(tt + 1) * 128], rhs=Wo.t[:, kc, hf * 512:(hf + 1) * 512],
                                start=(kc == 0), stop=(kc == 7)), r=[mT, Wo], w=[pY])
                        act(y_.t[:, hf * 512:(hf + 1) * 512], pY.t[:, :], AF.Identity, [pY], [y_])
                    post_norm_residual(y_, xt, GG, ss, junk, xmid[b, t0 + tt * 128:t0 + (tt + 1) * 128, :],
                                       HB("xmid%d" % l, b))
        stage_reset()

        Wu = sb([128, 8, 2 * DFF], BF16, "Wu")
        stg = [sb([128, 8, 256], F32, "stg") for _ in range(2)]
        wv = W["w_up"].t[l].rearrange("(kc p) n -> p kc n", p=128)
        load_cast(Wu, lambda c0, n: Wu.t[:, :, c0:c0 + n], lambda c0, n: wv[:, :, c0:c0 + n],
                  [((c0, 256), (c0, 256)) for c0 in range(0, 2 * DFF, 256)], stg, ["pool", "dve"])
        fcw = sb([128, 44, 3], F32, "fcw"); fcb = sb([128, 44], F32, "fcb")
        for j in range(3):
            P.dma(fcw.t[:, :, j], W["ffn_conv_w"].t[l][j].rearrange("(c p) -> p c", p=128), w=[fcw],
                  allow_slow_non_contiguous=True)
        P.dma(fcb.t[:, :], W["ffn_conv_b"].t[l].rearrange("(c p) -> p c", p=128), w=[fcb],
              allow_slow_non_contiguous=True)
        Acol = sb([128, 8], F32, "Acol"); Bcol = sb([128, 8], F32, "Bcol"); tmpc = sb([128, 8], F32, "tmpc")
        xts = [sb([128, D], F32, "xt") for _ in range(2)]
        xs = sb([128, D], BF16, "xs"); junk = sb([128, D], BF16, "junk"); ss = sb([128, 2], F32, "ss")
        hTb = [sb([128, 8, 512], BF16, "hT") for _ in range(2)]
        halo = sb([128, 44, 2], F32, "halo")
        ub = [sb([128, 514], F32, "ub") for _ in range(4)]
        c1_ = [sb([128, 512], F32, "c1") for _ in range(2)]
        gs = sb([128, 512], F32, "gs")
        aT = [sb([128, 22, 512], BF16, "aT") for _ in range(2)]
        n4 = [0]
        for b in range(NSEQ):
            make_AB(l, b, "ffn_pre_g", 3, 4, Acol, Bcol, tmpc)
            P.op("pool", lambda e: e.memset(halo.t[:, :, :], 0.0), w=[halo])
            for tb in range(NB):
                t0 = tb * 512
                hT = hTb[tb % 2]; a_ = aT[tb % 2]
                for tt in range(4):
                    xt = xts[tt % 2]
                    P.dma(xt.t[:, :], xmid[b, t0 + tt * 128:t0 + (tt + 1) * 128, :], r=[HB("xmid%d" % l, b)], w=[xt])
                    norm_transpose(xt, hT, tt, Acol, Bcol, xs, ss, junk, 7)
                for j in range(22):
                    res = []
                    for gv in range(2):
                        ch = gv * 22 + j
                        n4[0] += 1
                        pU = ps[n4[0] % 4]; u_ = ub[n4[0] % 4]; c_ = c1_[gv]
                        for kc in range(8):
                            P.op("pe", lambda e, pU=pU, kc=kc, ch=ch: e.matmul(
                                pU.t[:, :], lhsT=Wu.t[:, kc, ch * 128:(ch + 1) * 128], rhs=hT.t[:, kc, :],
                                start=(kc == 0), stop=(kc == 7)), r=[Wu, hT], w=[pU])
                        P.op("pool", lambda e, u_=u_, ch=ch: e.tensor_copy(out=u_.t[:, 0:2], in_=halo.t[:, ch, :]),
                             r=[halo], w=[u_])
                        act(u_.t[:, 2:514], pU.t[:, :], AF.Identity, [pU], [u_])
                        P.op("pool", lambda e, u_=u_, ch=ch: e.tensor_copy(out=halo.t[:, ch, :], in_=u_.t[:, 512:514]),
                             r=[u_], w=[halo])
                        P.op("dve", lambda e, u_=u_, c_=c_, ch=ch: e.tensor_scalar(
                            out=c_.t[:, :], in0=u_.t[:, 2:514], scalar1=fcw.t[:, ch, 2:3], scalar2=fcb.t[:, ch:ch + 1],
                            op0=ALU.mult, op1=ALU.add), r=[u_, fcw, fcb], w=[c_])
                        P.op("dve", lambda e, u_=u_, c_=c_, ch=ch: e.scalar_tensor_tensor(
                            out=c_.t[:, :], in0=u_.t[:, 1:513], scalar=fcw.t[:, ch, 1:2], in1=c_.t[:, :],
                            op0=ALU.mult, op1=ALU.add), r=[u_, fcw, c_], w=[c_])
                        P.op("dve", lambda e, u_=u_, c_=c_, ch=ch: e.scalar_tensor_tensor(
                            out=c_.t[:, :], in0=u_.t[:, 0:512], scalar=fcw.t[:, ch, 0:1], in1=c_.t[:, :],
                            op0=ALU.mult, op1=ALU.add), r=[u_, fcw, c_], w=[c_])
                        res.append(c_)
                    act(gs.t[:, :], res[0].t[:, :], AF.Silu, [res[0]], [gs])
                    P.op("dve", lambda e, j=j, a_=a_, rv=res[1]: e.tensor_tensor(out=a_.t[:, j, :], in0=gs.t[:, :],
                                                                                 in1=rv.t[:, :], op=ALU.mult),
                         r=[gs, res[1]], w=[a_])
                P.dma(aTs[b][:, :, t0:t0 + 512], a_.t[:, :, :], r=[a_], w=[HB("aTs", b)])
        stage_reset()

        Wd = sb([128, 22, D], BF16, "Wd")
        stg = [sb([128, 8, 512], F32, "stg") for _ in range(2)]
        wdv = W["w_down"].t[l].rearrange("(kc p) n -> p kc n", p=128)
        pieces = []
        for k0 in (0, 8, 16):
            nk = min(8, 22 - k0)
            for c0 in (0, 512):
                pieces.append(((k0, nk, c0), (k0, nk, c0)))
        load_cast(Wd, lambda k0, nk, c0: Wd.t[:, k0:k0 + nk, c0:c0 + 512],
                  lambda k0, nk, c0: wdv[:, k0:k0 + nk, c0:c0 + 512], pieces, stg, ["pool", "dve"])
        GG = sb([128, D], F32, "GG"); tmpb = sb([128, D], F32, "tmpb")
        aT = [sb([128, 22, 512], BF16, "aT") for _ in range(2)]
        ysb = [sb([128, D], F32, "ysb") for _ in range(2)]
        xts = [sb([128, D], F32, "xt") for _ in range(2)]
        junk = sb([128, D], BF16, "junk"); ss = sb([128, 2], F32, "ss")
        for b in range(NSEQ):
            make_GG(l, b, "ffn_post_g", 5, GG, tmpb)
            for tb in range(NB):
                t0 = tb * 512
                a_ = aT[tb % 2]
                P.dma(a_.t[:, :, :], aTs[b][:, :, t0:t0 + 512], r=[HB("aTs", b)], w=[a_])
                for tt in range(4):
                    y_ = ysb[tt % 2]; xt = xts[tt % 2]
                    P.dma(xt.t[:, :], xmid[b, t0 + tt * 128:t0 + (tt + 1) * 128, :], r=[HB("xmid%d" % l, b)], w=[xt])
                    for hf in range(2):
                        pY = ps[(tt * 2 + hf) % 4]
                        for kc in range(22):
                            P.op("pe", lambda e, pY=pY, kc=kc, hf=hf, tt=tt: e.matmul(
                                pY.t[:, :], lhsT=a_.t[:, kc, tt * 128:(tt + 1) * 128], rhs=Wd.t[:, kc, hf * 512:(hf + 1) * 512],
                                start=(kc == 0), stop=(kc == 21)), r=[a_, Wd], w=[pY])
                        act(y_.t[:, hf * 512:(hf + 1) * 512], pY.t[:, :], AF.Identity, [pY], [y_])
                    post_norm_residual(y_, xt, GG, ss, junk, x_dst[b, t0 + tt * 128:t0 + (tt + 1) * 128, :],
                                       HB("x%d" % (l + 1), b) if l < DEPTH - 1 else out_t)
        stage_reset()


_CACHE = {}


def kernel(**inputs):
    NCORES = 8
    x = np.ascontiguousarray(inputs["x"], dtype=np.float32)
    Bt, S, _ = x.shape
    NSEQ = Bt // NCORES
    DEPTH = inputs["ada_w"].shape[0]
    key = (S, NSEQ, DEPTH)
    if key not in _CACHE:
        _CACHE[key] = build(S, NSEQ, DEPTH)[0]
    nc = _CACHE[key]
    consts = make_consts(S)
    wnames = ("ada_w", "ada_b", "mix_pre_g", "mix_post_g", "ffn_pre_g", "ffn_post_g", "w_in", "conv_a_w",
              "conv_a_b", "conv_a_ln_g", "conv_a_ln_b", "lam_q1", "lam_k1", "lam_q2", "lam_k2", "diff_subln_g",
              "w_branch", "w_out", "w_up", "ffn_conv_w", "ffn_conv_b", "w_down")
    shared = {n: np.ascontiguousarray(inputs[n], dtype=np.float32) for n in wnames}
    c = np.ascontiguousarray(inputs["c"], dtype=np.float32)
    pos = np.ascontiguousarray(inputs["positions"], dtype=np.int32)
    in_maps = []
    for i in range(NCORES):
        m = dict(shared)
        m["x"] = x[i * NSEQ:(i + 1) * NSEQ]
        m["c"] = c[i * NSEQ:(i + 1) * NSEQ]
        m["positions"] = pos[i * NSEQ:(i + 1) * NSEQ]
        m["consts"] = consts
        in_maps.append(m)
    res = run_bass_kernel_spmd(nc, in_maps, core_ids=list(range(NCORES)))
    return np.concatenate([r["out"] for r in res.results], axis=0).astype(np.float32)
```

```python
import math
import contextlib
import numpy as np
import concourse.bass as bass
import concourse.mybir as mybir
from concourse.bass_utils import run_bass_kernel_spmd

F32 = mybir.dt.float32
BF16 = mybir.dt.bfloat16
I32 = mybir.dt.int32
AF = mybir.ActivationFunctionType
ALU = mybir.AluOpType
AX = mybir.AxisListType

D = 1024
DIN = 6824
DFF = 2816
EPS = 1e-6
THETA = 10000.0
NIT = 22
MAGIC = 12582912.0
TWO_PI = 2.0 * math.pi
C1 = 6.28125
C2 = TWO_PI - C1
PI_LO = 3.1415925


class Buf:
    __slots__ = ("w", "r")

    def __init__(self):
        self.w = None
        self.r = []


class Tl:
    __slots__ = ("t", "b")

    def __init__(self, t):
        self.t = t
        self.b = Buf()


def _b(x):
    return x.b if isinstance(x, Tl) else x


class Op:
    __slots__ = ("ex", "q", "emit", "deps", "sig", "seq", "isdma")


class _Rec:
    def __init__(self):
        self.call = None

    def __getattr__(self, name):
        def f(*a, **k):
            self.call = (name, a, k)
            return None
        return f


class Prog:
    NSLOT = 8

    def __init__(self, nc):
        self.nc = nc
        self.ops = []
        self.slot_rr = {}

    def op(self, ex, emit, r=(), w=()):
        o = Op()
        rec = _Rec()
        emit(rec)
        nm_, a_, k_ = rec.call
        o.ex = ex; o.q = ex; o.isdma = False; o.sig = False; o.seq = None
        o.emit = lambda eng, nm_=nm_, a_=a_, k_=k_: getattr(eng, nm_)(*a_, **k_)
        self._deps(o, r, w)
        self.ops.append(o)
        return o

    def dma(self, out, in_, r=(), w=(), q="sp", **kw):
        o = Op()
        o.q = q; o.isdma = True; o.sig = True; o.seq = None
        s = self.slot_rr.get(q, 0)
        self.slot_rr[q] = (s + 1) % self.NSLOT
        o.ex = ("dma", q, s)
        o.emit = lambda eng: eng.dma_start(out=out, in_=in_, **kw)
        self._deps(o, r, w)
        self.ops.append(o)
        return o

    def _deps(self, o, reads, writes):
        deps = []
        for x in reads:
            b = _b(x)
            if b.w is not None:
                deps.append((b.w, True))
        for x in writes:
            b = _b(x)
            if b.w is not None:
                deps.append((b.w, False))
            for rr in b.r:
                deps.append((rr, False))
        o.deps = deps
        for x in reads:
            _b(x).r.append(o)
        for x in writes:
            b = _b(x)
            b.w = o
            b.r = []

    def barrier(self):
        last = {}
        for o in self.ops:
            if o.emit is not None:
                last[o.ex] = o
        prev = list(last.values())
        for q in ("pe", "act", "dve", "pool", "sp"):
            o = Op()
            o.ex = "bar_" + q; o.q = q; o.isdma = False; o.sig = False; o.seq = None
            o.emit = None
            o.deps = [(p, True) for p in prev]
            self.ops.append(o)

    def emit_all(self):
        nc = self.nc
        for o in self.ops:
            for (p, raw) in o.deps:
                if p.isdma:
                    continue
                if p.ex == o.ex and not raw and not o.isdma:
                    continue
                p.sig = True
        cnt = {}
        for o in self.ops:
            if o.isdma:
                cnt[o.ex] = cnt.get(o.ex, 0) + 16
                o.seq = cnt[o.ex]
            elif o.sig:
                cnt[o.ex] = cnt.get(o.ex, 0) + 1
                o.seq = cnt[o.ex]
        prev_on_slot = {}
        for o in self.ops:
            if o.isdma:
                p = prev_on_slot.get(o.ex)
                if p is not None:
                    o.deps.append((p, True))
                prev_on_slot[o.ex] = o
        stack = contextlib.ExitStack()
        sems = {}
        for o in self.ops:
            if o.seq is not None and o.ex not in sems:
                nm = "s_" + ("_".join(str(x) for x in o.ex) if isinstance(o.ex, tuple) else o.ex)
                sems[o.ex] = stack.enter_context(nc.semaphore(nm))
        byq = {"pe": [], "act": [], "dve": [], "pool": [], "sp": []}
        for o in self.ops:
            byq[o.q].append(o)
        self.n_inst = 0

        def run(eng, lst):
            seen = {}
            for o in lst:
                need = {}
                for (p, raw) in o.deps:
                    if (not p.isdma) and p.ex == o.ex and not raw and not o.isdma:
                        continue
                    if p.seq is None:
                        continue
                    if need.get(p.ex, 0) < p.seq:
                        need[p.ex] = p.seq
                for ex, v in need.items():
                    if seen.get(ex, 0) >= v:
                        continue
                    seen[ex] = v
                    eng.wait_ge(sems[ex], v)
                    self.n_inst += 1
                if o.emit is None:
                    continue
                ins = o.emit(eng)
                self.n_inst += 1
                if o.isdma:
                    ins.then_inc(sems[o.ex], 16)
                elif o.sig:
                    ins.then_inc(sems[o.ex], 1)

        with stack, nc.Block() as block:
            @block.tensor
            def _(e):
                run(e, byq["pe"])

            @block.scalar
            def _(e):
                run(e, byq["act"])

            @block.vector
            def _(e):
                run(e, byq["dve"])

            @block.gpsimd
            def _(e):
                run(e, byq["pool"])

            @block.sync
            def _(e):
                run(e, byq["sp"])


def make_consts(S=4096):
    p = np.arange(128)[:, None]
    f = np.arange(128)[None, :]
    c = np.zeros((128, 1024), np.float32)
    c[:, 0:128] = (p == f)
    c[:, 128:256] = (p <= f)
    c[:, 256:384] = (p < f)
    c[:, 384:512] = (p > f)
    c[:, 512:640] = np.where(f <= p, 0.0, -1e30)
    sw32 = np.where(f % 32 < 16, f + 16, f - 16)
    c[:, 640:768] = (p == sw32)
    sw64 = np.where(f % 64 < 32, f + 32, f - 32)
    c[:, 768:896] = (p == sw64)
    pp = np.arange(128)
    c[:, 896] = THETA ** (-(2.0 * (pp % 16)) / 32.0)
    c[:, 897] = THETA ** (-(2.0 * (pp % 32)) / 64.0)
    c[:, 898] = np.where(pp % 32 < 16, -1.0, 1.0)
    c[:, 899] = np.where(pp % 64 < 32, -1.0, 1.0)
    c[:, 900] = EPS
    c[:, 901] = 1.0
    c[:, 902] = 0.0
    c[:, 903] = float(min(256, S // 4)) - 0.5
    for i in range(NIT + 2):
        c[:, 904 + i] = 2.0 ** (-(i + 1))
    return c


class _Stop(Exception):
    pass


def build(S, NSEQ, DEPTH, debug=False, cwin=None, stop=None):
    h = {}
    try:
        _build(h, S, NSEQ, DEPTH, debug, cwin, stop)
    except _Stop:
        pass
    h["P"].barrier()
    h["P"].emit_all()
    return h["nc"], h["P"]


def _build(holder, S, NSEQ, DEPTH, debug, cwin, stop):
    nc = bass.Bass("TRN2", target_bir_lowering=False)
    P = Prog(nc)
    holder["nc"] = nc
    holder["P"] = P
    T = S // 128
    NB = S // 512
    TOPK = min(256, S // 4)
    dbg_kind = "ExternalOutput" if debug else "Internal"

    def din(name, shape, dt=F32):
        return Tl(nc.dram_tensor(name, list(shape), dt, kind="ExternalInput").ap())

    def dscr(name, shape, dt):
        return nc.dram_tensor(name, list(shape), dt, kind=dbg_kind).ap()

    x_in = din("x", [NSEQ, S, D])
    c_in = din("c", [NSEQ, D])
    pos_in = din("positions", [NSEQ, S], I32)
    consts_in = din("consts", [128, 1024])
    W = {}
    for nm, shp in (("ada_w", [DEPTH, D, 6 * D]), ("ada_b", [DEPTH, 6 * D]), ("mix_pre_g", [DEPTH, D]),
                    ("mix_post_g", [DEPTH, D]), ("ffn_pre_g", [DEPTH, D]), ("ffn_post_g", [DEPTH, D]),
                    ("w_in", [DEPTH, D, DIN]), ("conv_a_w", [DEPTH, 31, 256]), ("conv_a_b", [DEPTH, 256]),
                    ("conv_a_ln_g", [DEPTH, 256]), ("conv_a_ln_b", [DEPTH, 256]), ("lam_q1", [DEPTH, 32]),
                    ("lam_k1", [DEPTH, 32]), ("lam_q2", [DEPTH, 32]), ("lam_k2", [DEPTH, 32]),
                    ("diff_subln_g", [DEPTH, 64]), ("w_branch", [DEPTH, 4, 256, D]), ("w_out", [DEPTH, D, D]),
                    ("w_up", [DEPTH, D, 2 * DFF]), ("ffn_conv_w", [DEPTH, 3, 2 * DFF]),
                    ("ffn_conv_b", [DEPTH, 2 * DFF]), ("w_down", [DEPTH, DFF, D])):
        W[nm] = din(nm, shp)
    out_t = Tl(nc.dram_tensor("out", [NSEQ, S, D], F32, kind="ExternalOutput").ap())

    modbuf = Tl(dscr("modbuf", [DEPTH, NSEQ, 6 * D], F32))
    xmid = dscr("xmid", [NSEQ, S, D], F32)
    xlay = dscr("xlay", [NSEQ, S, D], F32)
    hTs = dscr("hTs", [NSEQ, 128, 8, S], BF16)
    fm = {k: dscr(k, [NSEQ, 128, 2, S], BF16) for k in
          ("gluT", "qbT", "kbT", "qcT", "kcT", "qdT", "qiT", "oaT", "obT", "ocT", "odT")}
    kdT = dscr("kdT", [NSEQ, 64, S], BF16)
    kiT = dscr("kiT", [NSEQ, 32, S], BF16)
    vbs = dscr("vbs", [NSEQ, S, 256], BF16)
    vcs = dscr("vcs", [NSEQ, S, 256], BF16)
    vds = dscr("vds", [NSEQ, S, 64], BF16)
    wis = dscr("wis", [NSEQ, S, 8], F32)
    aTs = dscr("aTs", [NSEQ, 128, 22, S], BF16)
    hb = {}

    def HB(name, b):
        k = (name, b)
        if k not in hb:
            hb[k] = Buf()
        return hb[k]

    SB_BASE = (nc.sbuf_base + 63) // 64 * 64
    SB_TOP = nc.sbuf_top
    off = [SB_BASE]
    uid = [0]

    def sb(shape, dt, name="t"):
        esz = 4 if dt in (F32, I32) else 2
        nbytes = int(np.prod(shape[1:])) * esz
        nbytes = (nbytes + 63) // 64 * 64
        uid[0] += 1
        t = nc.alloc_sbuf_tensor_at(f"{name}_{uid[0]}", list(shape), dt, offset=off[0])
        off[0] += nbytes
        assert off[0] <= SB_TOP, f"SBUF overflow at {name}: {off[0]} > {SB_TOP}"
        return Tl(t)

    ps = [Tl(nc.alloc_psum_tensor(f"psb{i}", [128, 512], F32)) for i in range(8)]

    def psbf(i):
        return ps[i].t[:, :].bitcast(BF16)

    cst = sb([128, 1024], F32, "cst")
    P.dma(cst.t[:, :], consts_in.t[:, :], w=[cst])
    cbf = sb([128, 896], BF16, "cbf")
    P.op("dve", lambda e: e.tensor_copy(out=cbf.t[:, :], in_=cst.t[:, 0:896]), r=[cst], w=[cbf])
    ones_bf = sb([128, 128], BF16, "ones")
    P.op("pool", lambda e: e.memset(ones_bf.t[:, :], 1.0), w=[ones_bf])
    ones32 = sb([128, 128], F32, "ones32")
    P.op("pool", lambda e: e.memset(ones32.t[:, :], 1.0), w=[ones32])
    ident_bf = cbf.t[:, 0:128]
    le_bf = cbf.t[:, 128:256]
    lt_bf = cbf.t[:, 256:384]
    gt_bf = cbf.t[:, 384:512]
    negm = cst.t[:, 512:640]
    perm32 = cbf.t[:, 640:768]
    perm64 = cbf.t[:, 768:896]
    invf = {32: cst.t[:, 896:897], 64: cst.t[:, 897:898]}
    sgn = {32: cst.t[:, 898:899], 64: cst.t[:, 899:900]}
    eps_c = cst.t[:, 900:901]
    one_c = cst.t[:, 901:902]
    pow2 = cst.t[:, 904:904 + NIT + 2]
    topk_c = cst.t[:, 903:904]
    zero_c = cst.t[:, 902:903]
    PERSIST = off[0]

    import os as _os
    SUB = int(_os.environ.get('SUB', '0'))

    def ck(n):
        if SUB == n:
            raise _Stop()

    stage_no = [0]

    def stage_reset():
        P.barrier()
        stage_no[0] += 1
        if stop is not None and stage_no[0] >= stop:
            raise _Stop()
        print('stage sbuf bytes', off[0], 'of', SB_TOP, flush=True)
        off[0] = PERSIST

    def act(out, in_, func, r, w, **kw):
        P.op("act", lambda e: e.activation(out=out, in_=in_, func=func, **kw), r=r, w=w)

    def rstd_from_ss(ss, n, r, w_tmp):
        act(ss, ss, AF.Ln, r, w_tmp, scale=1.0 / n, bias=eps_c[0:ss.shape[0], :])
        act(ss, ss, AF.Exp, w_tmp, w_tmp, scale=-0.5)

    def load_cast(dst_tl, dst_ap_fn, src_ap_fn, pieces, stg, eng_cycle):
        for i, (dargs, sargs) in enumerate(pieces):
            st = stg[i % len(stg)]
            d_ap = dst_ap_fn(*dargs)
            s_ap = src_ap_fn(*sargs)
            shp = list(d_ap.shape)
            if len(shp) == 3:
                st_ap = st.t[0:shp[0], 0:shp[1], 0:shp[2]]
            else:
                st_ap = st.t[0:shp[0], 0, 0:shp[1]]
            P.dma(st_ap, s_ap, w=[st])
            ex = eng_cycle[i % len(eng_cycle)]
            P.op(ex, lambda e, o=d_ap, a=st_ap: e.tensor_copy(out=o, in_=a), r=[st], w=[dst_tl])

    cT = sb([128, 8, NSEQ], F32, "cT")
    for b_ in range(NSEQ):
        P.dma(cT.t[:, :, b_], c_in.t[b_].rearrange("(c p) -> p c", p=128), w=[cT], allow_slow_non_contiguous=True)
    act(cT.t[:, :, :], cT.t[:, :, :], AF.Silu, [cT], [cT])
    adst = [sb([128, 8, 512], F32, "adst") for _ in range(2)]
    modsb = sb([NSEQ, 6 * D], F32, "modsb")
    adab = sb([NSEQ, 6 * D], F32, "adab")
    for l in range(DEPTH):
        P.dma(adab.t[:, :], W["ada_b"].t[l:l + 1, :].partition_broadcast(NSEQ), w=[adab])
        wv = W["ada_w"].t[l].rearrange("(kc p) n -> p kc n", p=128)
        for nb in range(12):
            st = adst[nb % 2]
            P.dma(st.t[:, :, :], wv[:, :, nb * 512:(nb + 1) * 512], w=[st])
            pb = ps[nb % 2]
            for kc in range(8):
                P.op("pe", lambda e, st=st, pb=pb, kc=kc: e.matmul(
                    pb.t[0:NSEQ, :], lhsT=cT.t[:, kc, :], rhs=st.t[:, kc, :], start=(kc == 0), stop=(kc == 7)),
                    r=[cT, st], w=[pb])
            P.op("dve", lambda e, pb=pb, nb=nb: e.tensor_tensor(
                out=modsb.t[:, nb * 512:(nb + 1) * 512], in0=pb.t[0:NSEQ, :],
                in1=adab.t[:, nb * 512:(nb + 1) * 512], op=ALU.add), r=[pb, adab], w=[modsb])
        P.dma(modbuf.t[l], modsb.t[:, :], r=[modsb], w=[modbuf])
    stage_reset()

    def col8(ap1d):
        return ap1d.rearrange("(c p) -> p c", p=128)

    def norm_transpose(xt, hT, tt, Acol, Bcol, xs, ss, junk, pbank):
        act(junk.t[:, :], xt.t[:, :], AF.Square, [xt], [junk, ss], accum_out=ss.t[:, 0:1])
        rstd_from_ss(ss.t[:, 0:1], D, [ss], [ss])
        P.op("dve", lambda e: e.tensor_scalar(out=xs.t[:, :], in0=xt.t[:, :], scalar1=ss.t[:, 0:1], scalar2=None,
                                              op0=ALU.mult), r=[xt, ss], w=[xs])
        pv = psbf(pbank)
        for c in range(8):
            P.op("pe", lambda e, c=c: e.transpose(pv[:, c * 128:(c + 1) * 128], xs.t[:, c * 128:(c + 1) * 128],
                                                  ident_bf), r=[xs, cbf], w=[ps[pbank]])
        for c in range(8):
            act(hT.t[:, c, tt * 128:(tt + 1) * 128], pv[:, c * 128:(c + 1) * 128], AF.Identity,
                [ps[pbank], Acol, Bcol], [hT], scale=Acol.t[:, c:c + 1], bias=Bcol.t[:, c:c + 1])

    def post_norm_residual(ysb, xt, GG, ss, junk, dst_ap, dst_buf):
        act(junk.t[:, :], ysb.t[:, :], AF.Square, [ysb], [junk, ss], accum_out=ss.t[:, 0:1])
        rstd_from_ss(ss.t[:, 0:1], D, [ss], [ss])
        P.op("dve", lambda e: e.scalar_tensor_tensor(out=ysb.t[:, :], in0=ysb.t[:, :], scalar=ss.t[:, 0:1],
                                                     in1=GG.t[:, :], op0=ALU.mult, op1=ALU.mult),
             r=[ysb, ss, GG], w=[ysb])
        P.op("dve", lambda e: e.tensor_tensor(out=ysb.t[:, :], in0=ysb.t[:, :], in1=xt.t[:, :], op=ALU.add),
             r=[ysb, xt], w=[ysb])
        P.dma(dst_ap, ysb.t[:, :], r=[ysb], w=[dst_buf])

    def mod_cols(l, b, k):
        return col8(modbuf.t[l, b, k * D:(k + 1) * D])

    def make_AB(l, b, gname, ksh, ksc, Acol, Bcol, tmp):
        P.dma(Bcol.t[:, :], mod_cols(l, b, ksh), r=[modbuf], w=[Bcol], allow_slow_non_contiguous=True)
        P.dma(tmp.t[:, :], mod_cols(l, b, ksc), r=[modbuf], w=[tmp], allow_slow_non_contiguous=True)
        P.dma(Acol.t[:, :], col8(W[gname].t[l]), w=[Acol], allow_slow_non_contiguous=True)
        P.op("dve", lambda e: e.scalar_tensor_tensor(out=Acol.t[:, :], in0=tmp.t[:, :], scalar=1.0, in1=Acol.t[:, :],
                                                     op0=ALU.add, op1=ALU.mult), r=[tmp, Acol], w=[Acol])

    def make_GG(l, b, gname, kg, GG, tmpb):
        P.dma(GG.t[:, :], W[gname].t[l:l + 1, :].partition_broadcast(128), w=[GG])
        P.dma(tmpb.t[:, :], modbuf.t[l, b:b + 1, kg * D:(kg + 1) * D].partition_broadcast(128), r=[modbuf], w=[tmpb])
        P.op("dve", lambda e: e.tensor_tensor(out=GG.t[:, :], in0=GG.t[:, :], in1=tmpb.t[:, :], op=ALU.mult),
             r=[GG, tmpb], w=[GG])

    for l in range(DEPTH):
        x_src = x_in.t if l == 0 else xlay
        x_dst = out_t.t if l == DEPTH - 1 else xlay
        lam_init = 0.8 - 0.6 * math.exp(-0.3 * l)

        W1 = sb([128, 8, 2728], BF16, "W1")
        stg = [sb([128, 8, 512], F32, "stg") for _ in range(2)]
        wv = W["w_in"].t[l].rearrange("(kc p) n -> p kc n", p=128)
        pieces = []
        c0 = 0
        while c0 < 2728:
            n = min(512, 2728 - c0)
            pieces.append(((c0, n), (c0, n)))
            c0 += n
        load_cast(W1, lambda c0, n: W1.t[:, :, c0:c0 + n], lambda c0, n: wv[:, :, c0:c0 + n], pieces, stg,
                  ["pool", "dve"])
        Acol = sb([128, 8], F32, "Acol"); Bcol = sb([128, 8], F32, "Bcol"); tmpc = sb([128, 8], F32, "tmpc")
        xts = [sb([128, D], F32, "xt") for _ in range(2)]
        xs = sb([128, D], BF16, "xs"); junk = sb([128, D], BF16, "junk"); ss = sb([128, 2], F32, "ss")
        hTb = [sb([128, 8, 512], BF16, "hT") for _ in range(2)]
        posi = sb([128, 512], I32, "posi"); posf = sb([128, 512], F32, "posf")
        ang = sb([128, 512], F32, "ang"); kk = sb([128, 512], F32, "kk"); rr = sb([128, 512], F32, "rr")
        r2 = sb([128, 512], F32, "r2"); mm_ = sb([128, 512], F32, "mm")
        cosT = {32: sb([128, 512], F32, "cos32"), 64: sb([128, 512], F32, "cos64")}
        sinT = {32: sb([128, 512], F32, "sin32"), 64: sb([128, 512], F32, "sin64")}
        qsb = [sb([128, 512], BF16, "qsb") for _ in range(2)]
        t1 = sb([128, 512], F32, "t1"); t2 = sb([128, 512], F32, "t2")
        outb = [sb([128, 512], BF16, "outb") for _ in range(3)]
        sg = sb([128, 512], F32, "sg")
        vtm = [sb([128, 512], BF16, "vtm") for _ in range(2)]
        vtd = [sb([128, 64], BF16, "vtd") for _ in range(2)]
        wtm = [sb([128, 8], F32, "wtm") for _ in range(2)]
        ob_i = [0]

        def next_outb():
            ob_i[0] += 1
            return outb[ob_i[0] % 3]

        for b in range(NSEQ):
            make_AB(l, b, "mix_pre_g", 0, 1, Acol, Bcol, tmpc)
            for tb in range(NB):
                t0 = tb * 512
                hT = hTb[(b * NB + tb) % 2]
                for tt in range(4):
                    xt = xts[tt % 2]
                    P.dma(xt.t[:, :], x_src[b, t0 + tt * 128:t0 + (tt + 1) * 128, :],
                          r=[HB("x%d" % l, b)], w=[xt])
                    norm_transpose(xt, hT, tt, Acol, Bcol, xs, ss, junk, 7)
                P.dma(hTs[b][:, :, t0:t0 + 512], hT.t[:, :, :], r=[hT], w=[HB("hTs", b)])
                ck(1)
                P.dma(posi.t[:, :], pos_in.t[b:b + 1, t0:t0 + 512].partition_broadcast(128), w=[posi])
                P.op("dve", lambda e: e.tensor_copy(out=posf.t[:, :], in_=posi.t[:, :]), r=[posi], w=[posf])
                for dd in (32, 64):
                    P.op("dve", lambda e, dd=dd: e.tensor_scalar(out=ang.t[:, :], in0=posf.t[:, :], scalar1=invf[dd],
                                                               scalar2=None, op0=ALU.mult), r=[posf, cst], w=[ang])
                    P.op("dve", lambda e: e.tensor_scalar(out=kk.t[:, :], in0=ang.t[:, :], scalar1=1.0 / TWO_PI,
                                                          scalar2=MAGIC, op0=ALU.mult, op1=ALU.add), r=[ang], w=[kk])
                    P.op("dve", lambda e: e.tensor_scalar(out=kk.t[:, :], in0=kk.t[:, :], scalar1=MAGIC, scalar2=None,
                                                          op0=ALU.subtract), r=[kk], w=[kk])
                    P.op("dve", lambda e: e.scalar_tensor_tensor(out=rr.t[:, :], in0=kk.t[:, :], scalar=-C1,
                                                                 in1=ang.t[:, :], op0=ALU.mult, op1=ALU.add),
                         r=[kk, ang], w=[rr])
                    P.op("dve", lambda e: e.scalar_tensor_tensor(out=rr.t[:, :], in0=kk.t[:, :], scalar=-C2,
                                                                 in1=rr.t[:, :], op0=ALU.mult, op1=ALU.add),
                         r=[kk, rr], w=[rr])
                    P.op("dve", lambda e: e.tensor_scalar(out=rr.t[:, :], in0=rr.t[:, :], scalar1=PI_LO,
                                                          scalar2=-PI_LO, op0=ALU.min, op1=ALU.max), r=[rr], w=[rr])
                    act(sinT[dd].t[:, :], rr.t[:, :], AF.Sin, [rr, cst], [sinT[dd]], scale=sgn[dd])
                    P.op("dve", lambda e: e.tensor_scalar(out=r2.t[:, :], in0=rr.t[:, :], scalar1=math.pi / 2,
                                                          scalar2=None, op0=ALU.add), r=[rr], w=[r2])
                    P.op("dve", lambda e: e.tensor_scalar(out=mm_.t[:, :], in0=r2.t[:, :], scalar1=math.pi,
                                                          scalar2=-TWO_PI, op0=ALU.is_gt, op1=ALU.mult),
                         r=[r2], w=[mm_])
                    P.op("dve", lambda e: e.tensor_tensor(out=r2.t[:, :], in0=r2.t[:, :], in1=mm_.t[:, :],
                                                          op=ALU.add), r=[r2, mm_], w=[r2])
                    P.op("dve", lambda e: e.tensor_scalar(out=r2.t[:, :], in0=r2.t[:, :], scalar1=PI_LO,
                                                          scalar2=-PI_LO, op0=ALU.min, op1=ALU.max), r=[r2], w=[r2])
                    act(cosT[dd].t[:, :], r2.t[:, :], AF.Sin, [r2], [cosT[dd]])

                ck(2)
                bank = [0]

                def proj_fm(c0, m):
                    pb = ps[bank[0] % 4]
                    bank[0] += 1
                    for kc in range(8):
                        P.op("pe", lambda e, pb=pb, kc=kc: e.matmul(pb.t[0:m, :], lhsT=W1.t[:, kc, c0:c0 + m],
                                                                    rhs=hT.t[:, kc, :], start=(kc == 0),
                                                                    stop=(kc == 7)), r=[W1, hT], w=[pb])
                    return pb

                def store(dst_ap, srct, m, key):
                    P.dma(dst_ap, srct.t[0:m, :], r=[srct], w=[HB(key, b)])

                for cc in range(2):
                    pa = proj_fm(cc * 128, 128)
                    pg = proj_fm(256 + cc * 128, 128)
                    act(sg.t[:, :], pg.t[:, :], AF.Sigmoid, [pg], [sg])
                    ob = next_outb()
                    P.op("dve", lambda e, pa=pa, ob=ob: e.tensor_tensor(out=ob.t[:, :], in0=pa.t[:, :], in1=sg.t[:, :],
                                                                        op=ALU.mult), r=[pa, sg], w=[ob])
                    store(fm["gluT"][b][:, cc, t0:t0 + 512], ob, 128, "gluT")

                ck(3)

                def roped(c0, m, dd, dst_ap, key):
                    pq = proj_fm(c0, m)
                    q = qsb[bank[0] % 2]
                    act(q.t[0:m, :], pq.t[0:m, :], AF.Identity, [pq], [q])
                    ck(8)
                    pw = ps[4 + bank[0] % 2]
                    pm = perm32 if dd == 32 else perm64
                    P.op("pe", lambda e: e.matmul(pw.t[0:m, :], lhsT=pm[0:m, 0:m], rhs=q.t[0:m, :], start=True,
                                                  stop=True), r=[q, cbf], w=[pw])
                    ck(9)
                    P.op("dve", lambda e: e.tensor_tensor(out=t1.t[0:m, :], in0=q.t[0:m, :], in1=cosT[dd].t[0:m, :],
                                                          op=ALU.mult), r=[q, cosT[dd]], w=[t1])
                    P.op("dve", lambda e: e.tensor_tensor(out=t2.t[0:m, :], in0=pw.t[0:m, :], in1=sinT[dd].t[0:m, :],
                                                          op=ALU.mult), r=[pw, sinT[dd]], w=[t2])
                    ob = next_outb()
                    P.op("dve", lambda e: e.tensor_tensor(out=ob.t[0:m, :], in0=t1.t[0:m, :], in1=t2.t[0:m, :],
                                                          op=ALU.add), r=[t1, t2], w=[ob])
                    store(dst_ap, ob, m, key)

                def plain(c0, m, dst_ap, key):
                    pq = proj_fm(c0, m)
                    ob = next_outb()
                    act(ob.t[0:m, :], pq.t[0:m, :], AF.Identity, [pq], [ob])
                    store(dst_ap, ob, m, key)

                for cc in range(2):
                    roped(512 + cc * 128, 128, 32, fm["qbT"][b][:, cc, t0:t0 + 512], "qbT")
                    ck(5)
                    roped(768 + cc * 128, 128, 32, fm["kbT"][b][:, cc, t0:t0 + 512], "kbT")
                    ck(6)
                    plain(1280 + cc * 128, 128, fm["qcT"][b][:, cc, t0:t0 + 512], "qcT")
                    ck(7)
                    plain(1536 + cc * 128, 128, fm["kcT"][b][:, cc, t0:t0 + 512], "kcT")
                    roped(2048 + cc * 128, 128, 64, fm["qdT"][b][:, cc, t0:t0 + 512], "qdT")
                    roped(2432 + cc * 128, 128, 32, fm["qiT"][b][:, cc, t0:t0 + 512], "qiT")
                roped(2304, 64, 64, kdT[b][:, t0:t0 + 512], "kdT")
                roped(2688, 32, 32, kiT[b][:, t0:t0 + 512], "kiT")
                ck(4)
                for tt in range(4):
                    pv_ = ps[6]
                    for kc in range(8):
                        P.op("pe", lambda e, kc=kc, tt=tt: e.matmul(pv_.t[:, 0:256], lhsT=hT.t[:, kc, tt * 128:(tt + 1) * 128],
                                                                    rhs=W1.t[:, kc, 1024:1280], start=(kc == 0),
                                                                    stop=(kc == 7)), r=[W1, hT], w=[pv_])
                    for kc in range(8):
                        P.op("pe", lambda e, kc=kc, tt=tt: e.matmul(pv_.t[:, 256:512], lhsT=hT.t[:, kc, tt * 128:(tt + 1) * 128],
                                                                    rhs=W1.t[:, kc, 1792:2048], start=(kc == 0),
                                                                    stop=(kc == 7)), r=[W1, hT], w=[pv_])
                    v = vtm[tt % 2]
                    act(v.t[:, :], pv_.t[:, :], AF.Identity, [pv_], [v])
                    P.dma(vbs[b, t0 + tt * 128:t0 + (tt + 1) * 128, :], v.t[:, 0:256], r=[v], w=[HB("vbs", b)])
                    P.dma(vcs[b, t0 + tt * 128:t0 + (tt + 1) * 128, :], v.t[:, 256:512], r=[v], w=[HB("vcs", b)])
                    pv2 = ps[5]
                    for kc in range(8):
                        P.op("pe", lambda e, kc=kc, tt=tt: e.matmul(pv2.t[:, 0:64], lhsT=hT.t[:, kc, tt * 128:(tt + 1) * 128],
                                                                    rhs=W1.t[:, kc, 2368:2432], start=(kc == 0),
                                                                    stop=(kc == 7)), r=[W1, hT], w=[pv2])
                    for kc in range(8):
                        P.op("pe", lambda e, kc=kc, tt=tt: e.matmul(pv2.t[:, 64:72], lhsT=hT.t[:, kc, tt * 128:(tt + 1) * 128],
                                                                    rhs=W1.t[:, kc, 2720:2728], start=(kc == 0),
                                                                    stop=(kc == 7)), r=[W1, hT], w=[pv2])
                    vd_ = vtd[tt % 2]; wt_ = wtm[tt % 2]
                    act(vd_.t[:, :], pv2.t[:, 0:64], AF.Identity, [pv2], [vd_])
                    act(wt_.t[:, :], pv2.t[:, 64:72], AF.Identity, [pv2], [wt_])
                    P.dma(vds[b, t0 + tt * 128:t0 + (tt + 1) * 128, :], vd_.t[:, :], r=[vd_], w=[HB("vds", b)])
                    P.dma(wis[b, t0 + tt * 128:t0 + (tt + 1) * 128, :], wt_.t[:, :], r=[wt_], w=[HB("wis", b)])
        stage_reset()

        cw = sb([128, 2, 31], F32, "cw")
        for cc in range(2):
            P.dma(cw.t[:, cc, :], W["conv_a_w"].t[l][:, cc * 128:(cc + 1) * 128].rearrange("j p -> p j"), w=[cw],
                  allow_slow_non_contiguous=True)
        cb3 = sb([128, 6], F32, "cb3")
        for i, nm in enumerate(("conv_a_b", "conv_a_ln_g", "conv_a_ln_b")):
            P.dma(cb3.t[:, 2 * i:2 * i + 2], W[nm].t[l].rearrange("(c p) -> p c", p=128), w=[cb3],
                  allow_slow_non_contiguous=True)
        dg = sb([128, 62, 128], BF16, "dg")
        for cc in range(2):
            for j in range(31):
                P.op("dve", lambda e, cc=cc, j=j: e.tensor_scalar(out=dg.t[:, cc * 31 + j, :], in0=cst.t[:, 0:128],
                                                                 scalar1=cw.t[:, cc, j:j + 1], scalar2=None,
                                                                 op0=ALU.mult), r=[cst, cw], w=[dg])
        o256 = sb([128, 128], F32, "o256")
        P.op("pool", lambda e: e.memset(o256.t[:, :], 1.0 / 256.0), w=[o256])
        gl = sb([128, 2, S + 32], BF16, "gl")
        cv = [sb([128, 2, 512], F32, "cv") for _ in range(2)]
        sq = sb([128, 2, 512], F32, "sq")
        mean_sb = sb([128, 512], F32, "mean_sb")
        m2 = sb([128, 512], F32, "m2"); var = sb([128, 512], F32, "var"); xc = sb([128, 512], F32, "xc")
        oa = [sb([128, 512], BF16, "oa") for _ in range(2)]
        for b in range(NSEQ):
            P.op("pool", lambda e: e.memset(gl.t[:, :, 0:32], 0.0), w=[gl])
            P.dma(gl.t[:, :, 32:32 + S], fm["gluT"][b][:, :, :], r=[HB("gluT", b)], w=[gl])
            for tb in range(NB):
                t0 = tb * 512
                cvt = cv[tb % 2]
                for cc in range(2):
                    pb = ps[cc]
                    for j in range(31):
                        P.op("pe", lambda e, cc=cc, j=j, pb=pb: e.matmul(
                            pb.t[:, :], lhsT=dg.t[:, cc * 31 + j, :], rhs=gl.t[:, cc, t0 + 2 + j:t0 + 2 + j + 512],
                            start=(j == 0), stop=(j == 30)), r=[dg, gl], w=[pb])
                    act(cvt.t[:, cc, :], pb.t[:, :], AF.Identity, [pb, cb3], [cvt], bias=cb3.t[:, cc:cc + 1])
                    act(sq.t[:, cc, :], cvt.t[:, cc, :], AF.Square, [cvt], [sq])
                pm_ = ps[2]; pe2 = ps[3]
                for cc in range(2):
                    P.op("pe", lambda e, cc=cc: e.matmul(pm_.t[:, :], lhsT=o256.t[:, :], rhs=cvt.t[:, cc, :],
                                                         start=(cc == 0), stop=(cc == 1)), r=[o256, cvt], w=[pm_])
                for cc in range(2):
                    P.op("pe", lambda e, cc=cc: e.matmul(pe2.t[:, :], lhsT=o256.t[:, :], rhs=sq.t[:, cc, :],
                                                         start=(cc == 0), stop=(cc == 1)), r=[o256, sq], w=[pe2])
                act(mean_sb.t[:, :], pm_.t[:, :], AF.Identity, [pm_], [mean_sb])
                act(m2.t[:, :], mean_sb.t[:, :], AF.Square, [mean_sb], [m2])
                P.op("dve", lambda e: e.tensor_tensor(out=var.t[:, :], in0=pe2.t[:, :], in1=m2.t[:, :], op=ALU.subtract),
                     r=[pe2, m2], w=[var])
                act(var.t[:, :], var.t[:, :], AF.Ln, [var], [var], bias=eps_c)
                act(var.t[:, :], var.t[:, :], AF.Exp, [var], [var], scale=-0.5)
                for cc in range(2):
                    P.op("dve", lambda e, cc=cc: e.tensor_tensor(out=xc.t[:, :], in0=cvt.t[:, cc, :], in1=mean_sb.t[:, :],
                                                                 op=ALU.subtract), r=[cvt, mean_sb], w=[xc])
                    P.op("dve", lambda e: e.tensor_tensor(out=xc.t[:, :], in0=xc.t[:, :], in1=var.t[:, :], op=ALU.mult),
                         r=[xc, var], w=[xc])
                    o_ = oa[cc]
                    act(o_.t[:, :], xc.t[:, :], AF.Silu, [xc, cb3], [o_], scale=cb3.t[:, 2 + cc:3 + cc],
                        bias=cb3.t[:, 4 + cc:5 + cc])
                    P.dma(fm["oaT"][b][:, cc, t0:t0 + 512], o_.t[:, :], r=[o_], w=[HB("oaT", b)])
        stage_reset()

        lamt = sb([128, 8], F32, "lamt")
        lq = sb([128, 4, 32], F32, "lq")
        for i, nm in enumerate(("lam_q1", "lam_k1", "lam_q2", "lam_k2")):
            P.dma(lq.t[:, i, :], W[nm].t[l:l + 1, :].partition_broadcast(128), w=[lq])
        lj = sb([128, 32], F32, "lj")
        P.op("pool", lambda e: e.memset(lamt.t[:, :], 0.0), w=[lamt])
        for i in range(2):
            P.op("dve", lambda e, i=i: e.tensor_tensor(out=lj.t[:, :], in0=lq.t[:, 2 * i, :], in1=lq.t[:, 2 * i + 1, :],
                                                       op=ALU.mult), r=[lq], w=[lj])
            P.op("dve", lambda e, i=i: e.tensor_reduce(out=lamt.t[:, i:i + 1], in_=lj.t[:, :], axis=AX.X, op=ALU.add),
                 r=[lj, lamt], w=[lamt])
            act(lamt.t[:, i:i + 1], lamt.t[:, i:i + 1], AF.Exp, [lamt], [lamt])
        P.op("dve", lambda e: e.tensor_tensor(out=lamt.t[:, 2:3], in0=lamt.t[:, 1:2], in1=lamt.t[:, 0:1],
                                              op=ALU.subtract), r=[lamt], w=[lamt])
        P.op("dve", lambda e: e.tensor_scalar(out=lamt.t[:, 2:3], in0=lamt.t[:, 2:3], scalar1=-lam_init, scalar2=None,
                                              op0=ALU.add), r=[lamt], w=[lamt])
        neglam = lamt.t[:, 2:3]
        gsub = sb([128, 1], F32, "gsub")
        for hh in range(2):
            P.dma(gsub.t[hh * 64:(hh + 1) * 64, :], W["diff_subln_g"].t[l].rearrange("(d o) -> d o", o=1), w=[gsub],
                  allow_slow_non_contiguous=True)
        P.op("dve", lambda e: e.tensor_scalar(out=gsub.t[:, :], in0=gsub.t[:, :], scalar1=1.0 - lam_init, scalar2=None,
                                              op0=ALU.mult), r=[gsub], w=[gsub])
        o64 = sb([128, 64], F32, "o64")
        P.op("pool", lambda e: e.memset(o64.t[:, :], 1.0 / 64.0), w=[o64])
        qT = sb([128, 2, S], BF16, "qT"); kT = sb([128, 2, S], BF16, "kT"); vv = sb([128, T, 256], BF16, "vv")
        pt = [sb([128, 512], BF16, "pt") for _ in range(3)]
        rc = [sb([64, 512], F32, "rc") for _ in range(2)]
        tO = [sb([64, 512], F32, "tO") for _ in range(2)]
        od = sb([64, 512], F32, "od"); osq = sb([64, 512], F32, "osq"); rs = sb([64, 512], F32, "rs")
        obo = [sb([64, 512], BF16, "obo") for _ in range(2)]
        pti = [0]
        SC_B = 32 ** -0.5
        for b in range(NSEQ):
            P.dma(qT.t[:, :, :], fm["qbT"][b][:, :, :], r=[HB("qbT", b)], w=[qT])
            P.dma(kT.t[:, :, :], fm["kbT"][b][:, :, :], r=[HB("kbT", b)], w=[kT])
            P.dma(vv.t[:, :, :], vbs[b].rearrange("(t p) f -> p t f", p=128), r=[HB("vbs", b)], w=[vv])
            for h in range(4):
                cc = h // 2
                for qb in range(NB):
                    q0 = qb * 512
                    nkt = qb * 4 + 4
                    def phS(kt, m):
                        i = kt - qb * 4
                        cs = max(i, 0) * 128
                        j = (h % 2) * 2 + m
                        kw = {"tile_position": (96, 0)} if j == 3 else {}
                        pS = ps[(kt * 2 + m) % 3]
                        P.op("pe", lambda e: e.matmul(
                            pS.t[:, cs:512], lhsT=kT.t[32 * j:32 * j + 32, cc, kt * 128:(kt + 1) * 128],
                            rhs=qT.t[32 * j:32 * j + 32, cc, q0 + cs:q0 + 512], start=True, stop=True, **kw),
                            r=[kT, qT], w=[pS])
                        pti[0] += 1
                        p_ = pt[pti[0] % 3]
                        act(p_.t[:, cs:512], pS.t[:, cs:512], AF.Exp, [pS], [p_], scale=SC_B)
                        if i >= 0:
                            P.op("dve", lambda e: e.tensor_tensor(
                                out=p_.t[:, cs:cs + 128], in0=p_.t[:, cs:cs + 128], in1=le_bf, op=ALU.mult),
                                r=[p_, cbf], w=[p_])
                        return (kt, m, cs, p_)

                    def phV(c):
                        kt, m, cs, p_ = c
                        pO = ps[3 + m]; pSm = ps[5 + m]
                        P.op("pe", lambda e: e.matmul(
                            pO.t[0:64, cs:512], lhsT=vv.t[:, kt, h * 64:(h + 1) * 64], rhs=p_.t[:, cs:512],
                            start=(kt == 0), stop=(kt == nkt - 1)), r=[vv, p_], w=[pO])
                        P.op("pe", lambda e: e.matmul(
                            pSm.t[0:64, cs:512], lhsT=ones_bf.t[:, 0:64], rhs=p_.t[:, cs:512],
                            start=(kt == 0), stop=(kt == nkt - 1)), r=[ones_bf, p_], w=[pSm])

                    items = [(kt, m) for kt in range(nkt) for m in range(2)]
                    ctxs = {0: phS(*items[0]), 1: phS(*items[1])}
                    for idx in range(len(items)):
                        phV(ctxs.pop(idx))
                        if idx + 2 < len(items):
                            ctxs[idx + 2] = phS(*items[idx + 2])
                    for m in range(2):
                        P.op("dve", lambda e, m=m: e.reciprocal(out=rc[m].t[:, :], in_=ps[5 + m].t[0:64, :]),
                             r=[ps[5 + m]], w=[rc[m]])
                        P.op("dve", lambda e, m=m: e.tensor_tensor(out=tO[m].t[:, :], in0=ps[3 + m].t[0:64, :],
                                                                   in1=rc[m].t[:, :], op=ALU.mult),
                             r=[ps[3 + m], rc[m]], w=[tO[m]])
                    P.op("dve", lambda e: e.scalar_tensor_tensor(out=od.t[:, :], in0=tO[1].t[:, :], scalar=neglam[0:64, :],
                                                                 in1=tO[0].t[:, :], op0=ALU.mult, op1=ALU.add),
                         r=[tO[0], tO[1], lamt], w=[od])
                    act(osq.t[:, :], od.t[:, :], AF.Square, [od], [osq])
                    pst_ = ps[7]
                    P.op("pe", lambda e: e.matmul(pst_.t[0:64, :], lhsT=o64.t[0:64, :], rhs=osq.t[:, :], start=True,
                                                  stop=True), r=[o64, osq], w=[pst_])
                    act(rs.t[:, :], pst_.t[0:64, :], AF.Ln, [pst_], [rs], bias=eps_c[0:64, :])
                    act(rs.t[:, :], rs.t[:, :], AF.Exp, [rs], [rs], scale=-0.5)
                    o_ = obo[(h * NB + qb) % 2]
                    hb0 = (h % 2) * 64
                    P.op("dve", lambda e, o_=o_, hb0=hb0: e.scalar_tensor_tensor(
                        out=o_.t[:, :], in0=od.t[:, :], scalar=gsub.t[0:64, :], in1=rs.t[:, :],
                        op0=ALU.mult, op1=ALU.mult), r=[od, gsub, rs], w=[o_])
                    P.dma(fm["obT"][b][hb0:hb0 + 64, cc, q0:q0 + 512], o_.t[:, :], r=[o_], w=[HB("obT", b)])
        stage_reset()

        qT = sb([128, 2, S], BF16, "qT"); kT = sb([128, 2, S], BF16, "kT"); vv = sb([128, T, 256], BF16, "vv")
        ee = [sb([128, 512], F32, "ee") for _ in range(4)]
        spb = [sb([128, 512], BF16, "spb") for _ in range(4)]
        lsum2 = [sb([128, 512], BF16, "lsum") for _ in range(2)]
        a1 = [sb([128, 512], F32, "a1") for _ in range(4)]
        wT = [sb([128, 512], BF16, "wT") for _ in range(4)]
        oco = [sb([64, 512], BF16, "oco") for _ in range(2)]
        SC_C = 64 ** -0.5
        it = [0]
        for b in range(NSEQ):
            P.dma(qT.t[:, :, :], fm["qcT"][b][:, :, :], r=[HB("qcT", b)], w=[qT])
            P.dma(kT.t[:, :, :], fm["kcT"][b][:, :, :], r=[HB("kcT", b)], w=[kT])
            P.dma(vv.t[:, :, :], vcs[b].rearrange("(t p) f -> p t f", p=128), r=[HB("vcs", b)], w=[vv])
            for hp in (0, 2):
                for qb in range(NB):
                    q0 = qb * 512
                    top = qb * 4 + 3
                    lo_kt = 0 if cwin is None else max(0, top - 3 - cwin)
                    for hh in range(2):
                        P.op("pool", lambda e, hh=hh: e.memset(lsum2[hh].t[:, :], 0.0), w=[lsum2[hh]])

                    def phA(hh, kt):
                        h = hp + hh
                        cc = h // 2
                        hb0 = (h % 2) * 64
                        i = kt - qb * 4
                        cs = max(i, 0) * 128
                        it[0] += 1
                        n = it[0]
                        c = dict(h=h, i=i, cs=cs, kt=kt, pZ=ps[n % 3], pL=ps[3 + n % 3], e_=ee[n % 4], s_=spb[n % 4],
                                 a_=a1[n % 4], w_=wT[n % 4], pO=ps[6 + hh], lsum=lsum2[hh], first=(kt == top))
                        pZ, e_, s_ = c["pZ"], c["e_"], c["s_"]
                        P.op("pe", lambda e: e.matmul(
                            pZ.t[:, cs:512], lhsT=kT.t[hb0:hb0 + 64, cc, kt * 128:(kt + 1) * 128],
                            rhs=qT.t[hb0:hb0 + 64, cc, q0 + cs:q0 + 512], start=True, stop=True), r=[kT, qT], w=[pZ])
                        act(e_.t[:, cs:512], pZ.t[:, cs:512], AF.Exp, [pZ], [e_], scale=SC_C)
                        act(s_.t[:, cs:512], e_.t[:, cs:512], AF.Ln, [e_], [s_], bias=one_c)
                        if i >= 0:
                            P.op("dve", lambda e: e.tensor_tensor(
                                out=s_.t[:, cs:cs + 128], in0=s_.t[:, cs:cs + 128], in1=lt_bf, op=ALU.mult),
                                r=[s_, cbf], w=[s_])
                        return c

                    def phB(c):
                        pZ, pL, s_, a_, w_, lsum, cs, first, i, kt = (c[k] for k in
                                                                     ("pZ", "pL", "s_", "a_", "w_", "lsum", "cs", "first", "i", "kt"))
                        P.op("pe", lambda e: e.matmul(pL.t[:, cs:512], lhsT=gt_bf, rhs=s_.t[:, cs:512], start=True,
                                                      stop=first), r=[cbf, s_], w=[pL])
                        if not first:
                            P.op("pe", lambda e: e.matmul(pL.t[:, cs:512], lhsT=ones_bf.t[:, :], rhs=lsum.t[:, cs:512],
                                                          start=False, stop=True), r=[ones_bf, lsum], w=[pL])
                        if kt > lo_kt:
                            P.op("dve", lambda e: e.tensor_tensor(
                                out=lsum.t[:, cs:512], in0=lsum.t[:, cs:512], in1=s_.t[:, cs:512], op=ALU.add),
                                r=[lsum, s_], w=[lsum])
                        P.op("dve", lambda e: e.scalar_tensor_tensor(
                            out=a_.t[:, cs:512], in0=pZ.t[:, cs:512], scalar=SC_C, in1=s_.t[:, cs:512],
                            op0=ALU.mult, op1=ALU.subtract), r=[pZ, s_], w=[a_])
                        P.op("dve", lambda e: e.tensor_tensor(
                            out=a_.t[:, cs:512], in0=a_.t[:, cs:512], in1=pL.t[:, cs:512], op=ALU.subtract),
                            r=[a_, pL], w=[a_])
                        act(w_.t[:, cs:512], a_.t[:, cs:512], AF.Exp, [a_], [w_])
                        if i >= 0:
                            P.op("dve", lambda e: e.tensor_tensor(
                                out=w_.t[:, cs:cs + 128], in0=w_.t[:, cs:cs + 128], in1=lt_bf, op=ALU.mult),
                                r=[w_, cbf], w=[w_])

                    def phC(c):
                        pO, w_, cs, first, kt, h = (c[k] for k in ("pO", "w_", "cs", "first", "kt", "h"))
                        P.op("pe", lambda e: e.matmul(
                            pO.t[0:64, cs:512], lhsT=vv.t[:, kt, h * 64:(h + 1) * 64], rhs=w_.t[:, cs:512],
                            start=first, stop=(kt == lo_kt)), r=[vv, w_], w=[pO])

                    kts = list(range(top, lo_kt - 1, -1))
                    ctxA = [phA(hh, kts[0]) for hh in range(2)]
                    for ki, kt in enumerate(kts):
                        cur = ctxA
                        phB(cur[0])
                        if ki + 1 < len(kts):
                            ctxA = [phA(0, kts[ki + 1])]
                        phB(cur[1])
                        if ki + 1 < len(kts):
                            ctxA.append(phA(1, kts[ki + 1]))
                        phC(cur[0])
                        phC(cur[1])
                    for hh in range(2):
                        h = hp + hh
                        o_ = oco[hh]
                        act(o_.t[:, :], ps[6 + hh].t[0:64, :], AF.Identity, [ps[6 + hh]], [o_])
                        P.dma(fm["ocT"][b][(h % 2) * 64:(h % 2) * 64 + 64, h // 2, q0:q0 + 512], o_.t[:, :], r=[o_],
                              w=[HB("ocT", b)])
        stage_reset()

        qiT = sb([128, 2, S], BF16, "qiT"); ki4 = sb([128, S], BF16, "ki4")
        wi_ = sb([128, T, 8], F32, "wi")
        qdT = sb([128, 2, S], BF16, "qdT"); kd2 = sb([128, S], BF16, "kd2"); vd = sb([128, T, 64], BF16, "vd")
        sc = [sb([128, S], F32, "sc") for _ in range(2)]
        rl = [sb([128, 512], F32, "rl") for _ in range(3)]
        st = [sb([128, 8], F32, "st") for _ in range(2)]
        steps = [sb([128, NIT + 2], F32, "steps") for _ in range(2)]
        cnt = [sb([128, NIT + 2], F32, "cnt") for _ in range(2)]
        g2s = [sb([128, 1], F32, "g2") for _ in range(2)]
        jks = [sb([128, S], BF16, "jk") for _ in range(2)]
        mb = [sb([128, S], BF16, "mb") for _ in range(2)]
        MT = [sb([128, T, 128], BF16, "MT") for _ in range(2)]
        pd = [sb([128, 4, 128], BF16, "pd") for _ in range(3)]
        rcd = sb([64, 512], F32, "rcd")
        odo = [sb([64, 4, 128], BF16, "odo") for _ in range(2)]
        SC_D = 64 ** -0.5
        n_ = [0]
        for b in range(NSEQ):
            P.dma(qiT.t[:, :, :], fm["qiT"][b][:, :, :], r=[HB("qiT", b)], w=[qiT])
            for g in range(4):
                P.dma(ki4.t[32 * g:32 * g + 32, :], kiT[b][:, :], r=[HB("kiT", b)], w=[ki4])
            P.dma(wi_.t[:, :, :], wis[b].rearrange("(t p) f -> p t f", p=128), r=[HB("wis", b)], w=[wi_])
            P.dma(qdT.t[:, :, :], fm["qdT"][b][:, :, :], r=[HB("qdT", b)], w=[qdT])
            for g in range(2):
                P.dma(kd2.t[64 * g:64 * g + 64, :], kdT[b][:, :], r=[HB("kdT", b)], w=[kd2])
            P.dma(vd.t[:, :, :], vds[b].rearrange("(t p) f -> p t f", p=128), r=[HB("vds", b)], w=[vd])
            def sel_(qt):
                return ((qt + 1) * 128, sc[qt % 2], st[qt % 2], steps[qt % 2], cnt[qt % 2], mb[qt % 2], MT[qt % 2],
                        g2s[qt % 2], jks[qt % 2])

            def phase1(qt):
                kl, s_, st_, stp, cn, m_, mt_, g2, jk = sel_(qt)
                for kb in range((kl + 511) // 512):
                    k0 = kb * 512
                    nk = min(512, kl - k0)
                    for hi in range(8):
                        n_[0] += 1
                        pI = ps[n_[0] % 3]
                        r_ = rl[n_[0] % 3]
                        g = hi % 4
                        kw = {"tile_position": (96, 0)} if g == 3 else {}
                        P.op("pe", lambda e, pI=pI, g=g, hi=hi, kw=kw, k0=k0, nk=nk: e.matmul(
                            pI.t[:, 0:nk], lhsT=qiT.t[32 * g:32 * g + 32, hi // 4, qt * 128:(qt + 1) * 128],
                            rhs=ki4.t[32 * g:32 * g + 32, k0:k0 + nk], start=True, stop=True, **kw),
                            r=[qiT, ki4], w=[pI])
                        act(r_.t[:, 0:nk], pI.t[:, 0:nk], AF.Relu, [pI], [r_])
                        if hi == 0:
                            P.op("dve", lambda e, r_=r_, k0=k0, nk=nk: e.tensor_scalar(
                                out=s_.t[:, k0:k0 + nk], in0=r_.t[:, 0:nk], scalar1=wi_.t[:, qt, 0:1], scalar2=None,
                                op0=ALU.mult), r=[r_, wi_], w=[s_])
                        else:
                            P.op("dve", lambda e, r_=r_, k0=k0, nk=nk, hi=hi: e.scalar_tensor_tensor(
                                out=s_.t[:, k0:k0 + nk], in0=r_.t[:, 0:nk], scalar=wi_.t[:, qt, hi:hi + 1],
                                in1=s_.t[:, k0:k0 + nk], op0=ALU.mult, op1=ALU.add), r=[r_, wi_, s_], w=[s_])
                ck(20)
                P.op("dve", lambda e: e.tensor_reduce(out=st_.t[:, 0:1], in_=s_.t[:, 0:kl], axis=AX.X, op=ALU.min),
                     r=[s_], w=[st_])
                P.op("dve", lambda e: e.tensor_tensor(out=s_.t[:, kl - 128:kl], in0=s_.t[:, kl - 128:kl], in1=negm,
                                                      op=ALU.add), r=[s_, cst], w=[s_])
                P.op("dve", lambda e: e.tensor_reduce(out=st_.t[:, 1:2], in_=s_.t[:, 0:kl], axis=AX.X, op=ALU.max),
                     r=[s_, st_], w=[st_])
                P.op("dve", lambda e: e.tensor_tensor(out=st_.t[:, 2:3], in0=st_.t[:, 1:2], in1=st_.t[:, 0:1],
                                                      op=ALU.subtract), r=[st_], w=[st_])
                P.op("dve", lambda e: e.tensor_scalar(out=st_.t[:, 2:3], in0=st_.t[:, 2:3], scalar1=1.02, scalar2=0.002,
                                                      op0=ALU.mult, op1=ALU.add), r=[st_], w=[st_])
                P.op("dve", lambda e: e.scalar_tensor_tensor(out=st_.t[:, 3:4], in0=st_.t[:, 2:3], scalar=-0.5,
                                                             in1=st_.t[:, 1:2], op0=ALU.mult, op1=ALU.add),
                     r=[st_], w=[st_])
                P.op("dve", lambda e: e.tensor_scalar(out=stp.t[:, :], in0=pow2, scalar1=st_.t[:, 2:3], scalar2=None,
                                                      op0=ALU.mult), r=[st_, cst], w=[stp])
                ck(21)
                P.op("pool", lambda e: e.memset(cn.t[:, :], 0.0), w=[cn])

            def phase2(qt, itn):
                kl, s_, st_, stp, cn, m_, mt_, g2, jk = sel_(qt)
                P.op("dve", lambda e, itn=itn: e.tensor_scalar(
                    out=jk.t[:, 0:kl], in0=s_.t[:, 0:kl], scalar1=st_.t[:, 3:4], scalar2=zero_c, op0=ALU.is_ge,
                    op1=ALU.add, accum_out=cn.t[:, itn:itn + 1]), r=[s_, st_, cn], w=[jk, cn])
                P.op("dve", lambda e, itn=itn: e.tensor_scalar(
                    out=g2.t[:, :], in0=cn.t[:, itn:itn + 1], scalar1=topk_c, scalar2=stp.t[:, itn:itn + 1],
                    op0=ALU.is_ge, op1=ALU.mult), r=[cn, stp, cst], w=[g2])
                P.op("dve", lambda e, itn=itn: e.scalar_tensor_tensor(
                    out=st_.t[:, 3:4], in0=st_.t[:, 3:4], scalar=stp.t[:, itn + 1:itn + 2], in1=g2.t[:, :],
                    op0=ALU.subtract, op1=ALU.add), r=[st_, stp, g2], w=[st_])

            def phase3(qt):
                kl, s_, st_, stp, cn, m_, mt_, g2, jk = sel_(qt)
                P.op("dve", lambda e: e.tensor_tensor(out=st_.t[:, 3:4], in0=st_.t[:, 3:4], in1=stp.t[:, NIT:NIT + 1],
                                                      op=ALU.subtract), r=[st_, stp], w=[st_])
                P.op("dve", lambda e: e.tensor_scalar(out=m_.t[:, 0:kl], in0=s_.t[:, 0:kl], scalar1=st_.t[:, 3:4],
                                                      scalar2=None, op0=ALU.is_ge), r=[s_, st_], w=[m_])
                ck(22)
                for k8 in range((qt + 8) // 8):
                    nt_ = min(8, qt + 1 - k8 * 8)
                    pT = ps[3 + (n_[0] + k8) % 2]
                    pTv = pT.t[:, :].bitcast(BF16)
                    for u in range(nt_):
                        kt = k8 * 8 + u
                        P.op("pe", lambda e, pTv=pTv, u=u, kt=kt: e.transpose(
                            pTv[:, u * 128:(u + 1) * 128], m_.t[:, kt * 128:(kt + 1) * 128], ident_bf),
                            r=[m_, cbf], w=[pT])
                    act(mt_.t[:, k8 * 8:k8 * 8 + nt_, :], pTv[:, 0:nt_ * 128].rearrange("p (u q) -> p u q", q=128),
                        AF.Identity, [pT], [mt_])
                ck(23)
                pO = ps[5]; pSm = ps[6]
                for kt in range(qt + 1):
                    n_[0] += 1
                    pAB = (ps[0], ps[1]) if n_[0] % 2 == 0 else (ps[2], ps[7])
                    p_ = pd[n_[0] % 3]
                    for h in range(4):
                        hb0 = (h % 2) * 64
                        pS = pAB[h % 2]
                        P.op("pe", lambda e, pS=pS, h=h, hb0=hb0, kt=kt: e.matmul(
                            pS.t[:, (h // 2) * 128:(h // 2 + 1) * 128], lhsT=kd2.t[hb0:hb0 + 64, kt * 128:(kt + 1) * 128],
                            rhs=qdT.t[hb0:hb0 + 64, h // 2, qt * 128:(qt + 1) * 128], start=True, stop=True),
                            r=[kd2, qdT], w=[pS])
                    for g in range(2):
                        act(p_.t[:, 2 * g:2 * g + 2, :], pAB[g].t[:, 0:256].rearrange("p (h q) -> p h q", q=128), AF.Exp,
                            [pAB[g]], [p_], scale=SC_D)
                    for hp in range(4):
                        P.op("dve", lambda e, p_=p_, kt=kt, hp=hp: e.tensor_tensor(
                            out=p_.t[:, hp, :], in0=p_.t[:, hp, :], in1=mt_.t[:, kt, :], op=ALU.mult),
                            r=[p_, mt_], w=[p_])
                    pr = p_.t[:, :, :].rearrange("p h q -> p (h q)")
                    P.op("pe", lambda e, pr=pr, kt=kt: e.matmul(pO.t[0:64, :], lhsT=vd.t[:, kt, :], rhs=pr,
                                                                start=(kt == 0), stop=(kt == qt)), r=[vd, p_], w=[pO])
                    P.op("pe", lambda e, pr=pr, kt=kt: e.matmul(pSm.t[0:64, :], lhsT=ones_bf.t[:, 0:64], rhs=pr,
                                                                start=(kt == 0), stop=(kt == qt)), r=[ones_bf, p_], w=[pSm])
                P.op("dve", lambda e: e.reciprocal(out=rcd.t[:, :], in_=pSm.t[0:64, :]), r=[pSm], w=[rcd])
                o_ = odo[qt % 2]
                P.op("dve", lambda e, o_=o_: e.tensor_tensor(out=o_.t[:, :, :].rearrange("p h q -> p (h q)"),
                                                             in0=pO.t[0:64, :], in1=rcd.t[:, :], op=ALU.mult),
                     r=[pO, rcd], w=[o_])
                for hp in range(4):
                    h = (hp % 2) * 2 + hp // 2
                    hb0 = (h % 2) * 64
                    P.dma(fm["odT"][b][hb0:hb0 + 64, h // 2, qt * 128:(qt + 1) * 128], o_.t[:, hp, :], r=[o_],
                          w=[HB("odT", b)])

            for qp in range(0, T, 2):
                for qt in (qp, qp + 1):
                    phase1(qt)
                for itn in range(NIT):
                    for qt in (qp, qp + 1):
                        phase2(qt, itn)
                for qt in (qp, qp + 1):
                    phase3(qt)
        stage_reset()

        Wg = sb([128, 8, 4096], BF16, "Wg"); Wbr = sb([128, 8, D], BF16, "Wbr"); Wo = sb([128, 8, D], BF16, "Wo")
        stg = [sb([128, 8, 256], F32, "stg") for _ in range(2)]
        wv = W["w_in"].t[l].rearrange("(kc p) n -> p kc n", p=128)
        load_cast(Wg, lambda c0, n: Wg.t[:, :, c0:c0 + n], lambda c0, n: wv[:, :, 2728 + c0:2728 + c0 + n],
                  [((c0, 256), (c0, 256)) for c0 in range(0, 4096, 256)], stg, ["pool", "dve"])
        wbv = W["w_branch"].t[l].rearrange("i (kc p) n -> p (i kc) n", p=128)
        load_cast(Wbr, lambda c0, n: Wbr.t[:, :, c0:c0 + n], lambda c0, n: wbv[:, :, c0:c0 + n],
                  [((c0, 256), (c0, 256)) for c0 in range(0, D, 256)], stg, ["pool", "dve"])
        wov = W["w_out"].t[l].rearrange("(kc p) n -> p kc n", p=128)
        load_cast(Wo, lambda c0, n: Wo.t[:, :, c0:c0 + n], lambda c0, n: wov[:, :, c0:c0 + n],
                  [((c0, 256), (c0, 256)) for c0 in range(0, D, 256)], stg, ["pool", "dve"])
        GG = sb([128, D], F32, "GG"); tmpb = sb([128, D], F32, "tmpb")
        hTb = [sb([128, 8, 512], BF16, "hT") for _ in range(2)]
        oTb = [[sb([128, 2, 512], BF16, "oT") for _ in range(4)] for _ in range(2)]
        sgm = [sb([128, 512], F32, "sgm") for _ in range(2)]
        acc = sb([128, 512], F32, "acc"); tm = sb([128, 512], F32, "tm")
        mT = sb([128, 8, 512], BF16, "mT")
        ysb = [sb([128, D], F32, "ysb") for _ in range(2)]
        xts = [sb([128, D], F32, "xt") for _ in range(2)]
        junk = sb([128, D], BF16, "junk"); ss = sb([128, 2], F32, "ss")
        onames = ("oaT", "obT", "ocT", "odT")
        n3 = [0]
        for b in range(NSEQ):
            make_GG(l, b, "mix_post_g", 2, GG, tmpb)
            for tb in range(NB):
                t0 = tb * 512
                hT = hTb[tb % 2]; oT = oTb[tb % 2]
                P.dma(hT.t[:, :, :], hTs[b][:, :, t0:t0 + 512], r=[HB("hTs", b)], w=[hT])
                for i in range(4):
                    P.dma(oT[i].t[:, :, :], fm[onames[i]][b][:, :, t0:t0 + 512], r=[HB(onames[i], b)], w=[oT[i]])
                for oc in range(8):
                    for i in range(4):
                        n3[0] += 1
                        pG = ps[n3[0] % 2]; pB = ps[2 + n3[0] % 2]; sg_ = sgm[n3[0] % 2]
                        for kc in range(8):
                            P.op("pe", lambda e, pG=pG, kc=kc, i=i, oc=oc: e.matmul(
                                pG.t[:, :], lhsT=Wg.t[:, kc, i * D + oc * 128:i * D + (oc + 1) * 128], rhs=hT.t[:, kc, :],
                                start=(kc == 0), stop=(kc == 7)), r=[Wg, hT], w=[pG])
                        for kc in range(2):
                            P.op("pe", lambda e, pB=pB, kc=kc, i=i, oc=oc: e.matmul(
                                pB.t[:, :], lhsT=Wbr.t[:, i * 2 + kc, oc * 128:(oc + 1) * 128], rhs=oT[i].t[:, kc, :],
                                start=(kc == 0), stop=(kc == 1)), r=[Wbr, oT[i]], w=[pB])
                        act(sg_.t[:, :], pG.t[:, :], AF.Sigmoid, [pG], [sg_])
                        if i == 0:
                            P.op("dve", lambda e, sg_=sg_, pB=pB: e.tensor_tensor(out=acc.t[:, :], in0=sg_.t[:, :],
                                                                                  in1=pB.t[:, :], op=ALU.mult),
                                 r=[sg_, pB], w=[acc])
                        else:
                            P.op("dve", lambda e, sg_=sg_, pB=pB: e.tensor_tensor(out=tm.t[:, :], in0=sg_.t[:, :],
                                                                                  in1=pB.t[:, :], op=ALU.mult),
                                 r=[sg_, pB], w=[tm])
                            if i < 3:
                                P.op("dve", lambda e: e.tensor_tensor(out=acc.t[:, :], in0=acc.t[:, :], in1=tm.t[:, :],
                                                                       op=ALU.add), r=[acc, tm], w=[acc])
                            else:
                                P.op("dve", lambda e, oc=oc: e.tensor_tensor(out=mT.t[:, oc, :], in0=acc.t[:, :],
                                                                              in1=tm.t[:, :], op=ALU.add),
                                     r=[acc, tm], w=[mT])
                for tt in range(4):
                    y_ = ysb[tt % 2]; xt = xts[tt % 2]
                    P.dma(xt.t[:, :], x_src[b, t0 + tt * 128:t0 + (tt + 1) * 128, :], r=[HB("x%d" % l, b)], w=[xt])
                    for hf in range(2):
                        pY = ps[4 + hf]
                        for kc in range(8):
                            P.op("pe", lambda e, pY=pY, kc=kc, hf=hf, tt=tt: e.matmul(
                                pY.t[:, :], lhsT=mT.t[:, kc, tt * 128:(tt + 1) * 128], rhs=Wo.t[:, kc, hf * 512:(hf + 1) * 512],
                                start=(kc == 0), stop=(kc == 7)), r=[mT, Wo], w=[pY])
                        act(y_.t[:, hf * 512:(hf + 1) * 512], pY.t[:, :], AF.Identity, [pY], [y_])
                    post_norm_residual(y_, xt, GG, ss, junk, xmid[b, t0 + tt * 128:t0 + (tt + 1) * 128, :],
                                       HB("xmid%d" % l, b))
        stage_reset()

        Wu = sb([128, 8, 2 * DFF], BF16, "Wu")
        stg = [sb([128, 8, 256], F32, "stg") for _ in range(2)]
        wv = W["w_up"].t[l].rearrange("(kc p) n -> p kc n", p=128)
        load_cast(Wu, lambda c0, n: Wu.t[:, :, c0:c0 + n], lambda c0, n: wv[:, :, c0:c0 + n],
                  [((c0, 256), (c0, 256)) for c0 in range(0, 2 * DFF, 256)], stg, ["pool", "dve"])
        fcw = sb([128, 44, 3], F32, "fcw"); fcb = sb([128, 44], F32, "fcb")
        for j in range(3):
            P.dma(fcw.t[:, :, j], W["ffn_conv_w"].t[l][j].rearrange("(c p) -> p c", p=128), w=[fcw],
                  allow_slow_non_contiguous=True)
        P.dma(fcb.t[:, :], W["ffn_conv_b"].t[l].rearrange("(c p) -> p c", p=128), w=[fcb],
              allow_slow_non_contiguous=True)
        Acol = sb([128, 8], F32, "Acol"); Bcol = sb([128, 8], F32, "Bcol"); tmpc = sb([128, 8], F32, "tmpc")
        xts = [sb([128, D], F32, "xt") for _ in range(2)]
        xs = sb([128, D], BF16, "xs"); junk = sb([128, D], BF16, "junk"); ss = sb([128, 2], F32, "ss")
        hTb = [sb([128, 8, 512], BF16, "hT") for _ in range(2)]
        halo = sb([128, 44, 2], F32, "halo")
        ub = [sb([128, 514], F32, "ub") for _ in range(4)]
        c1_ = [sb([128, 512], F32, "c1") for _ in range(2)]
        gs = sb([128, 512], F32, "gs")
        aT = [sb([128, 22, 512], BF16, "aT") for _ in range(2)]
        n4 = [0]
        for b in range(NSEQ):
            make_AB(l, b, "ffn_pre_g", 3, 4, Acol, Bcol, tmpc)
            P.op("pool", lambda e: e.memset(halo.t[:, :, :], 0.0), w=[halo])
            for tb in range(NB):
                t0 = tb * 512
                hT = hTb[tb % 2]; a_ = aT[tb % 2]
                for tt in range(4):
                    xt = xts[tt % 2]
                    P.dma(xt.t[:, :], xmid[b, t0 + tt * 128:t0 + (tt + 1) * 128, :], r=[HB("xmid%d" % l, b)], w=[xt])
                    norm_transpose(xt, hT, tt, Acol, Bcol, xs, ss, junk, 7)
                for j in range(22):
                    res = []
                    for gv in range(2):
                        ch = gv * 22 + j
                        n4[0] += 1
                        pU = ps[n4[0] % 4]; u_ = ub[n4[0] % 4]; c_ = c1_[gv]
                        for kc in range(8):
                            P.op("pe", lambda e, pU=pU, kc=kc, ch=ch: e.matmul(
                                pU.t[:, :], lhsT=Wu.t[:, kc, ch * 128:(ch + 1) * 128], rhs=hT.t[:, kc, :],
                                start=(kc == 0), stop=(kc == 7)), r=[Wu, hT], w=[pU])
                        P.op("pool", lambda e, u_=u_, ch=ch: e.tensor_copy(out=u_.t[:, 0:2], in_=halo.t[:, ch, :]),
                             r=[halo], w=[u_])
                        act(u_.t[:, 2:514], pU.t[:, :], AF.Identity, [pU], [u_])
                        P.op("pool", lambda e, u_=u_, ch=ch: e.tensor_copy(out=halo.t[:, ch, :], in_=u_.t[:, 512:514]),
                             r=[u_], w=[halo])
                        P.op("dve", lambda e, u_=u_, c_=c_, ch=ch: e.tensor_scalar(
                            out=c_.t[:, :], in0=u_.t[:, 2:514], scalar1=fcw.t[:, ch, 2:3], scalar2=fcb.t[:, ch:ch + 1],
                            op0=ALU.mult, op1=ALU.add), r=[u_, fcw, fcb], w=[c_])
                        P.op("dve", lambda e, u_=u_, c_=c_, ch=ch: e.scalar_tensor_tensor(
                            out=c_.t[:, :], in0=u_.t[:, 1:513], scalar=fcw.t[:, ch, 1:2], in1=c_.t[:, :],
                            op0=ALU.mult, op1=ALU.add), r=[u_, fcw, c_], w=[c_])
                        P.op("dve", lambda e, u_=u_, c_=c_, ch=ch: e.scalar_tensor_tensor(
                            out=c_.t[:, :], in0=u_.t[:, 0:512], scalar=fcw.t[:, ch, 0:1], in1=c_.t[:, :],
                            op0=ALU.mult, op1=ALU.add), r=[u_, fcw, c_], w=[c_])
                        res.append(c_)
                    act(gs.t[:, :], res[0].t[:, :], AF.Silu, [res[0]], [gs])
                    P.op("dve", lambda e, j=j, a_=a_, rv=res[1]: e.tensor_tensor(out=a_.t[:, j, :], in0=gs.t[:, :],
                                                                                 in1=rv.t[:, :], op=ALU.mult),
                         r=[gs, res[1]], w=[a_])
                P.dma(aTs[b][:, :, t0:t0 + 512], a_.t[:, :, :], r=[a_], w=[HB("aTs", b)])
        stage_reset()

        Wd = sb([128, 22, D], BF16, "Wd")
        stg = [sb([128, 8, 512], F32, "stg") for _ in range(2)]
        wdv = W["w_down"].t[l].rearrange("(kc p) n -> p kc n", p=128)
        pieces = []
        for k0 in (0, 8, 16):
            nk = min(8, 22 - k0)
            for c0 in (0, 512):
                pieces.append(((k0, nk, c0), (k0, nk, c0)))
        load_cast(Wd, lambda k0, nk, c0: Wd.t[:, k0:k0 + nk, c0:c0 + 512],
                  lambda k0, nk, c0: wdv[:, k0:k0 + nk, c0:c0 + 512], pieces, stg, ["pool", "dve"])
        GG = sb([128, D], F32, "GG"); tmpb = sb([128, D], F32, "tmpb")
        aT = [sb([128, 22, 512], BF16, "aT") for _ in range(2)]
        ysb = [sb([128, D], F32, "ysb") for _ in range(2)]
        xts = [sb([128, D], F32, "xt") for _ in range(2)]
        junk = sb([128, D], BF16, "junk"); ss = sb([128, 2], F32, "ss")
        for b in range(NSEQ):
            make_GG(l, b, "ffn_post_g", 5, GG, tmpb)
            for tb in range(NB):
                t0 = tb * 512
                a_ = aT[tb % 2]
                P.dma(a_.t[:, :, :], aTs[b][:, :, t0:t0 + 512], r=[HB("aTs", b)], w=[a_])
                for tt in range(4):
                    y_ = ysb[tt % 2]; xt = xts[tt % 2]
                    P.dma(xt.t[:, :], xmid[b, t0 + tt * 128:t0 + (tt + 1) * 128, :], r=[HB("xmid%d" % l, b)], w=[xt])
                    for hf in range(2):
                        pY = ps[(tt * 2 + hf) % 4]
                        for kc in range(22):
                            P.op("pe", lambda e, pY=pY, kc=kc, hf=hf, tt=tt: e.matmul(
                                pY.t[:, :], lhsT=a_.t[:, kc, tt * 128:(tt + 1) * 128], rhs=Wd.t[:, kc, hf * 512:(hf + 1) * 512],
                                start=(kc == 0), stop=(kc == 21)), r=[a_, Wd], w=[pY])
                        act(y_.t[:, hf * 512:(hf + 1) * 512], pY.t[:, :], AF.Identity, [pY], [y_])
                    post_norm_residual(y_, xt, GG, ss, junk, x_dst[b, t0 + tt * 128:t0 + (tt + 1) * 128, :],
                                       HB("x%d" % (l + 1), b) if l < DEPTH - 1 else out_t)
        stage_reset()


_CACHE = {}


def kernel(**inputs):
    NCORES = 8
    x = np.ascontiguousarray(inputs["x"], dtype=np.float32)
    Bt, S, _ = x.shape
    NSEQ = Bt // NCORES
    DEPTH = inputs["ada_w"].shape[0]
    key = (S, NSEQ, DEPTH)
    if key not in _CACHE:
        _CACHE[key] = build(S, NSEQ, DEPTH)[0]
    nc = _CACHE[key]
    consts = make_consts(S)
    wnames = ("ada_w", "ada_b", "mix_pre_g", "mix_post_g", "ffn_pre_g", "ffn_post_g", "w_in", "conv_a_w",
              "conv_a_b", "conv_a_ln_g", "conv_a_ln_b", "lam_q1", "lam_k1", "lam_q2", "lam_k2", "diff_subln_g",
              "w_branch", "w_out", "w_up", "ffn_conv_w", "ffn_conv_b", "w_down")
    shared = {n: np.ascontiguousarray(inputs[n], dtype=np.float32) for n in wnames}
    c = np.ascontiguousarray(inputs["c"], dtype=np.float32)
    pos = np.ascontiguousarray(inputs["positions"], dtype=np.int32)
    in_maps = []
    for i in range(NCORES):
        m = dict(shared)
        m["x"] = x[i * NSEQ:(i + 1) * NSEQ]
        m["c"] = c[i * NSEQ:(i + 1) * NSEQ]
        m["positions"] = pos[i * NSEQ:(i + 1) * NSEQ]
        m["consts"] = consts
        in_maps.append(m)
    res = run_bass_kernel_spmd(nc, in_maps, core_ids=list(range(NCORES)))
    return np.concatenate([r["out"] for r in res.results], axis=0).astype(np.float32)
```

```python
import math
import contextlib
import numpy as np
import concourse.bass as bass
import concourse.mybir as mybir
from concourse.bass_utils import run_bass_kernel_spmd

F32 = mybir.dt.float32
BF16 = mybir.dt.bfloat16
I32 = mybir.dt.int32
AF = mybir.ActivationFunctionType
ALU = mybir.AluOpType
AX = mybir.AxisListType

D = 1024
DIN = 6824
DFF = 2816
EPS = 1e-6
THETA = 10000.0
NIT = 22
MAGIC = 12582912.0
TWO_PI = 2.0 * math.pi
C1 = 6.28125
C2 = TWO_PI - C1
PI_LO = 3.1415925


class Buf:
    __slots__ = ("w", "r")

    def __init__(self):
        self.w = None
        self.r = []


class Tl:
    __slots__ = ("t", "b")

    def __init__(self, t):
        self.t = t
        self.b = Buf()


def _b(x):
    return x.b if isinstance(x, Tl) else x


class Op:
    __slots__ = ("ex", "q", "emit", "deps", "sig", "seq", "isdma")


class _Rec:
    def __init__(self):
        self.call = None

    def __getattr__(self, name):
        def f(*a, **k):
            self.call = (name, a, k)
            return None
        return f


class Prog:
    NSLOT = 8

    def __init__(self, nc):
        self.nc = nc
        self.ops = []
        self.slot_rr = {}

    def op(self, ex, emit, r=(), w=()):
        o = Op()
        rec = _Rec()
        emit(rec)
        nm_, a_, k_ = rec.call
        o.ex = ex; o.q = ex; o.isdma = False; o.sig = False; o.seq = None
        o.emit = lambda eng, nm_=nm_, a_=a_, k_=k_: getattr(eng, nm_)(*a_, **k_)
        self._deps(o, r, w)
        self.ops.append(o)
        return o

    def dma(self, out, in_, r=(), w=(), q="sp", **kw):
        o = Op()
        o.q = q; o.isdma = True; o.sig = True; o.seq = None
        s = self.slot_rr.get(q, 0)
        self.slot_rr[q] = (s + 1) % self.NSLOT
        o.ex = ("dma", q, s)
        o.emit = lambda eng: eng.dma_start(out=out, in_=in_, **kw)
        self._deps(o, r, w)
        self.ops.append(o)
        return o

    def _deps(self, o, reads, writes):
        deps = []
        for x in reads:
            b = _b(x)
            if b.w is not None:
                deps.append((b.w, True))
        for x in writes:
            b = _b(x)
            if b.w is not None:
                deps.append((b.w, False))
            for rr in b.r:
                deps.append((rr, False))
        o.deps = deps
        for x in reads:
            _b(x).r.append(o)
        for x in writes:
            b = _b(x)
            b.w = o
            b.r = []

    def barrier(self):
        last = {}
        for o in self.ops:
            if o.emit is not None:
                last[o.ex] = o
        prev = list(last.values())
        for q in ("pe", "act", "dve", "pool", "sp"):
            o = Op()
            o.ex = "bar_" + q; o.q = q; o.isdma = False; o.sig = False; o.seq = None
            o.emit = None
            o.deps = [(p, True) for p in prev]
            self.ops.append(o)

    def emit_all(self):
        nc = self.nc
        for o in self.ops:
            for (p, raw) in o.deps:
                if p.isdma:
                    continue
                if p.ex == o.ex and not raw and not o.isdma:
                    continue
                p.sig = True
        cnt = {}
        for o in self.ops:
            if o.isdma:
                cnt[o.ex] = cnt.get(o.ex, 0) + 16
                o.seq = cnt[o.ex]
            elif o.sig:
                cnt[o.ex] = cnt.get(o.ex, 0) + 1
                o.seq = cnt[o.ex]
        prev_on_slot = {}
        for o in self.ops:
            if o.isdma:
                p = prev_on_slot.get(o.ex)
                if p is not None:
                    o.deps.append((p, True))
                prev_on_slot[o.ex] = o
        stack = contextlib.ExitStack()
        sems = {}
        for o in self.ops:
            if o.seq is not None and o.ex not in sems:
                nm = "s_" + ("_".join(str(x) for x in o.ex) if isinstance(o.ex, tuple) else o.ex)
                sems[o.ex] = stack.enter_context(nc.semaphore(nm))
        byq = {"pe": [], "act": [], "dve": [], "pool": [], "sp": []}
        for o in self.ops:
            byq[o.q].append(o)
        self.n_inst = 0

        def run(eng, lst):
            seen = {}
            for o in lst:
                need = {}
                for (p, raw) in o.deps:
                    if (not p.isdma) and p.ex == o.ex and not raw and not o.isdma:
                        continue
                    if p.seq is None:
                        continue
                    if need.get(p.ex, 0) < p.seq:
                        need[p.ex] = p.seq
                for ex, v in need.items():
                    if seen.get(ex, 0) >= v:
                        continue
                    seen[ex] = v
                    eng.wait_ge(sems[ex], v)
                    self.n_inst += 1
                if o.emit is None:
                    continue
                ins = o.emit(eng)
                self.n_inst += 1
                if o.isdma:
                    ins.then_inc(sems[o.ex], 16)
                elif o.sig:
                    ins.then_inc(sems[o.ex], 1)

        with stack, nc.Block() as block:
            @block.tensor
            def _(e):
                run(e, byq["pe"])

            @block.scalar
            def _(e):
                run(e, byq["act"])

            @block.vector
            def _(e):
                run(e, byq["dve"])

            @block.gpsimd
            def _(e):
                run(e, byq["pool"])

            @block.sync
            def _(e):
                run(e, byq["sp"])


def make_consts(S=4096):
    p = np.arange(128)[:, None]
    f = np.arange(128)[None, :]
    c = np.zeros((128, 1024), np.float32)
    c[:, 0:128] = (p == f)
    c[:, 128:256] = (p <= f)
    c[:, 256:384] = (p < f)
    c[:, 384:512] = (p > f)
    c[:, 512:640] = np.where(f <= p, 0.0, -1e30)
    sw32 = np.where(f % 32 < 16, f + 16, f - 16)
    c[:, 640:768] = (p == sw32)
    sw64 = np.where(f % 64 < 32, f + 32, f - 32)
    c[:, 768:896] = (p == sw64)
    pp = np.arange(128)
    c[:, 896] = THETA ** (-(2.0 * (pp % 16)) / 32.0)
    c[:, 897] = THETA ** (-(2.0 * (pp % 32)) / 64.0)
    c[:, 898] = np.where(pp % 32 < 16, -1.0, 1.0)
    c[:, 899] = np.where(pp % 64 < 32, -1.0, 1.0)
    c[:, 900] = EPS
    c[:, 901] = 1.0
    c[:, 902] = 0.0
    c[:, 903] = float(min(256, S // 4)) - 0.5
    for i in range(NIT + 2):
        c[:, 904 + i] = 2.0 ** (-(i + 1))
    return c


class _Stop(Exception):
    pass


def build(S, NSEQ, DEPTH, debug=False, cwin=None, stop=None):
    h = {}
    try:
        _build(h, S, NSEQ, DEPTH, debug, cwin, stop)
    except _Stop:
        pass
    h["P"].barrier()
    h["P"].emit_all()
    return h["nc"], h["P"]


def _build(holder, S, NSEQ, DEPTH, debug, cwin, stop):
    nc = bass.Bass("TRN2", target_bir_lowering=False)
    P = Prog(nc)
    holder["nc"] = nc
    holder["P"] = P
    T = S // 128
    NB = S // 512
    TOPK = min(256, S // 4)
    dbg_kind = "ExternalOutput" if debug else "Internal"

    def din(name, shape, dt=F32):
        return Tl(nc.dram_tensor(name, list(shape), dt, kind="ExternalInput").ap())

    def dscr(name, shape, dt):
        return nc.dram_tensor(name, list(shape), dt, kind=dbg_kind).ap()

    x_in = din("x", [NSEQ, S, D])
    c_in = din("c", [NSEQ, D])
    pos_in = din("positions", [NSEQ, S], I32)
    consts_in = din("consts", [128, 1024])
    W = {}
    for nm, shp in (("ada_w", [DEPTH, D, 6 * D]), ("ada_b", [DEPTH, 6 * D]), ("mix_pre_g", [DEPTH, D]),
                    ("mix_post_g", [DEPTH, D]), ("ffn_pre_g", [DEPTH, D]), ("ffn_post_g", [DEPTH, D]),
                    ("w_in", [DEPTH, D, DIN]), ("conv_a_w", [DEPTH, 31, 256]), ("conv_a_b", [DEPTH, 256]),
                    ("conv_a_ln_g", [DEPTH, 256]), ("conv_a_ln_b", [DEPTH, 256]), ("lam_q1", [DEPTH, 32]),
                    ("lam_k1", [DEPTH, 32]), ("lam_q2", [DEPTH, 32]), ("lam_k2", [DEPTH, 32]),
                    ("diff_subln_g", [DEPTH, 64]), ("w_branch", [DEPTH, 4, 256, D]), ("w_out", [DEPTH, D, D]),
                    ("w_up", [DEPTH, D, 2 * DFF]), ("ffn_conv_w", [DEPTH, 3, 2 * DFF]),
                    ("ffn_conv_b", [DEPTH, 2 * DFF]), ("w_down", [DEPTH, DFF, D])):
        W[nm] = din(nm, shp)
    out_t = Tl(nc.dram_tensor("out", [NSEQ, S, D], F32, kind="ExternalOutput").ap())

    modbuf = Tl(dscr("modbuf", [DEPTH, NSEQ, 6 * D], F32))
    xmid = dscr("xmid", [NSEQ, S, D], F32)
    xlay = dscr("xlay", [NSEQ, S, D], F32)
    hTs = dscr("hTs", [NSEQ, 128, 8, S], BF16)
    fm = {k: dscr(k, [NSEQ, 128, 2, S], BF16) for k in
          ("gluT", "qbT", "kbT", "qcT", "kcT", "qdT", "qiT", "oaT", "obT", "ocT", "odT")}
    kdT = dscr("kdT", [NSEQ, 64, S], BF16)
    kiT = dscr("kiT", [NSEQ, 32, S], BF16)
    vbs = dscr("vbs", [NSEQ, S, 256], BF16)
    vcs = dscr("vcs", [NSEQ, S, 256], BF16)
    vds = dscr("vds", [NSEQ, S, 64], BF16)
    wis = dscr("wis", [NSEQ, S, 8], F32)
    aTs = dscr("aTs", [NSEQ, 128, 22, S], BF16)
    hb = {}

    def HB(name, b):
        k = (name, b)
        if k not in hb:
            hb[k] = Buf()
        return hb[k]

    SB_BASE = (nc.sbuf_base + 63) // 64 * 64
    SB_TOP = nc.sbuf_top
    off = [SB_BASE]
    uid = [0]

    def sb(shape, dt, name="t"):
        esz = 4 if dt in (F32, I32) else 2
        nbytes = int(np.prod(shape[1:])) * esz
        nbytes = (nbytes + 63) // 64 * 64
        uid[0] += 1
        t = nc.alloc_sbuf_tensor_at(f"{name}_{uid[0]}", list(shape), dt, offset=off[0])
        off[0] += nbytes
        assert off[0] <= SB_TOP, f"SBUF overflow at {name}: {off[0]} > {SB_TOP}"
        return Tl(t)

    ps = [Tl(nc.alloc_psum_tensor(f"psb{i}", [128, 512], F32)) for i in range(8)]

    def psbf(i):
        return ps[i].t[:, :].bitcast(BF16)

    cst = sb([128, 1024], F32, "cst")
    P.dma(cst.t[:, :], consts_in.t[:, :], w=[cst])
    cbf = sb([128, 896], BF16, "cbf")
    P.op("dve", lambda e: e.tensor_copy(out=cbf.t[:, :], in_=cst.t[:, 0:896]), r=[cst], w=[cbf])
    ones_bf = sb([128, 128], BF16, "ones")
    P.op("pool", lambda e: e.memset(ones_bf.t[:, :], 1.0), w=[ones_bf])
    ones32 = sb([128, 128], F32, "ones32")
    P.op("pool", lambda e: e.memset(ones32.t[:, :], 1.0), w=[ones32])
    ident_bf = cbf.t[:, 0:128]
    le_bf = cbf.t[:, 128:256]
    lt_bf = cbf.t[:, 256:384]
    gt_bf = cbf.t[:, 384:512]
    negm = cst.t[:, 512:640]
    perm32 = cbf.t[:, 640:768]
    perm64 = cbf.t[:, 768:896]
    invf = {32: cst.t[:, 896:897], 64: cst.t[:, 897:898]}
    sgn = {32: cst.t[:, 898:899], 64: cst.t[:, 899:900]}
    eps_c = cst.t[:, 900:901]
    one_c = cst.t[:, 901:902]
    pow2 = cst.t[:, 904:904 + NIT + 2]
    topk_c = cst.t[:, 903:904]
    zero_c = cst.t[:, 902:903]
    PERSIST = off[0]

    import os as _os
    SUB = int(_os.environ.get('SUB', '0'))

    def ck(n):
        if SUB == n:
            raise _Stop()

    stage_no = [0]

    def stage_reset():
        P.barrier()
        stage_no[0] += 1
        if stop is not None and stage_no[0] >= stop:
            raise _Stop()
        print('stage sbuf bytes', off[0], 'of', SB_TOP, flush=True)
        off[0] = PERSIST

    def act(out, in_, func, r, w, **kw):
        P.op("act", lambda e: e.activation(out=out, in_=in_, func=func, **kw), r=r, w=w)

    def rstd_from_ss(ss, n, r, w_tmp):
        act(ss, ss, AF.Ln, r, w_tmp, scale=1.0 / n, bias=eps_c[0:ss.shape[0], :])
        act(ss, ss, AF.Exp, w_tmp, w_tmp, scale=-0.5)

    def load_cast(dst_tl, dst_ap_fn, src_ap_fn, pieces, stg, eng_cycle):
        for i, (dargs, sargs) in enumerate(pieces):
            st = stg[i % len(stg)]
            d_ap = dst_ap_fn(*dargs)
            s_ap = src_ap_fn(*sargs)
            shp = list(d_ap.shape)
            if len(shp) == 3:
                st_ap = st.t[0:shp[0], 0:shp[1], 0:shp[2]]
            else:
                st_ap = st.t[0:shp[0], 0, 0:shp[1]]
            P.dma(st_ap, s_ap, w=[st])
            ex = eng_cycle[i % len(eng_cycle)]
            P.op(ex, lambda e, o=d_ap, a=st_ap: e.tensor_copy(out=o, in_=a), r=[st], w=[dst_tl])

    cT = sb([128, 8, NSEQ], F32, "cT")
    for b_ in range(NSEQ):
        P.dma(cT.t[:, :, b_], c_in.t[b_].rearrange("(c p) -> p c", p=128), w=[cT], allow_slow_non_contiguous=True)
    act(cT.t[:, :, :], cT.t[:, :, :], AF.Silu, [cT], [cT])
    adst = [sb([128, 8, 512], F32, "adst") for _ in range(2)]
    modsb = sb([NSEQ, 6 * D], F32, "modsb")
    adab = sb([NSEQ, 6 * D], F32, "adab")
    for l in range(DEPTH):
        P.dma(adab.t[:, :], W["ada_b"].t[l:l + 1, :].partition_broadcast(NSEQ), w=[adab])
        wv = W["ada_w"].t[l].rearrange("(kc p) n -> p kc n", p=128)
        for nb in range(12):
            st = adst[nb % 2]
            P.dma(st.t[:, :, :], wv[:, :, nb * 512:(nb + 1) * 512], w=[st])
            pb = ps[nb % 2]
            for kc in range(8):
                P.op("pe", lambda e, st=st, pb=pb, kc=kc: e.matmul(
                    pb.t[0:NSEQ, :], lhsT=cT.t[:, kc, :], rhs=st.t[:, kc, :], start=(kc == 0), stop=(kc == 7)),
                    r=[cT, st], w=[pb])
            P.op("dve", lambda e, pb=pb, nb=nb: e.tensor_tensor(
                out=modsb.t[:, nb * 512:(nb + 1) * 512], in0=pb.t[0:NSEQ, :],
                in1=adab.t[:, nb * 512:(nb + 1) * 512], op=ALU.add), r=[pb, adab], w=[modsb])
        P.dma(modbuf.t[l], modsb.t[:, :], r=[modsb], w=[modbuf])
    stage_reset()

    def col8(ap1d):
        return ap1d.rearrange("(c p) -> p c", p=128)

    def norm_transpose(xt, hT, tt, Acol, Bcol, xs, ss, junk, pbank):
        act(junk.t[:, :], xt.t[:, :], AF.Square, [xt], [junk, ss], accum_out=ss.t[:, 0:1])
        rstd_from_ss(ss.t[:, 0:1], D, [ss], [ss])
        P.op("dve", lambda e: e.tensor_scalar(out=xs.t[:, :], in0=xt.t[:, :], scalar1=ss.t[:, 0:1], scalar2=None,
                                              op0=ALU.mult), r=[xt, ss], w=[xs])
        pv = psbf(pbank)
        for c in range(8):
            P.op("pe", lambda e, c=c: e.transpose(pv[:, c * 128:(c + 1) * 128], xs.t[:, c * 128:(c + 1) * 128],
                                                  ident_bf), r=[xs, cbf], w=[ps[pbank]])
        for c in range(8):
            act(hT.t[:, c, tt * 128:(tt + 1) * 128], pv[:, c * 128:(c + 1) * 128], AF.Identity,
                [ps[pbank], Acol, Bcol], [hT], scale=Acol.t[:, c:c + 1], bias=Bcol.t[:, c:c + 1])

    def post_norm_residual(ysb, xt, GG, ss, junk, dst_ap, dst_buf):
        act(junk.t[:, :], ysb.t[:, :], AF.Square, [ysb], [junk, ss], accum_out=ss.t[:, 0:1])
        rstd_from_ss(ss.t[:, 0:1], D, [ss], [ss])
        P.op("dve", lambda e: e.scalar_tensor_tensor(out=ysb.t[:, :], in0=ysb.t[:, :], scalar=ss.t[:, 0:1],
                                                     in1=GG.t[:, :], op0=ALU.mult, op1=ALU.mult),
             r=[ysb, ss, GG], w=[ysb])
        P.op("dve", lambda e: e.tensor_tensor(out=ysb.t[:, :], in0=ysb.t[:, :], in1=xt.t[:, :], op=ALU.add),
             r=[ysb, xt], w=[ysb])
        P.dma(dst_ap, ysb.t[:, :], r=[ysb], w=[dst_buf])

    def mod_cols(l, b, k):
        return col8(modbuf.t[l, b, k * D:(k + 1) * D])

    def make_AB(l, b, gname, ksh, ksc, Acol, Bcol, tmp):
        P.dma(Bcol.t[:, :], mod_cols(l, b, ksh), r=[modbuf], w=[Bcol], allow_slow_non_contiguous=True)
        P.dma(tmp.t[:, :], mod_cols(l, b, ksc), r=[modbuf], w=[tmp], allow_slow_non_contiguous=True)
        P.dma(Acol.t[:, :], col8(W[gname].t[l]), w=[Acol], allow_slow_non_contiguous=True)
        P.op("dve", lambda e: e.scalar_tensor_tensor(out=Acol.t[:, :], in0=tmp.t[:, :], scalar=1.0, in1=Acol.t[:, :],
                                                     op0=ALU.add, op1=ALU.mult), r=[tmp, Acol], w=[Acol])

    def make_GG(l, b, gname, kg, GG, tmpb):
        P.dma(GG.t[:, :], W[gname].t[l:l + 1, :].partition_broadcast(128), w=[GG])
        P.dma(tmpb.t[:, :], modbuf.t[l, b:b + 1, kg * D:(kg + 1) * D].partition_broadcast(128), r=[modbuf], w=[tmpb])
        P.op("dve", lambda e: e.tensor_tensor(out=GG.t[:, :], in0=GG.t[:, :], in1=tmpb.t[:, :], op=ALU.mult),
             r=[GG, tmpb], w=[GG])

    for l in range(DEPTH):
        x_src = x_in.t if l == 0 else xlay
        x_dst = out_t.t if l == DEPTH - 1 else xlay
        lam_init = 0.8 - 0.6 * math.exp(-0.3 * l)

        W1 = sb([128, 8, 2728], BF16, "W1")
        stg = [sb([128, 8, 512], F32, "stg") for _ in range(2)]
        wv = W["w_in"].t[l].rearrange("(kc p) n -> p kc n", p=128)
        pieces = []
        c0 = 0
        while c0 < 2728:
            n = min(512, 2728 - c0)
            pieces.append(((c0, n), (c0, n)))
            c0 += n
        load_cast(W1, lambda c0, n: W1.t[:, :, c0:c0 + n], lambda c0, n: wv[:, :, c0:c0 + n], pieces, stg,
                  ["pool", "dve"])
        Acol = sb([128, 8], F32, "Acol"); Bcol = sb([128, 8], F32, "Bcol"); tmpc = sb([128, 8], F32, "tmpc")
        xts = [sb([128, D], F32, "xt") for _ in range(2)]
        xs = sb([128, D], BF16, "xs"); junk = sb([128, D], BF16, "junk"); ss = sb([128, 2], F32, "ss")
        hTb = [sb([128, 8, 512], BF16, "hT") for _ in range(2)]
        posi = sb([128, 512], I32, "posi"); posf = sb([128, 512], F32, "posf")
        ang = sb([128, 512], F32, "ang"); kk = sb([128, 512], F32, "kk"); rr = sb([128, 512], F32, "rr")
        r2 = sb([128, 512], F32, "r2"); mm_ = sb([128, 512], F32, "mm")
        cosT = {32: sb([128, 512], F32, "cos32"), 64: sb([128, 512], F32, "cos64")}
        sinT = {32: sb([128, 512], F32, "sin32"), 64: sb([128, 512], F32, "sin64")}
        qsb = [sb([128, 512], BF16, "qsb") for _ in range(2)]
        t1 = sb([128, 512], F32, "t1"); t2 = sb([128, 512], F32, "t2")
        outb = [sb([128, 512], BF16, "outb") for _ in range(3)]
        sg = sb([128, 512], F32, "sg")
        vtm = [sb([128, 512], BF16, "vtm") for _ in range(2)]
        vtd = [sb([128, 64], BF16, "vtd") for _ in range(2)]
        wtm = [sb([128, 8], F32, "wtm") for _ in range(2)]
        ob_i = [0]

        def next_outb():
            ob_i[0] += 1
            return outb[ob_i[0] % 3]

        for b in range(NSEQ):
            make_AB(l, b, "mix_pre_g", 0, 1, Acol, Bcol, tmpc)
            for tb in range(NB):
                t0 = tb * 512
                hT = hTb[(b * NB + tb) % 2]
                for tt in range(4):
                    xt = xts[tt % 2]
                    P.dma(xt.t[:, :], x_src[b, t0 + tt * 128:t0 + (tt + 1) * 128, :],
                          r=[HB("x%d" % l, b)], w=[xt])
                    norm_transpose(xt, hT, tt, Acol, Bcol, xs, ss, junk, 7)
                P.dma(hTs[b][:, :, t0:t0 + 512], hT.t[:, :, :], r=[hT], w=[HB("hTs", b)])
                ck(1)
                P.dma(posi.t[:, :], pos_in.t[b:b + 1, t0:t0 + 512].partition_broadcast(128), w=[posi])
                P.op("dve", lambda e: e.tensor_copy(out=posf.t[:, :], in_=posi.t[:, :]), r=[posi], w=[posf])
                for dd in (32, 64):
                    P.op("dve", lambda e, dd=dd: e.tensor_scalar(out=ang.t[:, :], in0=posf.t[:, :], scalar1=invf[dd],
                                                               scalar2=None, op0=ALU.mult), r=[posf, cst], w=[ang])
                    P.op("dve", lambda e: e.tensor_scalar(out=kk.t[:, :], in0=ang.t[:, :], scalar1=1.0 / TWO_PI,
                                                          scalar2=MAGIC, op0=ALU.mult, op1=ALU.add), r=[ang], w=[kk])
                    P.op("dve", lambda e: e.tensor_scalar(out=kk.t[:, :], in0=kk.t[:, :], scalar1=MAGIC, scalar2=None,
                                                          op0=ALU.subtract), r=[kk], w=[kk])
                    P.op("dve", lambda e: e.scalar_tensor_tensor(out=rr.t[:, :], in0=kk.t[:, :], scalar=-C1,
                                                                 in1=ang.t[:, :], op0=ALU.mult, op1=ALU.add),
                         r=[kk, ang], w=[rr])
                    P.op("dve", lambda e: e.scalar_tensor_tensor(out=rr.t[:, :], in0=kk.t[:, :], scalar=-C2,
                                                                 in1=rr.t[:, :], op0=ALU.mult, op1=ALU.add),
                         r=[kk, rr], w=[rr])
                    P.op("dve", lambda e: e.tensor_scalar(out=rr.t[:, :], in0=rr.t[:, :], scalar1=PI_LO,
                                                          scalar2=-PI_LO, op0=ALU.min, op1=ALU.max), r=[rr], w=[rr])
                    act(sinT[dd].t[:, :], rr.t[:, :], AF.Sin, [rr, cst], [sinT[dd]], scale=sgn[dd])
                    P.op("dve", lambda e: e.tensor_scalar(out=r2.t[:, :], in0=rr.t[:, :], scalar1=math.pi / 2,
                                                          scalar2=None, op0=ALU.add), r=[rr], w=[r2])
                    P.op("dve", lambda e: e.tensor_scalar(out=mm_.t[:, :], in0=r2.t[:, :], scalar1=math.pi,
                                                          scalar2=-TWO_PI, op0=ALU.is_gt, op1=ALU.mult),
                         r=[r2], w=[mm_])
                    P.op("dve", lambda e: e.tensor_tensor(out=r2.t[:, :], in0=r2.t[:, :], in1=mm_.t[:, :],
                                                          op=ALU.add), r=[r2, mm_], w=[r2])
                    P.op("dve", lambda e: e.tensor_scalar(out=r2.t[:, :], in0=r2.t[:, :], scalar1=PI_LO,
                                                          scalar2=-PI_LO, op0=ALU.min, op1=ALU.max), r=[r2], w=[r2])
                    act(cosT[dd].t[:, :], r2.t[:, :], AF.Sin, [r2], [cosT[dd]])

                ck(2)
                bank = [0]

                def proj_fm(c0, m):
                    pb = ps[bank[0] % 4]
                    bank[0] += 1
                    for kc in range(8):
                        P.op("pe", lambda e, pb=pb, kc=kc: e.matmul(pb.t[0:m, :], lhsT=W1.t[:, kc, c0:c0 + m],
                                                                    rhs=hT.t[:, kc, :], start=(kc == 0),
                                                                    stop=(kc == 7)), r=[W1, hT], w=[pb])
                    return pb

                def store(dst_ap, srct, m, key):
                    P.dma(dst_ap, srct.t[0:m, :], r=[srct], w=[HB(key, b)])

                for cc in range(2):
                    pa = proj_fm(cc * 128, 128)
                    pg = proj_fm(256 + cc * 128, 128)
                    act(sg.t[:, :], pg.t[:, :], AF.Sigmoid, [pg], [sg])
                    ob = next_outb()
                    P.op("dve", lambda e, pa=pa, ob=ob: e.tensor_tensor(out=ob.t[:, :], in0=pa.t[:, :], in1=sg.t[:, :],
                                                                        op=ALU.mult), r=[pa, sg], w=[ob])
                    store(fm["gluT"][b][:, cc, t0:t0 + 512], ob, 128, "gluT")

                ck(3)

                def roped(c0, m, dd, dst_ap, key):
                    pq = proj_fm(c0, m)
                    q = qsb[bank[0] % 2]
                    act(q.t[0:m, :], pq.t[0:m, :], AF.Identity, [pq], [q])
                    ck(8)
                    pw = ps[4 + bank[0] % 2]
                    pm = perm32 if dd == 32 else perm64
                    P.op("pe", lambda e: e.matmul(pw.t[0:m, :], lhsT=pm[0:m, 0:m], rhs=q.t[0:m, :], start=True,
                                                  stop=True), r=[q, cbf], w=[pw])
                    ck(9)
                    P.op("dve", lambda e: e.tensor_tensor(out=t1.t[0:m, :], in0=q.t[0:m, :], in1=cosT[dd].t[0:m, :],
                                                          op=ALU.mult), r=[q, cosT[dd]], w=[t1])
                    P.op("dve", lambda e: e.tensor_tensor(out=t2.t[0:m, :], in0=pw.t[0:m, :], in1=sinT[dd].t[0:m, :],
                                                          op=ALU.mult), r=[pw, sinT[dd]], w=[t2])
                    ob = next_outb()
                    P.op("dve", lambda e: e.tensor_tensor(out=ob.t[0:m, :], in0=t1.t[0:m, :], in1=t2.t[0:m, :],
                                                          op=ALU.add), r=[t1, t2], w=[ob])
                    store(dst_ap, ob, m, key)

                def plain(c0, m, dst_ap, key):
                    pq = proj_fm(c0, m)
                    ob = next_outb()
                    act(ob.t[0:m, :], pq.t[0:m, :], AF.Identity, [pq], [ob])
                    store(dst_ap, ob, m, key)

                for cc in range(2):
                    roped(512 + cc * 128, 128, 32, fm["qbT"][b][:, cc, t0:t0 + 512], "qbT")
                    ck(5)
                    roped(768 + cc * 128, 128, 32, fm["kbT"][b][:, cc, t0:t0 + 512], "kbT")
                    ck(6)
                    plain(1280 + cc * 128, 128, fm["qcT"][b][:, cc, t0:t0 + 512], "qcT")
                    ck(7)
                    plain(1536 + cc * 128, 128, fm["kcT"][b][:, cc, t0:t0 + 512], "kcT")
                    roped(2048 + cc * 128, 128, 64, fm["qdT"][b][:, cc, t0:t0 + 512], "qdT")
                    roped(2432 + cc * 128, 128, 32, fm["qiT"][b][:, cc, t0:t0 + 512], "qiT")
                roped(2304, 64, 64, kdT[b][:, t0:t0 + 512], "kdT")
                roped(2688, 32, 32, kiT[b][:, t0:t0 + 512], "kiT")
                ck(4)
                for tt in range(4):
                    pv_ = ps[6]
                    for kc in range(8):
                        P.op("pe", lambda e, kc=kc, tt=tt: e.matmul(pv_.t[:, 0:256], lhsT=hT.t[:, kc, tt * 128:(tt + 1) * 128],
                                                                    rhs=W1.t[:, kc, 1024:1280], start=(kc == 0),
                                                                    stop=(kc == 7)), r=[W1, hT], w=[pv_])
                    for kc in range(8):
                        P.op("pe", lambda e, kc=kc, tt=tt: e.matmul(pv_.t[:, 256:512], lhsT=hT.t[:, kc, tt * 128:(tt + 1) * 128],
                                                                    rhs=W1.t[:, kc, 1792:2048], start=(kc == 0),
                                                                    stop=(kc == 7)), r=[W1, hT], w=[pv_])
                    v = vtm[tt % 2]
                    act(v.t[:, :], pv_.t[:, :], AF.Identity, [pv_], [v])
                    P.dma(vbs[b, t0 + tt * 128:t0 + (tt + 1) * 128, :], v.t[:, 0:256], r=[v], w=[HB("vbs", b)])
                    P.dma(vcs[b, t0 + tt * 128:t0 + (tt + 1) * 128, :], v.t[:, 256:512], r=[v], w=[HB("vcs", b)])
                    pv2 = ps[5]
                    for kc in range(8):
                        P.op("pe", lambda e, kc=kc, tt=tt: e.matmul(pv2.t[:, 0:64], lhsT=hT.t[:, kc, tt * 128:(tt + 1) * 128],
                                                                    rhs=W1.t[:, kc, 2368:2432], start=(kc == 0),
                                                                    stop=(kc == 7)), r=[W1, hT], w=[pv2])
                    for kc in range(8):
                        P.op("pe", lambda e, kc=kc, tt=tt: e.matmul(pv2.t[:, 64:72], lhsT=hT.t[:, kc, tt * 128:(tt + 1) * 128],
                                                                    rhs=W1.t[:, kc, 2720:2728], start=(kc == 0),
                                                                    stop=(kc == 7)), r=[W1, hT], w=[pv2])
                    vd_ = vtd[tt % 2]; wt_ = wtm[tt % 2]
                    act(vd_.t[:, :], pv2.t[:, 0:64], AF.Identity, [pv2], [vd_])
                    act(wt_.t[:, :], pv2.t[:, 64:72], AF.Identity, [pv2], [wt_])
                    P.dma(vds[b, t0 + tt * 128:t0 + (tt + 1) * 128, :], vd_.t[:, :], r=[vd_], w=[HB("vds", b)])
                    P.dma(wis[b, t0 + tt * 128:t0 + (tt + 1) * 128, :], wt_.t[:, :], r=[wt_], w=[HB("wis", b)])
        stage_reset()

        cw = sb([128, 2, 31], F32, "cw")
        for cc in range(2):
            P.dma(cw.t[:, cc, :], W["conv_a_w"].t[l][:, cc * 128:(cc + 1) * 128].rearrange("j p -> p j"), w=[cw],
                  allow_slow_non_contiguous=True)
        cb3 = sb([128, 6], F32, "cb3")
        for i, nm in enumerate(("conv_a_b", "conv_a_ln_g", "conv_a_ln_b")):
            P.dma(cb3.t[:, 2 * i:2 * i + 2], W[nm].t[l].rearrange("(c p) -> p c", p=128), w=[cb3],
                  allow_slow_non_contiguous=True)
        dg = sb([128, 62, 128], BF16, "dg")
        for cc in range(2):
            for j in range(31):
                P.op("dve", lambda e, cc=cc, j=j: e.tensor_scalar(out=dg.t[:, cc * 31 + j, :], in0=cst.t[:, 0:128],
                                                                 scalar1=cw.t[:, cc, j:j + 1], scalar2=None,
                                                                 op0=ALU.mult), r=[cst, cw], w=[dg])
        o256 = sb([128, 128], F32, "o256")
        P.op("pool", lambda e: e.memset(o256.t[:, :], 1.0 / 256.0), w=[o256])
        gl = sb([128, 2, S + 32], BF16, "gl")
        cv = [sb([128, 2, 512], F32, "cv") for _ in range(2)]
        sq = sb([128, 2, 512], F32, "sq")
        mean_sb = sb([128, 512], F32, "mean_sb")
        m2 = sb([128, 512], F32, "m2"); var = sb([128, 512], F32, "var"); xc = sb([128, 512], F32, "xc")
        oa = [sb([128, 512], BF16, "oa") for _ in range(2)]
        for b in range(NSEQ):
            P.op("pool", lambda e: e.memset(gl.t[:, :, 0:32], 0.0), w=[gl])
            P.dma(gl.t[:, :, 32:32 + S], fm["gluT"][b][:, :, :], r=[HB("gluT", b)], w=[gl])
            for tb in range(NB):
                t0 = tb * 512
                cvt = cv[tb % 2]
                for cc in range(2):
                    pb = ps[cc]
                    for j in range(31):
                        P.op("pe", lambda e, cc=cc, j=j, pb=pb: e.matmul(
                            pb.t[:, :], lhsT=dg.t[:, cc * 31 + j, :], rhs=gl.t[:, cc, t0 + 2 + j:t0 + 2 + j + 512],
                            start=(j == 0), stop=(j == 30)), r=[dg, gl], w=[pb])
                    act(cvt.t[:, cc, :], pb.t[:, :], AF.Identity, [pb, cb3], [cvt], bias=cb3.t[:, cc:cc + 1])
                    act(sq.t[:, cc, :], cvt.t[:, cc, :], AF.Square, [cvt], [sq])
                pm_ = ps[2]; pe2 = ps[3]
                for cc in range(2):
                    P.op("pe", lambda e, cc=cc: e.matmul(pm_.t[:, :], lhsT=o256.t[:, :], rhs=cvt.t[:, cc, :],
                                                         start=(cc == 0), stop=(cc == 1)), r=[o256, cvt], w=[pm_])
                for cc in range(2):
                    P.op("pe", lambda e, cc=cc: e.matmul(pe2.t[:, :], lhsT=o256.t[:, :], rhs=sq.t[:, cc, :],
                                                         start=(cc == 0), stop=(cc == 1)), r=[o256, sq], w=[pe2])
                act(mean_sb.t[:, :], pm_.t[:, :], AF.Identity, [pm_], [mean_sb])
                act(m2.t[:, :], mean_sb.t[:, :], AF.Square, [mean_sb], [m2])
                P.op("dve", lambda e: e.tensor_tensor(out=var.t[:, :], in0=pe2.t[:, :], in1=m2.t[:, :], op=ALU.subtract),
                     r=[pe2, m2], w=[var])
                act(var.t[:, :], var.t[:, :], AF.Ln, [var], [var], bias=eps_c)
                act(var.t[:, :], var.t[:, :], AF.Exp, [var], [var], scale=-0.5)
                for cc in range(2):
                    P.op("dve", lambda e, cc=cc: e.tensor_tensor(out=xc.t[:, :], in0=cvt.t[:, cc, :], in1=mean_sb.t[:, :],
                                                                 op=ALU.subtract), r=[cvt, mean_sb], w=[xc])
                    P.op("dve", lambda e: e.tensor_tensor(out=xc.t[:, :], in0=xc.t[:, :], in1=var.t[:, :], op=ALU.mult),
                         r=[xc, var], w=[xc])
                    o_ = oa[cc]
                    act(o_.t[:, :], xc.t[:, :], AF.Silu, [xc, cb3], [o_], scale=cb3.t[:, 2 + cc:3 + cc],
                        bias=cb3.t[:, 4 + cc:5 + cc])
                    P.dma(fm["oaT"][b][:, cc, t0:t0 + 512], o_.t[:, :], r=[o_], w=[HB("oaT", b)])
        stage_reset()

        lamt = sb([128, 8], F32, "lamt")
        lq = sb([128, 4, 32], F32, "lq")
        for i, nm in enumerate(("lam_q1", "lam_k1", "lam_q2", "lam_k2")):
            P.dma(lq.t[:, i, :], W[nm].t[l:l + 1, :].partition_broadcast(128), w=[lq])
        lj = sb([128, 32], F32, "lj")
        P.op("pool", lambda e: e.memset(lamt.t[:, :], 0.0), w=[lamt])
        for i in range(2):
            P.op("dve", lambda e, i=i: e.tensor_tensor(out=lj.t[:, :], in0=lq.t[:, 2 * i, :], in1=lq.t[:, 2 * i + 1, :],
                                                       op=ALU.mult), r=[lq], w=[lj])
            P.op("dve", lambda e, i=i: e.tensor_reduce(out=lamt.t[:, i:i + 1], in_=lj.t[:, :], axis=AX.X, op=ALU.add),
                 r=[lj, lamt], w=[lamt])
            act(lamt.t[:, i:i + 1], lamt.t[:, i:i + 1], AF.Exp, [lamt], [lamt])
        P.op("dve", lambda e: e.tensor_tensor(out=lamt.t[:, 2:3], in0=lamt.t[:, 1:2], in1=lamt.t[:, 0:1],
                                              op=ALU.subtract), r=[lamt], w=[lamt])
        P.op("dve", lambda e: e.tensor_scalar(out=lamt.t[:, 2:3], in0=lamt.t[:, 2:3], scalar1=-lam_init, scalar2=None,
                                              op0=ALU.add), r=[lamt], w=[lamt])
        neglam = lamt.t[:, 2:3]
        gsub = sb([128, 1], F32, "gsub")
        for hh in range(2):
            P.dma(gsub.t[hh * 64:(hh + 1) * 64, :], W["diff_subln_g"].t[l].rearrange("(d o) -> d o", o=1), w=[gsub],
                  allow_slow_non_contiguous=True)
        P.op("dve", lambda e: e.tensor_scalar(out=gsub.t[:, :], in0=gsub.t[:, :], scalar1=1.0 - lam_init, scalar2=None,
                                              op0=ALU.mult), r=[gsub], w=[gsub])
        o64 = sb([128, 64], F32, "o64")
        P.op("pool", lambda e: e.memset(o64.t[:, :], 1.0 / 64.0), w=[o64])
        qT = sb([128, 2, S], BF16, "qT"); kT = sb([128, 2, S], BF16, "kT"); vv = sb([128, T, 256], BF16, "vv")
        pt = [sb([128, 512], BF16, "pt") for _ in range(3)]
        rc = [sb([64, 512], F32, "rc") for _ in range(2)]
        tO = [sb([64, 512], F32, "tO") for _ in range(2)]
        od = sb([64, 512], F32, "od"); osq = sb([64, 512], F32, "osq"); rs = sb([64, 512], F32, "rs")
        obo = [sb([64, 512], BF16, "obo") for _ in range(2)]
        pti = [0]
        SC_B = 32 ** -0.5
        for b in range(NSEQ):
            P.dma(qT.t[:, :, :], fm["qbT"][b][:, :, :], r=[HB("qbT", b)], w=[qT])
            P.dma(kT.t[:, :, :], fm["kbT"][b][:, :, :], r=[HB("kbT", b)], w=[kT])
            P.dma(vv.t[:, :, :], vbs[b].rearrange("(t p) f -> p t f", p=128), r=[HB("vbs", b)], w=[vv])
            for h in range(4):
                cc = h // 2
                for qb in range(NB):
                    q0 = qb * 512
                    nkt = qb * 4 + 4
                    def phS(kt, m):
                        i = kt - qb * 4
                        cs = max(i, 0) * 128
                        j = (h % 2) * 2 + m
                        kw = {"tile_position": (96, 0)} if j == 3 else {}
                        pS = ps[(kt * 2 + m) % 3]
                        P.op("pe", lambda e: e.matmul(
                            pS.t[:, cs:512], lhsT=kT.t[32 * j:32 * j + 32, cc, kt * 128:(kt + 1) * 128],
                            rhs=qT.t[32 * j:32 * j + 32, cc, q0 + cs:q0 + 512], start=True, stop=True, **kw),
                            r=[kT, qT], w=[pS])
                        pti[0] += 1
                        p_ = pt[pti[0] % 3]
                        act(p_.t[:, cs:512], pS.t[:, cs:512], AF.Exp, [pS], [p_], scale=SC_B)
                        if i >= 0:
                            P.op("dve", lambda e: e.tensor_tensor(
                                out=p_.t[:, cs:cs + 128], in0=p_.t[:, cs:cs + 128], in1=le_bf, op=ALU.mult),
                                r=[p_, cbf], w=[p_])
                        return (kt, m, cs, p_)

                    def phV(c):
                        kt, m, cs, p_ = c
                        pO = ps[3 + m]; pSm = ps[5 + m]
                        P.op("pe", lambda e: e.matmul(
                            pO.t[0:64, cs:512], lhsT=vv.t[:, kt, h * 64:(h + 1) * 64], rhs=p_.t[:, cs:512],
                            start=(kt == 0), stop=(kt == nkt - 1)), r=[vv, p_], w=[pO])
                        P.op("pe", lambda e: e.matmul(
                            pSm.t[0:64, cs:512], lhsT=ones_bf.t[:, 0:64], rhs=p_.t[:, cs:512],
                            start=(kt == 0), stop=(kt == nkt - 1)), r=[ones_bf, p_], w=[pSm])

                    items = [(kt, m) for kt in range(nkt) for m in range(2)]
                    ctxs = {0: phS(*items[0]), 1: phS(*items[1])}
                    for idx in range(len(items)):
                        phV(ctxs.pop(idx))
                        if idx + 2 < len(items):
                            ctxs[idx + 2] = phS(*items[idx + 2])
                    for m in range(2):
                        P.op("dve", lambda e, m=m: e.reciprocal(out=rc[m].t[:, :], in_=ps[5 + m].t[0:64, :]),
                             r=[ps[5 + m]], w=[rc[m]])
                        P.op("dve", lambda e, m=m: e.tensor_tensor(out=tO[m].t[:, :], in0=ps[3 + m].t[0:64, :],
                                                                   in1=rc[m].t[:, :], op=ALU.mult),
                             r=[ps[3 + m], rc[m]], w=[tO[m]])
                    P.op("dve", lambda e: e.scalar_tensor_tensor(out=od.t[:, :], in0=tO[1].t[:, :], scalar=neglam[0:64, :],
                                                                 in1=tO[0].t[:, :], op0=ALU.mult, op1=ALU.add),
                         r=[tO[0], tO[1], lamt], w=[od])
                    act(osq.t[:, :], od.t[:, :], AF.Square, [od], [osq])
                    pst_ = ps[7]
                    P.op("pe", lambda e: e.matmul(pst_.t[0:64, :], lhsT=o64.t[0:64, :], rhs=osq.t[:, :], start=True,
                                                  stop=True), r=[o64, osq], w=[pst_])
                    act(rs.t[:, :], pst_.t[0:64, :], AF.Ln, [pst_], [rs], bias=eps_c[0:64, :])
                    act(rs.t[:, :], rs.t[:, :], AF.Exp, [rs], [rs], scale=-0.5)
                    o_ = obo[(h * NB + qb) % 2]
                    hb0 = (h % 2) * 64
                    P.op("dve", lambda e, o_=o_, hb0=hb0: e.scalar_tensor_tensor(
                        out=o_.t[:, :], in0=od.t[:, :], scalar=gsub.t[0:64, :], in1=rs.t[:, :],
                        op0=ALU.mult, op1=ALU.mult), r=[od, gsub, rs], w=[o_])
                    P.dma(fm["obT"][b][hb0:hb0 + 64, cc, q0:q0 + 512], o_.t[:, :], r=[o_], w=[HB("obT", b)])
        stage_reset()

        qT = sb([128, 2, S], BF16, "qT"); kT = sb([128, 2, S], BF16, "kT"); vv = sb([128, T, 256], BF16, "vv")
        ee = [sb([128, 512], F32, "ee") for _ in range(4)]
        spb = [sb([128, 512], BF16, "spb") for _ in range(4)]
        lsum2 = [sb([128, 512], BF16, "lsum") for _ in range(2)]
        a1 = [sb([128, 512], F32, "a1") for _ in range(4)]
        wT = [sb([128, 512], BF16, "wT") for _ in range(4)]
        oco = [sb([64, 512], BF16, "oco") for _ in range(2)]
        SC_C = 64 ** -0.5
        it = [0]
        for b in range(NSEQ):
            P.dma(qT.t[:, :, :], fm["qcT"][b][:, :, :], r=[HB("qcT", b)], w=[qT])
            P.dma(kT.t[:, :, :], fm["kcT"][b][:, :, :], r=[HB("kcT", b)], w=[kT])
            P.dma(vv.t[:, :, :], vcs[b].rearrange("(t p) f -> p t f", p=128), r=[HB("vcs", b)], w=[vv])
            for hp in (0, 2):
                for qb in range(NB):
                    q0 = qb * 512
                    top = qb * 4 + 3
                    lo_kt = 0 if cwin is None else max(0, top - 3 - cwin)
                    for hh in range(2):
                        P.op("pool", lambda e, hh=hh: e.memset(lsum2[hh].t[:, :], 0.0), w=[lsum2[hh]])

                    def phA(hh, kt):
                        h = hp + hh
                        cc = h // 2
                        hb0 = (h % 2) * 64
                        i = kt - qb * 4
                        cs = max(i, 0) * 128
                        it[0] += 1
                        n = it[0]
                        c = dict(h=h, i=i, cs=cs, kt=kt, pZ=ps[n % 3], pL=ps[3 + n % 3], e_=ee[n % 4], s_=spb[n % 4],
                                 a_=a1[n % 4], w_=wT[n % 4], pO=ps[6 + hh], lsum=lsum2[hh], first=(kt == top))
                        pZ, e_, s_ = c["pZ"], c["e_"], c["s_"]
                        P.op("pe", lambda e: e.matmul(
                            pZ.t[:, cs:512], lhsT=kT.t[hb0:hb0 + 64, cc, kt * 128:(kt + 1) * 128],
                            rhs=qT.t[hb0:hb0 + 64, cc, q0 + cs:q0 + 512], start=True, stop=True), r=[kT, qT], w=[pZ])
                        act(e_.t[:, cs:512], pZ.t[:, cs:512], AF.Exp, [pZ], [e_], scale=SC_C)
                        act(s_.t[:, cs:512], e_.t[:, cs:512], AF.Ln, [e_], [s_], bias=one_c)
                        if i >= 0:
                            P.op("dve", lambda e: e.tensor_tensor(
                                out=s_.t[:, cs:cs + 128], in0=s_.t[:, cs:cs + 128], in1=lt_bf, op=ALU.mult),
                                r=[s_, cbf], w=[s_])
                        return c

                    def phB(c):
                        pZ, pL, s_, a_, w_, lsum, cs, first, i, kt = (c[k] for k in
                                                                     ("pZ", "pL", "s_", "a_", "w_", "lsum", "cs", "first", "i", "kt"))
                        P.op("pe", lambda e: e.matmul(pL.t[:, cs:512], lhsT=gt_bf, rhs=s_.t[:, cs:512], start=True,
                                                      stop=first), r=[cbf, s_], w=[pL])
                        if not first:
                            P.op("pe", lambda e: e.matmul(pL.t[:, cs:512], lhsT=ones_bf.t[:, :], rhs=lsum.t[:, cs:512],
                                                          start=False, stop=True), r=[ones_bf, lsum], w=[pL])
                        if kt > lo_kt:
                            P.op("dve", lambda e: e.tensor_tensor(
                                out=lsum.t[:, cs:512], in0=lsum.t[:, cs:512], in1=s_.t[:, cs:512], op=ALU.add),
                                r=[lsum, s_], w=[lsum])
                        P.op("dve", lambda e: e.scalar_tensor_tensor(
                            out=a_.t[:, cs:512], in0=pZ.t[:, cs:512], scalar=SC_C, in1=s_.t[:, cs:512],
                            op0=ALU.mult, op1=ALU.subtract), r=[pZ, s_], w=[a_])
                        P.op("dve", lambda e: e.tensor_tensor(
                            out=a_.t[:, cs:512], in0=a_.t[:, cs:512], in1=pL.t[:, cs:512], op=ALU.subtract),
                            r=[a_, pL], w=[a_])
                        act(w_.t[:, cs:512], a_.t[:, cs:512], AF.Exp, [a_], [w_])
                        if i >= 0:
                            P.op("dve", lambda e: e.tensor_tensor(
                                out=w_.t[:, cs:cs + 128], in0=w_.t[:, cs:cs + 128], in1=lt_bf, op=ALU.mult),
                                r=[w_, cbf], w=[w_])

                    def phC(c):
                        pO, w_, cs, first, kt, h = (c[k] for k in ("pO", "w_", "cs", "first", "kt", "h"))
                        P.op("pe", lambda e: e.matmul(
                            pO.t[0:64, cs:512], lhsT=vv.t[:, kt, h * 64:(h + 1) * 64], rhs=w_.t[:, cs:512],
                            start=first, stop=(kt == lo_kt)), r=[vv, w_], w=[pO])

                    kts = list(range(top, lo_kt - 1, -1))
                    ctxA = [phA(hh, kts[0]) for hh in range(2)]
                    for ki, kt in enumerate(kts):
                        cur = ctxA
                        phB(cur[0])
                        if ki + 1 < len(kts):
                            ctxA = [phA(0, kts[ki + 1])]
                        phB(cur[1])
                        if ki + 1 < len(kts):
                            ctxA.append(phA(1, kts[ki + 1]))
                        phC(cur[0])
                        phC(cur[1])
                    for hh in range(2):
                        h = hp + hh
                        o_ = oco[hh]
                        act(o_.t[:, :], ps[6 + hh].t[0:64, :], AF.Identity, [ps[6 + hh]], [o_])
                        P.dma(fm["ocT"][b][(h % 2) * 64:(h % 2) * 64 + 64, h // 2, q0:q0 + 512], o_.t[:, :], r=[o_],
                              w=[HB("ocT", b)])
        stage_reset()

        qiT = sb([128, 2, S], BF16, "qiT"); ki4 = sb([128, S], BF16, "ki4")
        wi_ = sb([128, T, 8], F32, "wi")
        qdT = sb([128, 2, S], BF16, "qdT"); kd2 = sb([128, S], BF16, "kd2"); vd = sb([128, T, 64], BF16, "vd")
        sc = [sb([128, S], F32, "sc") for _ in range(2)]
        rl = [sb([128, 512], F32, "rl") for _ in range(3)]
        st = [sb([128, 8], F32, "st") for _ in range(2)]
        steps = [sb([128, NIT + 2], F32, "steps") for _ in range(2)]
        cnt = [sb([128, NIT + 2], F32, "cnt") for _ in range(2)]
        g2s = [sb([128, 1], F32, "g2") for _ in range(2)]
        jks = [sb([128, S], BF16, "jk") for _ in range(2)]
        mb = [sb([128, S], BF16, "mb") for _ in range(2)]
        MT = [sb([128, T, 128], BF16, "MT") for _ in range(2)]
        pd = [sb([128, 4, 128], BF16, "pd") for _ in range(3)]
        rcd = sb([64, 512], F32, "rcd")
        odo = [sb([64, 4, 128], BF16, "odo") for _ in range(2)]
        SC_D = 64 ** -0.5
        n_ = [0]
        for b in range(NSEQ):
            P.dma(qiT.t[:, :, :], fm["qiT"][b][:, :, :], r=[HB("qiT", b)], w=[qiT])
            for g in range(4):
                P.dma(ki4.t[32 * g:32 * g + 32, :], kiT[b][:, :], r=[HB("kiT", b)], w=[ki4])
            P.dma(wi_.t[:, :, :], wis[b].rearrange("(t p) f -> p t f", p=128), r=[HB("wis", b)], w=[wi_])
            P.dma(qdT.t[:, :, :], fm["qdT"][b][:, :, :], r=[HB("qdT", b)], w=[qdT])
            for g in range(2):
                P.dma(kd2.t[64 * g:64 * g + 64, :], kdT[b][:, :], r=[HB("kdT", b)], w=[kd2])
            P.dma(vd.t[:, :, :], vds[b].rearrange("(t p) f -> p t f", p=128), r=[HB("vds", b)], w=[vd])
            def sel_(qt):
                return ((qt + 1) * 128, sc[qt % 2], st[qt % 2], steps[qt % 2], cnt[qt % 2], mb[qt % 2], MT[qt % 2],
                        g2s[qt % 2], jks[qt % 2])

            def phase1(qt):
                kl, s_, st_, stp, cn, m_, mt_, g2, jk = sel_(qt)
                for kb in range((kl + 511) // 512):
                    k0 = kb * 512
                    nk = min(512, kl - k0)
                    for hi in range(8):
                        n_[0] += 1
                        pI = ps[n_[0] % 3]
                        r_ = rl[n_[0] % 3]
                        g = hi % 4
                        kw = {"tile_position": (96, 0)} if g == 3 else {}
                        P.op("pe", lambda e, pI=pI, g=g, hi=hi, kw=kw, k0=k0, nk=nk: e.matmul(
                            pI.t[:, 0:nk], lhsT=qiT.t[32 * g:32 * g + 32, hi // 4, qt * 128:(qt + 1) * 128],
                            rhs=ki4.t[32 * g:32 * g + 32, k0:k0 + nk], start=True, stop=True, **kw),
                            r=[qiT, ki4], w=[pI])
                        act(r_.t[:, 0:nk], pI.t[:, 0:nk], AF.Relu, [pI], [r_])
                        if hi == 0:
                            P.op("dve", lambda e, r_=r_, k0=k0, nk=nk: e.tensor_scalar(
                                out=s_.t[:, k0:k0 + nk], in0=r_.t[:, 0:nk], scalar1=wi_.t[:, qt, 0:1], scalar2=None,
                                op0=ALU.mult), r=[r_, wi_], w=[s_])
                        else:
                            P.op("dve", lambda e, r_=r_, k0=k0, nk=nk, hi=hi: e.scalar_tensor_tensor(
                                out=s_.t[:, k0:k0 + nk], in0=r_.t[:, 0:nk], scalar=wi_.t[:, qt, hi:hi + 1],
                                in1=s_.t[:, k0:k0 + nk], op0=ALU.mult, op1=ALU.add), r=[r_, wi_, s_], w=[s_])
                ck(20)
                P.op("dve", lambda e: e.tensor_reduce(out=st_.t[:, 0:1], in_=s_.t[:, 0:kl], axis=AX.X, op=ALU.min),
                     r=[s_], w=[st_])
                P.op("dve", lambda e: e.tensor_tensor(out=s_.t[:, kl - 128:kl], in0=s_.t[:, kl - 128:kl], in1=negm,
                                                      op=ALU.add), r=[s_, cst], w=[s_])
                P.op("dve", lambda e: e.tensor_reduce(out=st_.t[:, 1:2], in_=s_.t[:, 0:kl], axis=AX.X, op=ALU.max),
                     r=[s_, st_], w=[st_])
                P.op("dve", lambda e: e.tensor_tensor(out=st_.t[:, 2:3], in0=st_.t[:, 1:2], in1=st_.t[:, 0:1],
                                                      op=ALU.subtract), r=[st_], w=[st_])
                P.op("dve", lambda e: e.tensor_scalar(out=st_.t[:, 2:3], in0=st_.t[:, 2:3], scalar1=1.02, scalar2=0.002,
                                                      op0=ALU.mult, op1=ALU.add), r=[st_], w=[st_])
                P.op("dve", lambda e: e.scalar_tensor_tensor(out=st_.t[:, 3:4], in0=st_.t[:, 2:3], scalar=-0.5,
                                                             in1=st_.t[:, 1:2], op0=ALU.mult, op1=ALU.add),
                     r=[st_], w=[st_])
                P.op("dve", lambda e: e.tensor_scalar(out=stp.t[:, :], in0=pow2, scalar1=st_.t[:, 2:3], scalar2=None,
                                                      op0=ALU.mult), r=[st_, cst], w=[stp])
                ck(21)
                P.op("pool", lambda e: e.memset(cn.t[:, :], 0.0), w=[cn])

            def phase2(qt, itn):
                kl, s_, st_, stp, cn, m_, mt_, g2, jk = sel_(qt)
                P.op("dve", lambda e, itn=itn: e.tensor_scalar(
                    out=jk.t[:, 0:kl], in0=s_.t[:, 0:kl], scalar1=st_.t[:, 3:4], scalar2=zero_c, op0=ALU.is_ge,
                    op1=ALU.add, accum_out=cn.t[:, itn:itn + 1]), r=[s_, st_, cn], w=[jk, cn])
                P.op("dve", lambda e, itn=itn: e.tensor_scalar(
                    out=g2.t[:, :], in0=cn.t[:, itn:itn + 1], scalar1=topk_c, scalar2=stp.t[:, itn:itn + 1],
                    op0=ALU.is_ge, op1=ALU.mult), r=[cn, stp, cst], w=[g2])
                P.op("dve", lambda e, itn=itn: e.scalar_tensor_tensor(
                    out=st_.t[:, 3:4], in0=st_.t[:, 3:4], scalar=stp.t[:, itn + 1:itn + 2], in1=g2.t[:, :],
                    op0=ALU.subtract, op1=ALU.add), r=[st_, stp, g2], w=[st_])

            def phase3(qt):
                kl, s_, st_, stp, cn, m_, mt_, g2, jk = sel_(qt)
                P.op("dve", lambda e: e.tensor_tensor(out=st_.t[:, 3:4], in0=st_.t[:, 3:4], in1=stp.t[:, NIT:NIT + 1],
                                                      op=ALU.subtract), r=[st_, stp], w=[st_])
                P.op("dve", lambda e: e.tensor_scalar(out=m_.t[:, 0:kl], in0=s_.t[:, 0:kl], scalar1=st_.t[:, 3:4],
                                                      scalar2=None, op0=ALU.is_ge), r=[s_, st_], w=[m_])
                ck(22)
                for k8 in range((qt + 8) // 8):
                    nt_ = min(8, qt + 1 - k8 * 8)
                    pT = ps[3 + (n_[0] + k8) % 2]
                    pTv = pT.t[:, :].bitcast(BF16)
                    for u in range(nt_):
                        kt = k8 * 8 + u
                        P.op("pe", lambda e, pTv=pTv, u=u, kt=kt: e.transpose(
                            pTv[:, u * 128:(u + 1) * 128], m_.t[:, kt * 128:(kt + 1) * 128], ident_bf),
                            r=[m_, cbf], w=[pT])
                    act(mt_.t[:, k8 * 8:k8 * 8 + nt_, :], pTv[:, 0:nt_ * 128].rearrange("p (u q) -> p u q", q=128),
                        AF.Identity, [pT], [mt_])
                ck(23)
                pO = ps[5]; pSm = ps[6]

                def d_s(kt):
                    n_[0] += 1
                    pAB = (ps[0], ps[1]) if n_[0] % 2 == 0 else (ps[2], ps[7])
                    p_ = pd[n_[0] % 3]
                    for h in range(4):
                        hb0 = (h % 2) * 64
                        pS = pAB[h % 2]
                        P.op("pe", lambda e: e.matmul(
                            pS.t[:, (h // 2) * 128:(h // 2 + 1) * 128], lhsT=kd2.t[hb0:hb0 + 64, kt * 128:(kt + 1) * 128],
                            rhs=qdT.t[hb0:hb0 + 64, h // 2, qt * 128:(qt + 1) * 128], start=True, stop=True),
                            r=[kd2, qdT], w=[pS])
                    for g in range(2):
                        act(p_.t[:, 2 * g:2 * g + 2, :], pAB[g].t[:, 0:256].rearrange("p (h q) -> p h q", q=128), AF.Exp,
                            [pAB[g]], [p_], scale=SC_D)
                    for hp in range(4):
                        P.op("dve", lambda e: e.tensor_tensor(
                            out=p_.t[:, hp, :], in0=p_.t[:, hp, :], in1=mt_.t[:, kt, :], op=ALU.mult),
                            r=[p_, mt_], w=[p_])
                    return p_

                def d_v(kt, p_):
                    pr = p_.t[:, :, :].rearrange("p h q -> p (h q)")
                    P.op("pe", lambda e: e.matmul(pO.t[0:64, :], lhsT=vd.t[:, kt, :], rhs=pr,
                                                  start=(kt == 0), stop=(kt == qt)), r=[vd, p_], w=[pO])
                    P.op("pe", lambda e: e.matmul(pSm.t[0:64, :], lhsT=ones_bf.t[:, 0:64], rhs=pr,
                                                  start=(kt == 0), stop=(kt == qt)), r=[ones_bf, p_], w=[pSm])

                nxt = d_s(0)
                for kt in range(qt + 1):
                    cur = nxt
                    if kt + 1 <= qt:
                        nxt = d_s(kt + 1)
                    d_v(kt, cur)
                P.op("dve", lambda e: e.reciprocal(out=rcd.t[:, :], in_=pSm.t[0:64, :]), r=[pSm], w=[rcd])
                o_ = odo[qt % 2]
                P.op("dve", lambda e, o_=o_: e.tensor_tensor(out=o_.t[:, :, :].rearrange("p h q -> p (h q)"),
                                                             in0=pO.t[0:64, :], in1=rcd.t[:, :], op=ALU.mult),
                     r=[pO, rcd], w=[o_])
                for hp in range(4):
                    h = (hp % 2) * 2 + hp // 2
                    hb0 = (h % 2) * 64
                    P.dma(fm["odT"][b][hb0:hb0 + 64, h // 2, qt * 128:(qt + 1) * 128], o_.t[:, hp, :], r=[o_],
                          w=[HB("odT", b)])

            for qp in range(0, T, 2):
                for qt in (qp, qp + 1):
                    phase1(qt)
                for itn in range(NIT):
                    for qt in (qp, qp + 1):
                        phase2(qt, itn)
                for qt in (qp, qp + 1):
                    phase3(qt)
        stage_reset()

        Wg = sb([128, 8, 4096], BF16, "Wg"); Wbr = sb([128, 8, D], BF16, "Wbr"); Wo = sb([128, 8, D], BF16, "Wo")
        stg = [sb([128, 8, 256], F32, "stg") for _ in range(2)]
        wv = W["w_in"].t[l].rearrange("(kc p) n -> p kc n", p=128)
        load_cast(Wg, lambda c0, n: Wg.t[:, :, c0:c0 + n], lambda c0, n: wv[:, :, 2728 + c0:2728 + c0 + n],
                  [((c0, 256), (c0, 256)) for c0 in range(0, 4096, 256)], stg, ["pool", "dve"])
        wbv = W["w_branch"].t[l].rearrange("i (kc p) n -> p (i kc) n", p=128)
        load_cast(Wbr, lambda c0, n: Wbr.t[:, :, c0:c0 + n], lambda c0, n: wbv[:, :, c0:c0 + n],
                  [((c0, 256), (c0, 256)) for c0 in range(0, D, 256)], stg, ["pool", "dve"])
        wov = W["w_out"].t[l].rearrange("(kc p) n -> p kc n", p=128)
        load_cast(Wo, lambda c0, n: Wo.t[:, :, c0:c0 + n], lambda c0, n: wov[:, :, c0:c0 + n],
                  [((c0, 256), (c0, 256)) for c0 in range(0, D, 256)], stg, ["pool", "dve"])
        GG = sb([128, D], F32, "GG"); tmpb = sb([128, D], F32, "tmpb")
        hTb = [sb([128, 8, 512], BF16, "hT") for _ in range(2)]
        oTb = [[sb([128, 2, 512], BF16, "oT") for _ in range(4)] for _ in range(2)]
        sgm = [sb([128, 512], F32, "sgm") for _ in range(2)]
        acc = sb([128, 512], F32, "acc"); tm = sb([128, 512], F32, "tm")
        mT = sb([128, 8, 512], BF16, "mT")
        ysb = [sb([128, D], F32, "ysb") for _ in range(2)]
        xts = [sb([128, D], F32, "xt") for _ in range(2)]
        junk = sb([128, D], BF16, "junk"); ss = sb([128, 2], F32, "ss")
        onames = ("oaT", "obT", "ocT", "odT")
        n3 = [0]
        for b in range(NSEQ):
            make_GG(l, b, "mix_post_g", 2, GG, tmpb)
            for tb in range(NB):
                t0 = tb * 512
                hT = hTb[tb % 2]; oT = oTb[tb % 2]
                P.dma(hT.t[:, :, :], hTs[b][:, :, t0:t0 + 512], r=[HB("hTs", b)], w=[hT])
                for i in range(4):
                    P.dma(oT[i].t[:, :, :], fm[onames[i]][b][:, :, t0:t0 + 512], r=[HB(onames[i], b)], w=[oT[i]])
                for oc in range(8):
                    for i in range(4):
                        n3[0] += 1
                        pG = ps[n3[0] % 2]; pB = ps[2 + n3[0] % 2]; sg_ = sgm[n3[0] % 2]
                        for kc in range(8):
                            P.op("pe", lambda e, pG=pG, kc=kc, i=i, oc=oc: e.matmul(
                                pG.t[:, :], lhsT=Wg.t[:, kc, i * D + oc * 128:i * D + (oc + 1) * 128], rhs=hT.t[:, kc, :],
                                start=(kc == 0), stop=(kc == 7)), r=[Wg, hT], w=[pG])
                        for kc in range(2):
                            P.op("pe", lambda e, pB=pB, kc=kc, i=i, oc=oc: e.matmul(
                                pB.t[:, :], lhsT=Wbr.t[:, i * 2 + kc, oc * 128:(oc + 1) * 128], rhs=oT[i].t[:, kc, :],
                                start=(kc == 0), stop=(kc == 1)), r=[Wbr, oT[i]], w=[pB])
                        act(sg_.t[:, :], pG.t[:, :], AF.Sigmoid, [pG], [sg_])
                        if i == 0:
                            P.op("dve", lambda e, sg_=sg_, pB=pB: e.tensor_tensor(out=acc.t[:, :], in0=sg_.t[:, :],
                                                                                  in1=pB.t[:, :], op=ALU.mult),
                                 r=[sg_, pB], w=[acc])
                        else:
                            P.op("dve", lambda e, sg_=sg_, pB=pB: e.tensor_tensor(out=tm.t[:, :], in0=sg_.t[:, :],
                                                                                  in1=pB.t[:, :], op=ALU.mult),
                                 r=[sg_, pB], w=[tm])
                            if i < 3:
                                P.op("dve", lambda e: e.tensor_tensor(out=acc.t[:, :], in0=acc.t[:, :], in1=tm.t[:, :],
                                                                       op=ALU.add), r=[acc, tm], w=[acc])
                            else:
                                P.op("dve", lambda e, oc=oc: e.tensor_tensor(out=mT.t[:, oc, :], in0=acc.t[:, :],
                                                                              in1=tm.t[:, :], op=ALU.add),
                                     r=[acc, tm], w=[mT])
                for tt in range(4):
                    y_ = ysb[tt % 2]; xt = xts[tt % 2]
                    P.dma(xt.t[:, :], x_src[b, t0 + tt * 128:t0 + (tt + 1) * 128, :], r=[HB("x%d" % l, b)], w=[xt])
                    for hf in range(2):
                        pY = ps[4 + hf]
                        for kc in range(8):
                            P.op("pe", lambda e, pY=pY, kc=kc, hf=hf, tt=tt: e.matmul(
                                pY.t[:, :], lhsT=mT.t[:, kc, tt * 128:(tt + 1) * 128], rhs=Wo.t[:, kc, hf * 512:(hf + 1) * 512],
                                start=(kc == 0), stop=(kc == 7)), r=[mT, Wo], w=[pY])
                        act(y_.t[:, hf * 512:(hf + 1) * 512], pY.t[:, :], AF.Identity, [pY], [y_])
                    post_norm_residual(y_, xt, GG, ss, junk, xmid[b, t0 + tt * 128:t0 + (tt + 1) * 128, :],
                                       HB("xmid%d" % l, b))
        stage_reset()

        Wu = sb([128, 8, 2 * DFF], BF16, "Wu")
        stg = [sb([128, 8, 256], F32, "stg") for _ in range(2)]
        wv = W["w_up"].t[l].rearrange("(kc p) n -> p kc n", p=128)
        load_cast(Wu, lambda c0, n: Wu.t[:, :, c0:c0 + n], lambda c0, n: wv[:, :, c0:c0 + n],
                  [((c0, 256), (c0, 256)) for c0 in range(0, 2 * DFF, 256)], stg, ["pool", "dve"])
        fcw = sb([128, 44, 3], F32, "fcw"); fcb = sb([128, 44], F32, "fcb")
        for j in range(3):
            P.dma(fcw.t[:, :, j], W["ffn_conv_w"].t[l][j].rearrange("(c p) -> p c", p=128), w=[fcw],
                  allow_slow_non_contiguous=True)
        P.dma(fcb.t[:, :], W["ffn_conv_b"].t[l].rearrange("(c p) -> p c", p=128), w=[fcb],
              allow_slow_non_contiguous=True)
        Acol = sb([128, 8], F32, "Acol"); Bcol = sb([128, 8], F32, "Bcol"); tmpc = sb([128, 8], F32, "tmpc")
        xts = [sb([128, D], F32, "xt") for _ in range(2)]
        xs = sb([128, D], BF16, "xs"); junk = sb([128, D], BF16, "junk"); ss = sb([128, 2], F32, "ss")
        hTb = [sb([128, 8, 512], BF16, "hT") for _ in range(2)]
        halo = sb([128, 44, 2], F32, "halo")
        ub = [sb([128, 514], F32, "ub") for _ in range(4)]
        c1_ = [sb([128, 512], F32, "c1") for _ in range(2)]
        gs = sb([128, 512], F32, "gs")
        aT = [sb([128, 22, 512], BF16, "aT") for _ in range(2)]
        n4 = [0]
        for b in range(NSEQ):
            make_AB(l, b, "ffn_pre_g", 3, 4, Acol, Bcol, tmpc)
            P.op("pool", lambda e: e.memset(halo.t[:, :, :], 0.0), w=[halo])
            for tb in range(NB):
                t0 = tb * 512
                hT = hTb[tb % 2]; a_ = aT[tb % 2]
                for tt in range(4):
                    xt = xts[tt % 2]
                    P.dma(xt.t[:, :], xmid[b, t0 + tt * 128:t0 + (tt + 1) * 128, :], r=[HB("xmid%d" % l, b)], w=[xt])
                    norm_transpose(xt, hT, tt, Acol, Bcol, xs, ss, junk, 7)
                for j in range(22):
                    res = []
                    for gv in range(2):
                        ch = gv * 22 + j
                        n4[0] += 1
                        pU = ps[n4[0] % 4]; u_ = ub[n4[0] % 4]; c_ = c1_[gv]
                        for kc in range(8):
                            P.op("pe", lambda e, pU=pU, kc=kc, ch=ch: e.matmul(
                                pU.t[:, :], lhsT=Wu.t[:, kc, ch * 128:(ch + 1) * 128], rhs=hT.t[:, kc, :],
                                start=(kc == 0), stop=(kc == 7)), r=[Wu, hT], w=[pU])
                        P.op("pool", lambda e, u_=u_, ch=ch: e.tensor_copy(out=u_.t[:, 0:2], in_=halo.t[:, ch, :]),
                             r=[halo], w=[u_])
                        act(u_.t[:, 2:514], pU.t[:, :], AF.Identity, [pU], [u_])
                        P.op("pool", lambda e, u_=u_, ch=ch: e.tensor_copy(out=halo.t[:, ch, :], in_=u_.t[:, 512:514]),
                             r=[u_], w=[halo])
                        P.op("dve", lambda e, u_=u_, c_=c_, ch=ch: e.tensor_scalar(
                            out=c_.t[:, :], in0=u_.t[:, 2:514], scalar1=fcw.t[:, ch, 2:3], scalar2=fcb.t[:, ch:ch + 1],
                            op0=ALU.mult, op1=ALU.add), r=[u_, fcw, fcb], w=[c_])
                        P.op("dve", lambda e, u_=u_, c_=c_, ch=ch: e.scalar_tensor_tensor(
                            out=c_.t[:, :], in0=u_.t[:, 1:513], scalar=fcw.t[:, ch, 1:2], in1=c_.t[:, :],
                            op0=ALU.mult, op1=ALU.add), r=[u_, fcw, c_], w=[c_])
                        P.op("dve", lambda e, u_=u_, c_=c_, ch=ch: e.scalar_tensor_tensor(
                            out=c_.t[:, :], in0=u_.t[:, 0:512], scalar=fcw.t[:, ch, 0:1], in1=c_.t[:, :],
                            op0=ALU.mult, op1=ALU.add), r=[u_, fcw, c_], w=[c_])
                        res.append(c_)
                    act(gs.t[:, :], res[0].t[:, :], AF.Silu, [res[0]], [gs])
                    P.op("dve", lambda e, j=j, a_=a_, rv=res[1]: e.tensor_tensor(out=a_.t[:, j, :], in0=gs.t[:, :],
                                                                                 in1=rv.t[:, :], op=ALU.mult),
                         r=[gs, res[1]], w=[a_])
                P.dma(aTs[b][:, :, t0:t0 + 512], a_.t[:, :, :], r=[a_], w=[HB("aTs", b)])
        stage_reset()

        Wd = sb([128, 22, D], BF16, "Wd")
        stg = [sb([128, 8, 512], F32, "stg") for _ in range(2)]
        wdv = W["w_down"].t[l].rearrange("(kc p) n -> p kc n", p=128)
        pieces = []
        for k0 in (0, 8, 16):
            nk = min(8, 22 - k0)
            for c0 in (0, 512):
                pieces.append(((k0, nk, c0), (k0, nk, c0)))
        load_cast(Wd, lambda k0, nk, c0: Wd.t[:, k0:k0 + nk, c0:c0 + 512],
                  lambda k0, nk, c0: wdv[:, k0:k0 + nk, c0:c0 + 512], pieces, stg, ["pool", "dve"])
        GG = sb([128, D], F32, "GG"); tmpb = sb([128, D], F32, "tmpb")
        aT = [sb([128, 22, 512], BF16, "aT") for _ in range(2)]
        ysb = [sb([128, D], F32, "ysb") for _ in range(2)]
        xts = [sb([128, D], F32, "xt") for _ in range(2)]
        junk = sb([128, D], BF16, "junk"); ss = sb([128, 2], F32, "ss")
        for b in range(NSEQ):
            make_GG(l, b, "ffn_post_g", 5, GG, tmpb)
            for tb in range(NB):
                t0 = tb * 512
                a_ = aT[tb % 2]
                P.dma(a_.t[:, :, :], aTs[b][:, :, t0:t0 + 512], r=[HB("aTs", b)], w=[a_])
                for tt in range(4):
                    y_ = ysb[tt % 2]; xt = xts[tt % 2]
                    P.dma(xt.t[:, :], xmid[b, t0 + tt * 128:t0 + (tt + 1) * 128, :], r=[HB("xmid%d" % l, b)], w=[xt])
                    for hf in range(2):
                        pY = ps[(tt * 2 + hf) % 4]
                        for kc in range(22):
                            P.op("pe", lambda e, pY=pY, kc=kc, hf=hf, tt=tt: e.matmul(
                                pY.t[:, :], lhsT=a_.t[:, kc, tt * 128:(tt + 1) * 128], rhs=Wd.t[:, kc, hf * 512:(hf + 1) * 512],
                                start=(kc == 0), stop=(kc == 21)), r=[a_, Wd], w=[pY])
                        act(y_.t[:, hf * 512:(hf + 1) * 512], pY.t[:, :], AF.Identity, [pY], [y_])
                    post_norm_residual(y_, xt, GG, ss, junk, x_dst[b, t0 + tt * 128:t0 + (tt + 1) * 128, :],
                                       HB("x%d" % (l + 1), b) if l < DEPTH - 1 else out_t)
        stage_reset()


_CACHE = {}


def kernel(**inputs):
    NCORES = 8
    x = np.ascontiguousarray(inputs["x"], dtype=np.float32)
    Bt, S, _ = x.shape
    NSEQ = Bt // NCORES
    DEPTH = inputs["ada_w"].shape[0]
    key = (S, NSEQ, DEPTH)
    if key not in _CACHE:
        _CACHE[key] = build(S, NSEQ, DEPTH)[0]
    nc = _CACHE[key]
    consts = make_consts(S)
    wnames = ("ada_w", "ada_b", "mix_pre_g", "mix_post_g", "ffn_pre_g", "ffn_post_g", "w_in", "conv_a_w",
              "conv_a_b", "conv_a_ln_g", "conv_a_ln_b", "lam_q1", "lam_k1", "lam_q2", "lam_k2", "diff_subln_g",
              "w_branch", "w_out", "w_up", "ffn_conv_w", "ffn_conv_b", "w_down")
    shared = {n: np.ascontiguousarray(inputs[n], dtype=np.float32) for n in wnames}
    c = np.ascontiguousarray(inputs["c"], dtype=np.float32)
    pos = np.ascontiguousarray(inputs["positions"], dtype=np.int32)
    in_maps = []
    for i in range(NCORES):
        m = dict(shared)
        m["x"] = x[i * NSEQ:(i + 1) * NSEQ]
        m["c"] = c[i * NSEQ:(i + 1) * NSEQ]
        m["positions"] = pos[i * NSEQ:(i + 1) * NSEQ]
        m["consts"] = consts
        in_maps.append(m)
    res = run_bass_kernel_spmd(nc, in_maps, core_ids=list(range(NCORES)))
    return np.concatenate([r["out"] for r in res.results], axis=0).astype(np.float32)
```
